# Optimizing a Trainium2 kernel written in Bass

```python
import jax, jax.numpy as jnp
from jax import lax
import numpy as np

D_MODEL = 1024
BATCH = 4
SEQ = 8192
DEPTH = 1

GRID_W = 64
N_Q_HEADS = 8
N_KV_HEADS = 2
HEAD_DIM = 64
ATTN_WIDTH = N_Q_HEADS * HEAD_DIM
KV_WIDTH = N_KV_HEADS * HEAD_DIM
Q_BLOCK = 128
ROPE_THETA = 10000.0
DN_HEADS = 8
DN_HEAD_DIM = 64
DN_WIDTH = DN_HEADS * DN_HEAD_DIM
CONV_WIDTH = 5
CHUNK = 64
N_BRANCHES = 2
N_EXPERTS = 16
CAPACITY_FACTOR = 2
D_EXPERT = 1024
EPS = 1e-6
IN_SPLITS = (ATTN_WIDTH, KV_WIDTH, KV_WIDTH, DN_WIDTH, DN_WIDTH, DN_WIDTH, DN_WIDTH, 2 * DN_HEADS, 2 * DN_HEADS, N_BRANCHES * D_MODEL)
D_IN = sum(IN_SPLITS)

kernel_name = "hybrid_gqa_gdn_ec_moe_block"


def rmsnorm(x, w):
    xf = x.astype(jnp.float32)
    y = xf * lax.rsqrt(jnp.mean(xf * xf, axis=-1, keepdims=True) + EPS)
    return (y * w.astype(jnp.float32)).astype(x.dtype)


def l2norm(x):
    xf = x.astype(jnp.float32)
    return xf * lax.rsqrt(jnp.sum(xf * xf, axis=-1, keepdims=True) + EPS)


def heads(t, n):
    B, S, _ = t.shape
    return t.reshape(B, S, n, -1).transpose(0, 2, 1, 3)


def rope_axis(x, pos):
    d = x.shape[-1]
    freqs = ROPE_THETA ** (-(jnp.arange(d // 2, dtype=jnp.float32) * 2.0 / d))
    ang = pos.astype(jnp.float32)[:, None] * freqs[None, :]
    cos = jnp.concatenate([jnp.cos(ang), jnp.cos(ang)], axis=-1)
    sin = jnp.concatenate([jnp.sin(ang), jnp.sin(ang)], axis=-1)
    xf = x.astype(jnp.float32)
    x1, x2 = xf[..., : d // 2], xf[..., d // 2 :]
    rot = jnp.concatenate([-x2, x1], axis=-1)
    return (xf * cos + rot * sin).astype(x.dtype)


def axial_rope(x, row, col):
    hd = x.shape[-1] // 2
    return jnp.concatenate([rope_axis(x[..., :hd], row), rope_axis(x[..., hd:], col)], axis=-1)


def block_attention(q, k, v):
    B, Hq, S, dh = q.shape
    Hkv = k.shape[1]
    G = Hq // Hkv
    nb = S // Q_BLOCK
    qb = q.reshape(B, Hkv, G, nb, Q_BLOCK, dh).transpose(3, 0, 1, 2, 4, 5).astype(jnp.float32)
    kf = k.astype(jnp.float32)
    vf = v.astype(jnp.float32)
    scale = dh ** -0.5

    def one_block(qi):
        s = jnp.einsum('bhgqd,bhkd->bhgqk', qi, kf) * scale
        p = jax.nn.softmax(s, axis=-1)
        return jnp.einsum('bhgqk,bhkd->bhgqd', p, vf)

    o = lax.map(one_block, qb)
    return o.transpose(1, 2, 3, 0, 4, 5).reshape(B, Hq, S, dh).astype(q.dtype)


def short_conv(x, w):
    K, C = w.shape
    return lax.conv_general_dilated(x, w[:, None, :], window_strides=(1,), padding=[(K // 2, K // 2)],
                                    dimension_numbers=('NWC', 'WIO', 'NWC'), feature_group_count=C)


def gated_delta_chunked(q, k, v, beta, g):
    B, H, S, dk = q.shape
    dv = v.shape[-1]
    N = S // CHUNK
    q = q.reshape(B, H, N, CHUNK, dk)
    k = k.reshape(B, H, N, CHUNK, dk)
    v = v.reshape(B, H, N, CHUNK, dv)
    beta = beta.reshape(B, H, N, CHUNK)
    gc = jnp.cumsum(g.reshape(B, H, N, CHUNK), axis=-1)
    idx = jnp.arange(CHUNK)
    incl = idx[:, None] >= idx[None, :]
    strict = idx[:, None] > idx[None, :]
    decay = jnp.exp(jnp.where(incl, gc[..., :, None] - gc[..., None, :], -jnp.inf))
    kb = k * beta[..., None]
    vb = v * beta[..., None]
    L = jnp.where(strict, jnp.einsum('bhncd,bhnsd->bhncs', kb, k) * decay, 0.0)
    eye = jnp.eye(CHUNK, dtype=jnp.float32)
    T = lax.linalg.triangular_solve(eye + L, jnp.broadcast_to(eye, L.shape), left_side=True, lower=True,
                                    unit_diagonal=True)
    u = T @ vb
    w = T @ (kb * jnp.exp(gc)[..., None])
    a_intra = jnp.where(incl, jnp.einsum('bhncd,bhnsd->bhncs', q, k) * decay, 0.0)
    qg = q * jnp.exp(gc)[..., None]
    kd = k * jnp.exp(gc[..., -1:] - gc)[..., None]
    gl = jnp.exp(gc[..., -1])
    xs = tuple(jnp.moveaxis(t, 2, 0) for t in (u, w, a_intra, qg, kd, gl))

    def step(state, inp):
        u_n, w_n, a_n, qg_n, kd_n, gl_n = inp
        v_new = u_n - w_n @ state
        o_n = qg_n @ state + a_n @ v_new
        state = state * gl_n[..., None, None] + jnp.einsum('bhck,bhcv->bhkv', kd_n, v_new)
        return state, o_n

    _, o = lax.scan(step, jnp.zeros((B, H, dk, dv), jnp.float32), xs)
    return jnp.moveaxis(o, 0, 2).reshape(B, H, S, dv)


def hybrid_mixer(h, row, col, w_in, q_norm_w, k_norm_w, conv_w, a_log, dt_bias, dn_norm_w, w_attn_up, w_dn_up, w_o):
    B, S, D = h.shape
    proj = h @ w_in
    aq, ak, av, dq, dk, dv, dz, b_raw, a_raw, g_raw = jnp.split(proj, np.cumsum(IN_SPLITS)[:-1].tolist(), axis=-1)

    q = axial_rope(rmsnorm(heads(aq, N_Q_HEADS), q_norm_w), row, col)
    k = axial_rope(rmsnorm(heads(ak, N_KV_HEADS), k_norm_w), row, col)
    v = heads(av, N_KV_HEADS)
    attn = block_attention(q, k, v).transpose(0, 2, 1, 3).reshape(B, S, ATTN_WIDTH)

    qkv = jax.nn.silu(short_conv(jnp.concatenate([dq, dk, dv], axis=-1), conv_w))
    cq, ck, cv = jnp.split(qkv, 3, axis=-1)
    qd = l2norm(heads(cq, DN_HEADS)) * (DN_HEAD_DIM ** -0.5)
    kd = l2norm(heads(ck, DN_HEADS))
    vd = heads(cv, DN_HEADS).astype(jnp.float32)
    beta = jax.nn.sigmoid(b_raw.astype(jnp.float32)).reshape(B, S, 2, DN_HEADS).transpose(2, 0, 3, 1)
    a_in = a_raw.astype(jnp.float32).reshape(B, S, 2, DN_HEADS) + dt_bias.astype(jnp.float32)
    g = -jnp.exp(a_log.astype(jnp.float32))[:, None, :, None] * jax.nn.softplus(a_in.transpose(2, 0, 3, 1))
    flip = lambda t: jnp.flip(t, axis=2)
    o_fwd = gated_delta_chunked(qd, kd, vd, beta[0], g[0])
    o_bwd = flip(gated_delta_chunked(flip(qd), flip(kd), flip(vd), flip(beta[1]), flip(g[1])))
    o = (o_fwd + o_bwd).transpose(0, 2, 1, 3)
    o = rmsnorm(o, dn_norm_w) * jax.nn.silu(dz.astype(jnp.float32).reshape(B, S, DN_HEADS, DN_HEAD_DIM))
    dn = o.reshape(B, S, DN_WIDTH).astype(h.dtype)

    gate_a, gate_d = jnp.split(jax.nn.sigmoid(g_raw), N_BRANCHES, axis=-1)
    merged = gate_a * (attn @ w_attn_up) + gate_d * (dn @ w_dn_up)
    return merged @ w_o


def expert_choice_ffn(h, w_router, w_gate, w_up, w_down):
    B, S, D = h.shape
    cap = CAPACITY_FACTOR * S // N_EXPERTS
    aff = jax.nn.softmax((h @ w_router).astype(jnp.float32), axis=-1)
    gates, idx = lax.top_k(aff.transpose(0, 2, 1), cap)
    bidx = jnp.arange(B)[:, None, None]
    xe = h[bidx, idx]
    hg = jnp.einsum('becd,edf->becf', xe, w_gate)
    hu = jnp.einsum('becd,edf->becf', xe, w_up)
    y = jnp.einsum('becf,efd->becd', jax.nn.silu(hg) * hu, w_down)
    y = y * gates[..., None].astype(y.dtype)
    return jnp.zeros_like(h).at[bidx, idx].add(y)


def setup_inputs(seed: int = 0) -> dict:
    key = jax.random.key(seed)
    ks = jax.random.split(key, 24)
    f32 = jnp.float32

    def nrm(k, shape, fan_in):
        return jax.random.normal(k, shape, f32) * (fan_in ** -0.5)

    def gain(k, shape):
        return 1.0 + 0.02 * jax.random.normal(k, shape, f32)

    dt = jnp.exp(jax.random.uniform(ks[11], (DEPTH, 2, DN_HEADS), f32, np.log(1e-3), np.log(1e-1)))
    return {
        'x': jax.random.normal(ks[0], (BATCH, SEQ, D_MODEL), f32),
        'c': jax.random.normal(ks[1], (BATCH, D_MODEL), f32),
        'w_ada': nrm(ks[2], (DEPTH, D_MODEL, 6 * D_MODEL), D_MODEL),
        'b_ada': 0.01 * jax.random.normal(ks[3], (DEPTH, 6 * D_MODEL), f32),
        'norm1_w': gain(ks[4], (DEPTH, D_MODEL)),
        'w_in': nrm(ks[5], (DEPTH, D_MODEL, D_IN), D_MODEL),
        'q_norm_w': gain(ks[6], (DEPTH, HEAD_DIM)),
        'k_norm_w': gain(ks[7], (DEPTH, HEAD_DIM)),
        'conv_w': nrm(ks[8], (DEPTH, CONV_WIDTH, 3 * DN_WIDTH), CONV_WIDTH),
        'a_log': jnp.log(jax.random.uniform(ks[9], (DEPTH, 2, DN_HEADS), f32, 1.0, 16.0)),
        'dt_bias': dt + jnp.log(-jnp.expm1(-dt)),
        'dn_norm_w': gain(ks[10], (DEPTH, DN_HEAD_DIM)),
        'w_attn_up': nrm(ks[12], (DEPTH, ATTN_WIDTH, D_MODEL), ATTN_WIDTH),
        'w_dn_up': nrm(ks[13], (DEPTH, DN_WIDTH, D_MODEL), DN_WIDTH),
        'w_o': nrm(ks[14], (DEPTH, D_MODEL, D_MODEL), D_MODEL),
        'norm2_w': gain(ks[15], (DEPTH, D_MODEL)),
        'w_router': nrm(ks[16], (DEPTH, D_MODEL, N_EXPERTS), D_MODEL),
        'w_gate': nrm(ks[17], (DEPTH, N_EXPERTS, D_MODEL, D_EXPERT), D_MODEL),
        'w_up': nrm(ks[18], (DEPTH, N_EXPERTS, D_MODEL, D_EXPERT), D_MODEL),
        'w_down': nrm(ks[19], (DEPTH, N_EXPERTS, D_EXPERT, D_MODEL), D_EXPERT),
    }


def reference(x, c, w_ada, b_ada, norm1_w, w_in, q_norm_w, k_norm_w, conv_w, a_log, dt_bias, dn_norm_w,
              w_attn_up, w_dn_up, w_o, norm2_w, w_router, w_gate, w_up, w_down):
    B, S, D = x.shape
    rows = S // GRID_W
    row = jnp.broadcast_to(jnp.arange(rows)[:, None], (rows, GRID_W)).reshape(S)
    col = jnp.broadcast_to(jnp.arange(GRID_W)[None, :], (rows, GRID_W)).reshape(S)
    for l in range(DEPTH):
        mod = jax.nn.silu(c) @ w_ada[l] + b_ada[l]
        sh1, sc1, gt1, sh2, sc2, gt2 = [m[:, None, :] for m in jnp.split(mod, 6, axis=-1)]
        h = rmsnorm(x, norm1_w[l]) * (1.0 + sc1) + sh1
        x = x + gt1 * hybrid_mixer(h, row, col, w_in[l], q_norm_w[l], k_norm_w[l], conv_w[l], a_log[l], dt_bias[l],
                                   dn_norm_w[l], w_attn_up[l], w_dn_up[l], w_o[l])
        h2 = rmsnorm(x, norm2_w[l]) * (1.0 + sc2) + sh2
        x = x + gt2 * expert_choice_ffn(h2, w_router[l], w_gate[l], w_up[l], w_down[l])
    return x
```

```python
import numpy as np
import concourse.bass as bass
import concourse.mybir as mybir
from concourse.bass_utils import run_bass_kernel_spmd

F32 = mybir.dt.float32
BF16 = mybir.dt.bfloat16
I32 = mybir.dt.int32
AF = mybir.ActivationFunctionType
ALU = mybir.AluOpType
AX = mybir.AxisListType

ENGS = ("pe", "act", "dve", "pool", "sp")
NDSEM = 8


class Prog:
    def __init__(self, nc, stack):
        self.nc = nc
        self.ops = {e: [] for e in ENGS}
        self.res = {}
        self.waited = {e: {} for e in ENGS}
        self.esem = {e: stack.enter_context(nc.semaphore("es_" + e)) for e in ENGS}
        self.dsem = {}
        self.dcnt = {}
        self.dnext = {}
        for q in ("sp", "pool", "act", "cc"):
            self.dsem[q] = [stack.enter_context(nc.semaphore("ds_%s%d" % (q, i))) for i in range(NDSEM)]
            self.dcnt[q] = [0] * NDSEM
            self.dnext[q] = 0

    def _deps(self, eng, reads, writes):
        deps = []
        for k in reads:
            st = self.res.get(k)
            if st is not None and st["w"] is not None:
                deps.append(st["w"])
        for k in writes:
            st = self.res.get(k)
            if st is not None:
                if st["w"] is not None:
                    deps.append(st["w"])
                deps.extend(st["r"])
        out = []
        wd = self.waited[eng]
        for d in deps:
            if d[0] == "e":
                _, src, idx = d
                if src == "pe" and eng == "pe":
                    continue
                key = ("e", src)
                if wd.get(key, -1) >= idx:
                    continue
                wd[key] = idx
                tgt = self.ops[src][idx]
                assert tgt["sig"] or not tgt.get("frozen"), "dependency on an already emitted, unsignalled op"
                tgt["sig"] = True
                out.append(d)
            else:
                _, q, slot, val = d
                key = ("d", q, slot)
                if wd.get(key, -1) >= val:
                    continue
                wd[key] = val
                out.append(d)
        return out

    def _record(self, dep, reads, writes):
        for k in writes:
            self.res[k] = {"w": dep, "r": []}
        for k in reads:
            st = self.res.setdefault(k, {"w": None, "r": []})
            st["r"].append(dep)
            if len(st["r"]) > 12:
                last = {}
                for d in st["r"]:
                    last[d[:2] if d[0] == "e" else d[:3]] = d
                st["r"] = list(last.values())

    def op(self, eng, fn, r=(), w=()):
        waits = self._deps(eng, r, w)
        idx = len(self.ops[eng])
        self.ops[eng].append({"fn": fn, "waits": waits, "sig": False, "dma": None})
        self._record(("e", eng, idx), r, w)
        return idx

    def pe(self, fn, r=(), w=()):
        return self.op("pe", fn, r, w)

    def act(self, fn, r=(), w=()):
        return self.op("act", fn, r, w)

    def dve(self, fn, r=(), w=()):
        return self.op("dve", fn, r, w)

    def pool(self, fn, r=(), w=()):
        return self.op("pool", fn, r, w)

    def dma(self, q, fn, r=(), w=(), grp=None, inc=16):
        grp = grp or q
        waits = self._deps(q, r, w)
        slot = self.dnext[grp]
        self.dnext[grp] = (slot + 1) % NDSEM
        prev = self.dcnt[grp][slot]
        wd = self.waited[q]
        if prev > 0 and wd.get(("d", grp, slot), -1) < prev:
            wd[("d", grp, slot)] = prev
            waits.append(("d", grp, slot, prev))
        val = prev + inc
        self.dcnt[grp][slot] = val
        idx = len(self.ops[q])
        self.ops[q].append({"fn": fn, "waits": waits, "sig": False, "dma": (grp, slot, inc)})
        self._record(("d", grp, slot, val), r, w)
        return idx

    def barrier(self):
        allk = "__all__"
        last = []
        for e in ENGS:
            if self.ops[e] and not self.ops[e][-1].get("frozen") and self.ops[e][-1]["fn"] is not None:
                last.append(("e", e, len(self.ops[e]) - 1))
        for q in self.dsem:
            for s in range(NDSEM):
                if self.dcnt[q][s] > 0:
                    last.append(("d", q, s, self.dcnt[q][s]))
        self.res[allk] = {"w": None, "r": last}
        for e in ENGS:
            self.op(e, None, r=(), w=(allk,))
            self.res[allk] = {"w": None, "r": last}
        del self.res[allk]

    def flush(self):
        nc = self.nc
        self.barrier()
        if not hasattr(self, "emitted"):
            self.emitted = {e: 0 for e in ENGS}
            self.sigbase = {e: 0 for e in ENGS}
        for e in ENGS:
            c = self.sigbase[e]
            for o in self.ops[e][self.emitted[e]:]:
                if o["sig"] and o["dma"] is None:
                    c += 1
                o["sigval"] = c
            self.sigbase[e] = c

        def run(e, name):
            for o in self.ops[name][self.emitted[name]:]:
                for d in o["waits"]:
                    if d[0] == "e":
                        tgt = self.ops[d[1]][d[2]]
                        assert tgt["sig"] and "sigval" in tgt
                        e.wait_ge(self.esem[d[1]], tgt["sigval"])
                    else:
                        e.wait_ge(self.dsem[d[1]][d[2]], d[3])
                if o["fn"] is None:
                    assert not o["sig"]
                    continue
                ins = o["fn"](e)
                if o["dma"] is not None:
                    q, slot, inc = o["dma"]
                    ins.then_inc(self.dsem[q][slot], inc)
                elif o["sig"]:
                    ins.then_inc(self.esem[name], 1)
                o["fn"] = None
            self.emitted[name] = len(self.ops[name])

        with nc.Block() as block:
            @block.sync
            def _(e):
                run(e, "sp")

            @block.scalar
            def _(e):
                run(e, "act")

            @block.vector
            def _(e):
                run(e, "dve")

            @block.gpsimd
            def _(e):
                run(e, "pool")

            @block.tensor
            def _(e):
                run(e, "pe")
        for e in ENGS:
            for o in self.ops[e]:
                o["frozen"] = True

    def emit(self):
        self.flush()


S = 8192
D = 1024
NCH = S // 512
EPS = 1e-6
PAIRS = [[0, 1], [2, 3], [4, 5], [6, 7]]
SKIP = set()
ATT_LAG = 2
ATT_NS = 3
ATT_NP = 4


class TL:
    def __init__(self, t, k, view=None, psum=False):
        self.t = t if view is None else view
        self.k = k
        self.psum = psum

    def __getitem__(self, idx):
        return self.t[idx]


def _keys(xs):
    return [getattr(x, "k", x) for x in xs]


def _rw(r, w):
    r2 = [x for x in r if not getattr(x, "psum", False)]
    w2 = list(w) + [x for x in r if getattr(x, "psum", False)]
    return _keys(r2), _keys(w2)


class Ctx:
    def __init__(self, nc, P, stack, debug):
        self.nc = nc
        self.P = P
        self.stack = stack
        self.debug = debug
        self.dram = {}
        self.n = 0

    def sb(self, stack, name, shape, dt=F32):
        self.n += 1
        nm = "%s_%d" % (name, self.n)
        return TL(stack.enter_context(self.nc.sbuf_tensor(nm, list(shape), dt)), nm)

    def ps(self, stack, name, shape, dt=F32):
        self.n += 1
        nm = "%s_%d" % (name, self.n)
        full = 512 if dt == F32 else 1024
        t = stack.enter_context(self.nc.psum_tensor(nm, [128, full], dt))
        assert len(shape) == 2 and shape[1] <= full
        return TL(None, nm, view=t[0:shape[0], 0:shape[1]], psum=True)

    def inp(self, name, shape, dt=F32):
        self.dram[name] = self.nc.dram_tensor(name, list(shape), dt, kind="ExternalInput")
        return self.dram[name]

    def scratch(self, name, shape, dt=F32, dbg=False):
        kind = "ExternalOutput" if (dbg and self.debug) else "Internal"
        self.dram[name] = self.nc.dram_tensor(name, list(shape), dt, kind=kind)
        return self.dram[name]

    def mm(self, out, lhsT, rhs, start, stop, r, w):
        self.P.pe(lambda e: e.matmul(out, lhsT=lhsT, rhs=rhs, start=start, stop=stop), *_rw(r, w))

    def tr(self, out, in_, ident, r, w):
        self.P.pe(lambda e: e.transpose(out=out, in_=in_, identity=ident), *_rw(r, w))

    def actv(self, out, in_, func, r, w, bias=None, scale=None, accum=None):
        kw = {}
        if bias is not None:
            kw["bias"] = bias
        if scale is not None:
            kw["scale"] = scale
        if accum is not None:
            kw["accum_out"] = accum
        self.P.act(lambda e: e.activation(out=out, in_=in_, func=func, **kw), *_rw(r, w))

    def ew(self, eng, fn, r, w):
        self.P.op(eng, fn, *_rw(r, w))

    def dma(self, q, out, in_, r, w):
        self.P.dma(q, lambda e: e.dma_start(out=out, in_=in_), *_rw(r, w))


def load_w_bf16(cx, st, wd, ncols, dst, stage, key_prefix, engs=("dve", "pool")):
    src = wd.ap().rearrange("(k p) f -> p k f", p=128)
    i = 0
    for c0 in range(0, ncols, 512):
        c1 = min(ncols, c0 + 512)
        sg = stage[i % 2]
        cx.dma("sp", sg[:, :, 0:c1 - c0], src[:, :, c0:c1], r=[], w=[sg])
        eng = engs[i % len(engs)]
        cx.ew(eng, lambda e, sg=sg, c0=c0, c1=c1: e.tensor_copy(out=dst[:, :, c0:c1], in_=sg[:, :, 0:c1 - c0]),
              r=[sg], w=[dst])
        i += 1


from contextlib import ExitStack


class Consts:
    pass


def setup_consts(cx, C, din):
    st = cx.stack
    C.identf = cx.sb(st, "identf", [128, 128])
    C.identb = cx.sb(st, "identb", [128, 128], BF16)
    cx.ew("pool", lambda e: e.memset(C.identf[:], 1.0), [], [C.identf])
    cx.ew("pool", lambda e: e.affine_select(out=C.identf[:], in_=C.identf[:], pattern=[[-1, 128]],
                                            compare_op=ALU.is_equal, fill=0.0, base=0, channel_multiplier=1),
          [C.identf], [C.identf])
    cx.ew("dve", lambda e: e.tensor_copy(out=C.identb[:], in_=C.identf[:]), [C.identf], [C.identb])
    C.eps = cx.sb(st, "epsc", [128, 1])
    cx.ew("pool", lambda e: e.memset(C.eps[:], EPS), [], [C.eps])
    C.modF = cx.sb(st, "modF", [128, 48])
    C.gtrow = cx.sb(st, "gtrow", [128, 4096])
    C.sc1F = cx.sb(st, "sc1F", [128, 8])
    C.sc2F = cx.sb(st, "sc2F", [128, 8])


def phase_adaln(cx, C, din):
    with ExitStack() as st:
        cT = cx.sb(st, "cT", [128, 8])
        sc = cx.sb(st, "sc", [128, 8])
        screp = cx.sb(st, "screp", [128, 8, 128])
        wst = [cx.sb(st, "wst%d" % i, [128, 8, 512]) for i in range(2)]
        modp = cx.ps(st, "modp", [128, 48])
        rowp = [cx.ps(st, "rowp%d" % i, [128, 512]) for i in range(2)]
        badaF = cx.sb(st, "badaF", [128, 48])
        bgt = cx.sb(st, "bgt", [128, 4096])
        n2row = cx.sb(st, "n2row", [128, 1024])
        n1 = cx.sb(st, "n1", [128, 8])
        n2 = cx.sb(st, "n2", [128, 8])
        tmp = cx.sb(st, "tmpa", [128, 8])
        cx.dma("sp", cT[:], din["cT"].ap(), [], [cT])
        cx.dma("sp", badaF[:], din["b_adaF"].ap(), [], [badaF])
        cx.dma("sp", bgt[:], din["b_gt"].ap().to_broadcast([128, 4096]), [], [bgt])
        cx.dma("sp", n2row[:], din["norm2R"].ap().to_broadcast([128, 1024]), [], [n2row])
        cx.dma("sp", n1[:], din["norm1F"].ap(), [], [n1])
        cx.dma("sp", n2[:], din["norm2F"].ap(), [], [n2])
        cx.actv(sc[:], cT[:], AF.Silu, [cT], [sc])
        for k in range(8):
            cx.ew("dve", lambda e, k=k: e.tensor_copy(out=screp[:, k, :], in_=sc[:, k:k + 1].to_broadcast([128, 128])),
                  [sc], [screp])
        wsrc = din["w_ada"].ap().rearrange("(k p) f -> p k f", p=128)
        for ch in range(12):
            ws = wst[ch % 2]
            cx.dma("sp", ws[:], wsrc[:, :, ch * 512:(ch + 1) * 512], [], [ws])
            for j in range(4):
                ft = ch * 4 + j
                for k in range(8):
                    cx.mm(modp[:, ft:ft + 1], ws[:, k, j * 128:(j + 1) * 128], sc[:, k:k + 1], k == 0, k == 7,
                          [ws, sc], [modp])
            if ch in (4, 5, 10, 11, 6, 7, 8, 9):
                rp = rowp[ch % 2]
                gi = {4: 0, 5: 1, 10: 2, 11: 3, 6: 4, 7: 5, 8: 6, 9: 7}[ch]
                for k in range(8):
                    cx.mm(rp[:], screp[:, k, :], ws[:, k, :], k == 0, k == 7, [ws, screp], [rp])
                cx.ew("dve", lambda e, rp=rp, gi=gi: e.tensor_tensor(out=C.gtrow[:, gi * 512:(gi + 1) * 512], in0=rp[:],
                                                                      in1=bgt[:, gi * 512:(gi + 1) * 512], op=ALU.add),
                      [rp, bgt], [C.gtrow])
        cx.ew("dve", lambda e: e.tensor_tensor(out=C.modF[:], in0=modp[:], in1=badaF[:], op=ALU.add),
              [modp, badaF], [C.modF])
        cx.ew("dve", lambda e: e.tensor_scalar_add(out=C.gtrow[:, 3072:4096], in0=C.gtrow[:, 3072:4096], scalar1=1.0), [C.gtrow], [C.gtrow])
        cx.ew("dve", lambda e: e.tensor_tensor(out=C.gtrow[:, 3072:4096], in0=C.gtrow[:, 3072:4096], in1=n2row[:], op=ALU.mult),
              [C.gtrow, n2row], [C.gtrow])
        for (dst, nw, lo) in ((C.sc1F, n1, 8), (C.sc2F, n2, 32)):
            cx.ew("dve", lambda e, lo=lo: e.tensor_scalar_add(out=tmp[:], in0=C.modF[:, lo:lo + 8], scalar1=1.0),
                  [C.modF], [tmp])
            cx.ew("dve", lambda e, dst=dst, nw=nw: e.tensor_tensor(out=dst[:], in0=tmp[:], in1=nw[:], op=ALU.mult),
                  [tmp, nw], [dst])
        cx.P.flush()


def rms_rows(cx, xt, ssq, rs, junk, nj):
    for j in range(nj):
        cx.actv(junk[:, j, :], xt[:, j, :], AF.Square, [xt], [junk, ssq], accum=ssq[:, j:j + 1])
    cx.actv(rs[:, 0:nj], ssq[:, 0:nj], AF.Sqrt, [ssq], [rs], bias=cx.C.eps[:], scale=1.0 / D)
    cx.ew("dve", lambda e: e.reciprocal(out=rs[:, 0:nj], in_=rs[:, 0:nj]), [rs], [rs])


def phase_proj(cx, C, din, hf):
    sc = cx.dram
    with ExitStack() as st:
        xt = [cx.sb(st, "xt%d" % i, [128, 4, 1024]) for i in range(2)]
        stage = [TL(None, xt[i].k, view=xt[i][:].rearrange("p j (a b) -> p (j a) b", b=512)) for i in range(2)]
        w_fmA = cx.sb(st, "w_fmA", [128, 8, 1024], BF16)
        w_fmQ = cx.sb(st, "w_fmQ", [128, 8, 512], BF16)
        w_tm = cx.sb(st, "w_tm", [128, 8, 416], BF16)
        load_w_bf16(cx, st, din["w_fmA"], 1024, w_fmA, stage, "wa")
        load_w_bf16(cx, st, din["w_fmQ"], 512, w_fmQ, stage, "wq")
        load_w_bf16(cx, st, din["w_tm"], 416, w_tm, stage, "wt")
        rm = cx.sb(st, "rm", [128, 128])
        bones = cx.sb(st, "bones", [128, 128])
        qkw = cx.sb(st, "qkw", [128, 2])
        cx.dma("sp", rm[:], din["Rm"].ap(), [], [rm])
        cx.dma("sp", bones[:], din["bones"].ap(), [], [bones])
        cx.dma("sp", qkw[:], din["qkw"].ap(), [], [qkw])
        xb = [cx.sb(st, "xb%d" % i, [128, 4, 1024], BF16) for i in range(2)]
        hT = [cx.sb(st, "hT%d" % i, [128, 8, 512], BF16) for i in range(2)]
        ssq = [cx.sb(st, "ssq%d" % i, [128, 4]) for i in range(2)]
        rs = [cx.sb(st, "rs%d" % i, [128, 4]) for i in range(2)]
        cs = [cx.sb(st, "cos%d" % i, [128, 512]) for i in range(2)]
        sn = [cx.sb(st, "sin%d" % i, [128, 512]) for i in range(2)]
        dbuf = [cx.sb(st, "dbuf%d" % i, [128, 6, 512]) for i in range(2)]
        qo = [cx.sb(st, "qo%d" % i, [128, 6, 512], BF16) for i in range(2)]
        va = [cx.sb(st, "va%d" % i, [128, 4, 132], BF16) for i in range(2)]
        ba = [cx.sb(st, "ba%d" % i, [128, 4, 32]) for i in range(2)]
        dz = [cx.sb(st, "dz%d" % i, [128, 4, 256]) for i in range(2)]
        qx = [cx.sb(st, "qx%d" % i, [128, 512]) for i in range(3)]
        qsq = [cx.sb(st, "qsq%d" % i, [128, 512]) for i in range(3)]
        qrs = [cx.sb(st, "qrs%d" % i, [128, 512]) for i in range(3)]
        xn = [cx.sb(st, "xn%d" % i, [128, 512]) for i in range(3)]
        t1 = [cx.sb(st, "t1%d" % i, [128, 512]) for i in range(3)]
        t2 = [cx.sb(st, "t2%d" % i, [128, 512]) for i in range(3)]
        trp = [cx.ps(st, "trp%d" % i, [128, 512], BF16) for i in range(2)]
        pj = [cx.ps(st, "pj%d" % i, [128, 512]) for i in range(2)]
        ssp = cx.ps(st, "ssp", [128, 512])
        rtp = cx.ps(st, "rtp", [128, 512])
        tmp_ = [cx.ps(st, "tmp%d" % i, [128, 512]) for i in range(2)]
        for i in range(2):
            cx.ew("pool", lambda e, i=i: e.memset(va[i][:], 1.0), [], [va[i]])
        xsrc = din["x"].ap()
        npj = 0
        nqk = 0
        for c in range(NCH):
            b = c % 2
            own = c < 8
            t0 = c * 512
            cx.dma("sp", xt[b][:], xsrc[t0:t0 + 512, :].rearrange("(j p) d -> p j d", p=128), [], [xt[b]])
            cx.dma("sp", cs[b][:], din["cosT"].ap()[:, t0:t0 + 512], [], [cs[b]])
            cx.dma("sp", sn[b][:], din["sinT"].ap()[:, t0:t0 + 512], [], [sn[b]])
            rms_rows(cx, xt[b], ssq[b], rs[b], xb[b], 4)
            for j in range(4):
                cx.ew("dve", lambda e, j=j, b=b: e.tensor_scalar(out=xb[b][:, j, :], in0=xt[b][:, j, :],
                                                                 scalar1=rs[b][:, j:j + 1], scalar2=None, op0=ALU.mult),
                      [xt[b], rs[b]], [xb[b]])
            for k in range(8):
                tp = trp[k % 2]
                for j in range(4):
                    cx.tr(tp[:, j * 128:(j + 1) * 128], xb[b][:, j, k * 128:(k + 1) * 128], C.identb[:],
                          [xb[b], C.identb], [tp])
                cx.actv(hT[b][:, k, :], tp[:], AF.Identity, [tp, C.sc1F, C.modF], [hT[b]],
                        scale=C.sc1F[:, k:k + 1], bias=C.modF[:, k:k + 1])
            cx.dma("sp", sc["HT"].ap()[:, :, t0:t0 + 512], hT[b][:], [hT[b]], [("HT", c)])
            tiles = [("A", i) for i in range(8)] + ([("Q", i) for i in range(4)] if own else [])
            if "fm" in SKIP:
                tiles = []

            def stageB(s, wc):
                cx.mm(ssp[:], bones[:], qsq[s][:], True, True, [bones, qsq[s]], [ssp])
                cx.actv(qrs[s][:], ssp[:], AF.Sqrt, [ssp], [qrs[s]], bias=C.eps[:], scale=1.0 / 64)
                cx.ew("dve", lambda e, s=s: e.reciprocal(out=qrs[s][:], in_=qrs[s][:]), [qrs[s]], [qrs[s]])
                cx.ew("dve", lambda e, s=s, wc=wc: e.scalar_tensor_tensor(out=xn[s][:], in0=qx[s][:], scalar=wc,
                                                                          in1=qrs[s][:], op0=ALU.mult, op1=ALU.mult),
                      [qx[s], qrs[s], qkw], [xn[s]])

            def stageC(s, slot, b):
                cx.mm(rtp[:], rm[:], xn[s][:], True, True, [rm, xn[s]], [rtp])
                cx.ew("pool", lambda e, s=s, b=b: e.tensor_tensor(out=t1[s][:], in0=xn[s][:], in1=cs[b][:], op=ALU.mult),
                      [xn[s], cs[b]], [t1[s]])
                cx.ew("dve", lambda e, s=s, b=b: e.tensor_tensor(out=t2[s][:], in0=rtp[:], in1=sn[b][:], op=ALU.mult),
                      [rtp, sn[b]], [t2[s]])
                cx.ew("dve", lambda e, s=s, b=b, slot=slot: e.tensor_tensor(out=qo[b][:, slot, :], in0=t1[s][:], in1=t2[s][:],
                                                                             op=ALU.add),
                      [t1[s], t2[s]], [qo[b]])

            qB = []
            qC = []

            def advance():
                if qC:
                    qC.pop(0)()
                if qB:
                    qB.pop(0)()

            def mkB(s, wc, slot, b):
                def f_():
                    stageB(s, wc)
                    qC.append(lambda: stageC(s, slot, b))
                return f_

            for (kind, i) in tiles:
                p = pj[npj % 2]
                npj += 1
                wsrc_ = w_fmA if kind == "A" else w_fmQ
                for k in range(8):
                    cx.mm(p[:], wsrc_[:, k, i * 128:(i + 1) * 128], hT[b][:, k, :], k == 0, k == 7,
                          [wsrc_, hT[b]], [p])
                if kind == "A" and i >= 2:
                    cx.ew("act", lambda e, p=p, i=i, b=b: e.copy(out=dbuf[b][:, i - 2, :], in_=p[:]), [p], [dbuf[b]])
                    advance()
                    continue
                if "qk" in SKIP:
                    continue
                s = nqk % 3
                nqk += 1
                wc = qkw[:, 0:1] if kind == "Q" else qkw[:, 1:2]
                slot = i if kind == "A" else 2 + i
                cx.ew("act", lambda e, p=p, s=s: e.copy(out=qx[s][:], in_=p[:]), [p], [qx[s]])
                cx.actv(qsq[s][:], p[:], AF.Square, [p], [qsq[s]])
                advance()
                qB.append(mkB(s, wc, slot, b))
            cx.dma("sp", sc["DPRE"].ap()[:, :, t0:t0 + 512].rearrange("j p t -> p j t"), dbuf[b][:], [dbuf[b]], [("DPRE", c)])
            for j in range(4 if "tm" not in SKIP else 0):
                tp = tmp_[j % 2]
                for k in range(8):
                    cx.mm(tp[:, 0:416], hT[b][:, k, j * 128:(j + 1) * 128], w_tm[:, k, :], k == 0, k == 7,
                          [hT[b], w_tm], [tp])
                for kv in range(2 if "tmva" not in SKIP else 0):
                    cx.ew("dve", lambda e, tp=tp, j=j, kv=kv, b=b: e.tensor_copy(out=va[b][:, j, kv * 66:kv * 66 + 64],
                                                                                in_=tp[:, kv * 64:(kv + 1) * 64]),
                          [tp], [va[b]])
                if "tmba" not in SKIP:
                    cx.ew("dve", lambda e, tp=tp, j=j, b=b: e.tensor_copy(out=ba[b][:, j, :], in_=tp[:, 128:160]), [tp], [ba[b]])
                if "tmdz" not in SKIP:
                    cx.actv(dz[b][:, j, :], tp[:, 160:416], AF.Silu, [tp], [dz[b]])
                advance()
            while qB or qC:
                advance()
            cx.dma("sp", sc["KT"].ap()[:, :, t0:t0 + 512].rearrange("j p t -> p j t"), qo[b][:, 0:2, :], [qo[b]], [("KT", c)])
            if own:
                l0 = t0
                cx.dma("sp", sc["QT"].ap()[:, :, l0:l0 + 512].rearrange("j p t -> p j t"), qo[b][:, 2:6, :], [qo[b]],
                       [("QT", c)])
            cx.dma("sp", sc["VA"].ap()[t0:t0 + 512, :].rearrange("(j p) f -> p j f", p=128), va[b][:], [va[b]], [("VA", c)])
            cx.dma("sp", sc["BA"].ap()[t0:t0 + 512, :].rearrange("(j p) f -> p j f", p=128), ba[b][:], [ba[b]], [("BA", c)])
            cx.dma("sp", sc["DZS"].ap()[t0:t0 + 512, :].rearrange("(j p) f -> p j f", p=128), dz[b][:], [dz[b]], [("DZS", c)])
        cx.P.flush()


OFF = dict(aq=0, ak=512, av=640, dq=768, dk=1280, dv=1792, dz=2304, b=2816, a=2832, g=2848)


def _fm(v, ncol):
    return np.ascontiguousarray(np.asarray(v, np.float32).reshape(ncol, 128).T)


def const_tables():
    t = {}
    freqs = (np.float32(10000.0) ** (-(np.arange(16, dtype=np.float32) * np.float32(2.0) / np.float32(32)))).astype(np.float32)
    tok = np.arange(S)
    pos = np.stack([tok // 64, tok % 64], 0).astype(np.float32)
    cosT = np.zeros((128, S), np.float32)
    sinT = np.zeros((128, S), np.float32)
    rm = np.zeros((128, 128), np.float32)
    for p in range(128):
        d = p % 64
        a = d // 32
        i = d % 32
        ang = (pos[a] * freqs[i % 16]).astype(np.float32)
        cosT[p] = np.cos(ang)
        sinT[p] = np.sin(ang)
        if i < 16:
            rm[p + 16, p] = -1.0
        else:
            rm[p - 16, p] = 1.0
    t["cosT"], t["sinT"], t["Rm"] = cosT, sinT, rm
    bones = np.zeros((128, 128), np.float32)
    bones[:64, :64] = 1.0
    bones[64:, 64:] = 1.0
    t["bones"] = bones
    t["dnmask"] = dn_masks()
    return t


def core_inputs(inp, core, tabs):
    b, hf = core // 2, core % 2
    rev = hf == 1
    w_in = inp["w_in"][0]
    d = {}
    for k in ("Rm", "bones", "dnmask"):
        d[k] = tabs[k]
    d["cosT"] = np.ascontiguousarray(tabs["cosT"][:, ::-1]) if rev else tabs["cosT"]
    d["sinT"] = np.ascontiguousarray(tabs["sinT"][:, ::-1]) if rev else tabs["sinT"]
    xb = inp["x"][b]
    d["x"] = np.ascontiguousarray(xb[::-1] if rev else xb)
    d["cT"] = _fm(inp["c"][b], 8)
    d["w_ada"] = np.ascontiguousarray(inp["w_ada"][0])
    d["b_adaF"] = _fm(inp["b_ada"][0], 48)
    ba_ = inp["b_ada"][0]
    d["b_gt"] = np.ascontiguousarray(np.concatenate([ba_[2048:3072], ba_[5120:6144], ba_[3072:4096], ba_[4096:5120]])[None, :])
    d["norm2R"] = np.ascontiguousarray(inp["norm2_w"][0][None, :].astype(np.float32))
    d["norm1F"] = _fm(inp["norm1_w"][0], 8)
    d["norm2F"] = _fm(inp["norm2_w"][0], 8)
    ak = w_in[:, OFF["ak"]:OFF["ak"] + 128]
    h0 = hf * 256
    d["w_fmA"] = np.ascontiguousarray(np.concatenate(
        [ak[:, 0:64], ak[:, 0:64], ak[:, 64:128], ak[:, 64:128],
         w_in[:, OFF["dq"] + h0:OFF["dq"] + h0 + 256], w_in[:, OFF["dk"] + h0:OFF["dk"] + h0 + 256],
         w_in[:, OFF["dv"] + h0:OFF["dv"] + h0 + 256]], axis=1))
    d["w_fmQ"] = np.ascontiguousarray(w_in[:, 0:512])
    dirs = (1, 0) if rev else (0, 1)
    hs = slice(hf * 4, hf * 4 + 4)
    bcols = [w_in[:, OFF["b"] + dd * 8 + hf * 4:OFF["b"] + dd * 8 + hf * 4 + 4] for dd in dirs]
    acols = [w_in[:, OFF["a"] + dd * 8 + hf * 4:OFF["a"] + dd * 8 + hf * 4 + 4] for dd in dirs]
    pad = w_in[:, OFF["b"]:OFF["b"] + 16]
    d["w_tm"] = np.ascontiguousarray(np.concatenate(
        [w_in[:, OFF["av"]:OFF["av"] + 128]] + bcols + acols + [pad, w_in[:, OFF["dz"] + h0:OFF["dz"] + h0 + 256]], axis=1))
    cwl = []
    cw = inp["conv_w"][0][::-1] if rev else inp["conv_w"][0]
    for base in (0, 512, 1024):
        for jp in range(2):
            c0 = base + h0 + jp * 128
            cwl.append(cw[:, c0:c0 + 128].T)
    d["conv_wF"] = np.ascontiguousarray(np.stack(cwl, 1).astype(np.float32))
    al = np.stack([inp["a_log"][0][dd, hs] for dd in dirs], 0)
    db = np.stack([inp["dt_bias"][0][dd, hs] for dd in dirs], 0)
    d["agrow"] = np.ascontiguousarray(np.stack([al, db], 0)[None].astype(np.float32))
    d["w_gates"] = np.ascontiguousarray(w_in[:, OFF["g"]:OFF["g"] + 2048])
    d["w_attn_up"] = np.ascontiguousarray(inp["w_attn_up"][0])
    wdu = inp["w_dn_up"][0]
    d["w_dn_up"] = np.ascontiguousarray(np.concatenate([wdu[h0:h0 + 256], wdu[256 - h0:512 - h0]], 0))
    d["w_o"] = np.ascontiguousarray(inp["w_o"][0])
    d["w_router"] = np.ascontiguousarray(inp["w_router"][0])
    d["eoff"] = (np.arange(16, dtype=np.float32) * 1024.0)[None, :].astype(np.float32)
    d["w_gate"] = np.ascontiguousarray(inp["w_gate"][0])
    d["w_up"] = np.ascontiguousarray(inp["w_up"][0])
    d["w_down"] = np.ascontiguousarray(inp["w_down"][0])
    d["sel"] = np.array([[1.0, 0.0]] if rev else [[0.0, 1.0]], np.float32)
    d["dnw_row"] = np.ascontiguousarray(inp["dn_norm_w"][0][None, :].astype(np.float32))
    d["qkrow"] = np.ascontiguousarray(np.stack([inp["q_norm_w"][0], inp["k_norm_w"][0]], 0)[None].astype(np.float32))
    d["qkw"] = np.ascontiguousarray(np.stack([np.tile(inp["q_norm_w"][0], 2), np.tile(inp["k_norm_w"][0], 2)], 1).astype(np.float32))
    return d


INPUT_SHAPES = dict(
    x=[S, D], cT=[128, 8], w_ada=[D, 6144], b_adaF=[128, 48], b_gt=[1, 4096], norm2R=[1, 1024], norm1F=[128, 8], norm2F=[128, 8],
    w_fmA=[D, 1024], w_fmQ=[D, 512], w_tm=[D, 416], qkw=[128, 2],
    cosT=[128, S], sinT=[128, S], Rm=[128, 128], bones=[128, 128], qkrow=[1, 2, 64], conv_wF=[128, 6, 5], dnmask=[128, 9, 128], agrow=[1, 2, 2, 4], dnw_row=[1, 64], w_gates=[D, 2048], w_attn_up=[512, D], w_dn_up=[512, D], w_o=[D, D], w_router=[D, 16], sel=[1, 2], eoff=[1, 16], w_gate=[16, D, D], w_up=[16, D, D], w_down=[16, D, D],
)


def build(hf_sym=None, debug=False, upto=99, hf=0):
    nc = bass.Bass("TRN2", target_bir_lowering=False)
    with ExitStack() as st:
        P = Prog(nc, st)
        cx = Ctx(nc, P, st, debug)
        C = Consts()
        cx.C = C
        din = {k: cx.inp(k, shp) for k, shp in INPUT_SHAPES.items()}
        cx.scratch("HT", [128, 8, S], BF16, dbg=True)
        cx.scratch("QT", [4, 128, 4096], BF16, dbg=True)
        cx.scratch("KT", [2, 128, S], BF16, dbg=True)
        cx.scratch("VA", [S, 132], BF16, dbg=True)
        cx.scratch("DPRE", [6, 128, S], F32, dbg=True)
        cx.scratch("BA", [S, 32], F32, dbg=True)
        cx.scratch("DZS", [S, 256], F32, dbg=True)
        if debug:
            dbg_mod = nc.dram_tensor("dbg_mod", [128, 48 + 2048 + 16], F32, kind="ExternalOutput")
        setup_consts(cx, C, din)
        for e_ in range(16):
            cx.scratch("XE%d" % e_, [1024 + 128, 529], F32, dbg=False)
        if upto >= 8:
            moe_prefill(cx, C)
        phase_adaln(cx, C, din)
        if debug:
            cx.dma("sp", dbg_mod.ap()[:, 0:48], C.modF[:], [C.modF], ["dbg1"])
            cx.dma("sp", dbg_mod.ap()[:, 48:48 + 2048], C.gtrow[:, 0:2048], [C.gtrow], ["dbg2"])
            cx.dma("sp", dbg_mod.ap()[:, 2096:2104], C.sc1F[:], [C.sc1F], ["dbg3"])
            cx.dma("sp", dbg_mod.ap()[:, 2104:2112], C.sc2F[:], [C.sc2F], ["dbg4"])
        if upto >= 2 and "noproj" not in SKIP:
            phase_proj(cx, C, din, hf)
        cx.scratch("ATT", [512, 4096], BF16, dbg=True)
        merged_cd = upto >= 4 and "noattn" not in SKIP and "nodnpre" not in SKIP and "mergecd" in SKIP
        cx.scratch("DQT", [2, 128, S], BF16, dbg=True)
        cx.scratch("DKT", [2, 128, S], BF16, dbg=True)
        cx.scratch("DKV", [S, 512], BF16, dbg=True)
        if merged_cd:
            phase_attn(cx, C, din, hf, with_dnpre=True)
        else:
            if upto >= 3 and "noattn" not in SKIP:
                phase_attn(cx, C, din, hf)
            if upto >= 4 and "nodnpre" not in SKIP:
                phase_dn_pre(cx, C, din, hf)
        cx.scratch("OD", [2, S, 256], F32, dbg=True)
        wstack = ExitStack()
        if upto >= 7 and "nomerge" not in SKIP:
            merge_weights(cx, C, din, wstack)
        if upto >= 5 and "nodn" not in SKIP:
            phase_dn(cx, C, din, hf)
        cx.scratch("DNT", [256, 4096], BF16, dbg=True)
        cx.scratch("DNTR", [256, 4096], BF16, dbg=(GROUPS is None))
        cx.scratch("DNGR", [512, 4096], BF16, dbg=(GROUPS is None))
        if upto >= 6 and "nodnout" not in SKIP:
            phase_dn_out(cx, C, din, hf)
        out_x = nc.dram_tensor("out_x", [4096, D], F32, kind="ExternalOutput")
        cx.scratch("ACC", [4096 + 128, D], F32, dbg=True)
        cx.scratch("H2X", [4096, 529], F32, dbg=True)
        cx.scratch("AFF", [4096, 16], F32, dbg=False)
        cx.scratch("AFFG", [S, 16], F32, dbg=True)
        if upto >= 7 and "nomerge" not in SKIP:
            phase_merge(cx, C, din, hf, out_x)
        wstack.close()
        if debug:
            cx.scratch("DBGM", [128, 16], F32, dbg=True)
            cx.scratch("DBGD", [4096, 16], I32, dbg=True)
        if upto >= 8:
            phase_moe(cx, C, din, out_x)
        P.emit()
    return nc


def phase_attn(cx, C, din, hf, with_dnpre=False):
    sc = cx.dram
    NQC = 4096 // 256
    NKT = S // 128
    if "attn_small" in SKIP:
        NKT = 4
    with ExitStack() as st:
        kt2 = [cx.sb(st, "kt2_%d" % i, [128, S], BF16) for i in range(2)]
        qbd2 = [cx.sb(st, "qbd_%d" % i, [128, NQC, 512], BF16) for i in range(2)]
        qbd = [qbd2[i % 2] for i in range(4)]
        va = cx.sb(st, "va_all", [128, 64, 132], BF16)
        side = phase_dn_pre(cx, C, din, hf, host=st) if with_dnpre else []
        for i in range(2):
            cx.dma("sp", kt2[i][:], sc["KT"].ap()[i], [], [kt2[i]])
        for i in range(2):
            cx.ew("pool" if i % 2 else "dve", lambda e, i=i: e.memset(qbd2[i][:], 0.0), [], [qbd2[i]])

        def load_q(i):
            cx.dma("sp", qbd[i][0:64, :, 0:256], sc["QT"].ap()[i, 0:64, :].rearrange("p (c t) -> p c t", t=256), [], [qbd[i]])
            cx.dma("sp", qbd[i][64:128, :, 256:512], sc["QT"].ap()[i, 64:128, :].rearrange("p (c t) -> p c t", t=256), [], [qbd[i]])

        load_q(0)
        load_q(1)
        for g in range(4):
            cx.dma("sp", va[:, g * 16:(g + 1) * 16, :],
                   sc["VA"].ap()[g * 2048:(g + 1) * 2048, :].rearrange("(kt p) f -> p kt f", p=128), [], [va])
        wrow = cx.sb(st, "wrow", [128, 2, 64])
        wmax = cx.sb(st, "wmax", [128, 2])
        negb = cx.sb(st, "negb", [128, 1])
        cx.dma("sp", wrow[:], din["qkrow"].ap().to_broadcast([128, 2, 64]), [], [wrow])
        cx.ew("dve", lambda e: e.reduce_max(out=wmax[:], in_=wrow[:], axis=AX.X, apply_absolute_value=True), [wrow], [wmax])
        cx.ew("dve", lambda e: e.scalar_tensor_tensor(out=negb[:], in0=wmax[:, 0:1], scalar=-8.0, in1=wmax[:, 1:2],
                                                      op0=ALU.mult, op1=ALU.mult), [wmax], [negb])
        ones = cx.sb(st, "ones_att", [128, 64])
        cx.ew("pool", lambda e: e.memset(ones[:], 1.0), [], [ones])
        sT = [cx.ps(st, "sT%d" % i, [128, 512]) for i in range(ATT_NS)]
        oT = [cx.ps(st, "oT%d" % i, [128, 512]) for i in range(2)]
        bc = cx.ps(st, "bc", [128, 512])
        pT = [cx.sb(st, "pT%d" % i, [128, 512], BF16) for i in range(ATT_NP)]
        rinv = [cx.sb(st, "rinv%d" % i, [128, 512]) for i in range(2)]
        osb = [cx.sb(st, "osb%d" % i, [64, 512]) for i in range(2)]
        ao = [cx.sb(st, "ao%d" % i, [64, 512], BF16) for i in range(2)]
        it = 0
        ns = 0
        NJ = 4 if "attn_1h" not in SKIP else 1
        ngrp = NJ * NQC
        nside = 0
        for j in range(NJ):
            kv = j // 2
            if j >= 2:
                load_q(j)
            for qc in range(NQC):
                while side and nside < len(side) and nside * ngrp <= it * len(side):
                    side[nside]()
                    nside += 1
                o = oT[it % 2]
                u = it % 2

                def S_mm(kt):
                    s_ = sT[(ns + kt) % ATT_NS]
                    cx.mm(s_[:], kt2[kv][:, kt * 128:(kt + 1) * 128], qbd[j][:, qc, :], True, True, [kt2[kv], qbd[j]], [s_])
                    p_ = pT[(ns + kt) % ATT_NP]
                    cx.actv(p_[:], s_[:], AF.Exp, [s_, negb], [p_], bias=negb[:], scale=0.125)

                def PV_mm(kt):
                    p_ = pT[(ns + kt) % ATT_NP]
                    cx.mm(o[0:65, :], va[:, kt, kv * 66:kv * 66 + 65], p_[:], kt == 0, kt == NKT - 1, [va, p_], [o])

                LAG = ATT_LAG
                for kt in range(NKT + LAG):
                    if kt < NKT:
                        S_mm(kt)
                    if kt >= LAG:
                        PV_mm(kt - LAG)
                ns += NKT
                cx.ew("dve", lambda e, o=o, u=u: e.reciprocal(out=rinv[u][64:65, :], in_=o[64:65, :]), [o], [rinv[u]])
                cx.ew("act", lambda e, o=o, u=u: e.copy(out=osb[u][:], in_=o[0:64, :]), [o], [osb[u]])
                cx.mm(bc[0:64, :], ones[64:65, 0:64], rinv[u][64:65, :], True, True, [ones, rinv[u]], [bc])
                cx.ew("dve", lambda e, u=u: e.tensor_tensor(out=ao[u][:], in0=osb[u][:], in1=bc[0:64, :], op=ALU.mult),
                      [osb[u], bc], [ao[u]])
                for hh in range(2):
                    h = 2 * j + hh
                    cx.dma("sp", sc["ATT"].ap()[h * 64:(h + 1) * 64, qc * 256:(qc + 1) * 256], ao[u][:, hh * 256:(hh + 1) * 256],
                           [ao[u]], [("ATT", h, qc)])
                it += 1
        while side and nside < len(side):
            side[nside]()
            nside += 1
        cx.P.flush()


def phase_dn_pre(cx, C, din, hf, host=None):
    sc = cx.dram
    H = S // 2
    NB = 3 if host is None else 1
    with ExitStack() as st_own:
        st = st_own if host is None else host
        pre = [cx.sb(st, "pre%d" % i, [128, H + 4]) for i in range(3 if host is None else 2)]
        acc = [cx.sb(st, "acc%d" % i, [128, H]) for i in range(4 if host is None else 2)]
        cw = cx.sb(st, "cw", [128, 6, 5])
        bones = cx.sb(st, "bones2", [128, 128])
        cx.dma("sp", cw[:], din["conv_wF"].ap(), [], [cw])
        cx.dma("sp", bones[:], din["bones"].ap(), [], [bones])
        sqt = [cx.sb(st, "sqt%d" % i, [128, 512]) for i in range(3)]
        rst = [cx.sb(st, "rst%d" % i, [128, 512]) for i in range(3)]
        tmo = [cx.sb(st, "tmo%d" % i, [128, 512], BF16) for i in range(3)]
        abf = [cx.sb(st, "abf%d" % i, [128, H], BF16) for i in range(2 if host is None else 1)]
        ssp = [cx.ps(st, "ssp2_%d" % i, [128, 512]) for i in range(NB)]
        trp = [cx.ps(st, "trp2_%d" % i, [128, 512]) for i in range(NB)]
        NP_, NA_, NF_ = len(pre), len(acc), len(abf)
        nn = [0]

        def stage1(nt):
            ti, half = nt // 2, nt % 2
            p_, a_ = pre[nt % NP_], acc[nt % NA_]
            if half == 0:
                cx.ew("pool", lambda e, p_=p_: e.memset(p_[:, 0:2], 0.0), [], [p_])
                cx.dma("sp", p_[:, 2:2050], sc["DPRE"].ap()[ti, :, 0:2048], [], [p_])
                cx.dma("sp", p_[:, 2050:H + 4], sc["DPRE"].ap()[ti, :, 2048:H + 2], [], [p_])
            else:
                cx.ew("pool", lambda e, p_=p_: e.memset(p_[:, H + 2:H + 4], 0.0), [], [p_])
                cx.dma("sp", p_[:, 0:2050], sc["DPRE"].ap()[ti, :, H - 2:H + 2048], [], [p_])
                cx.dma("sp", p_[:, 2050:H + 2], sc["DPRE"].ap()[ti, :, H + 2048:S], [], [p_])
            cx.ew("dve", lambda e, p_=p_, a_=a_, ti=ti: e.tensor_scalar(out=a_[:], in0=p_[:, 0:H], scalar1=cw[:, ti, 0:1], scalar2=None,
                                                                      op0=ALU.mult), [p_, cw], [a_])
            for k in range(1, 5):
                cx.ew("dve", lambda e, p_=p_, a_=a_, ti=ti, k=k: e.scalar_tensor_tensor(out=a_[:], in0=p_[:, k:k + H], scalar=cw[:, ti, k:k + 1],
                                                                                       in1=a_[:], op0=ALU.mult, op1=ALU.add),
                      [p_, cw, a_], [a_])
            cx.actv(a_[:], a_[:], AF.Silu, [a_], [a_])

        def stage2(nt):
            ti, half = nt // 2, nt % 2
            kind, jp = ti // 2, ti % 2
            a_ = acc[nt % NA_]
            ab = abf[nt % NF_]
            c0 = half * H
            if kind < 2:
                for c in range(H // 512):
                    s = nn[0] % 3
                    nn[0] += 1
                    sl = slice(c * 512, (c + 1) * 512)
                    cx.ew("pool", lambda e, s=s, a_=a_, sl=sl: e.tensor_tensor(out=sqt[s][:], in0=a_[:, sl], in1=a_[:, sl], op=ALU.mult),
                          [a_], [sqt[s]])
                    sp_ = ssp[s % NB]
                    cx.mm(sp_[:], bones[:], sqt[s][:], True, True, [bones, sqt[s]], [sp_])
                    cx.actv(rst[s][:], sp_[:], AF.Sqrt, [sp_, C.eps], [rst[s]], bias=C.eps[:], scale=1.0)
                    cx.ew("dve", lambda e, s=s: e.reciprocal(out=rst[s][:], in_=rst[s][:]), [rst[s]], [rst[s]])
                    if kind == 0:
                        cx.ew("pool", lambda e, s=s: e.tensor_scalar(out=rst[s][:], in0=rst[s][:], scalar1=0.125, scalar2=None, op0=ALU.mult),
                              [rst[s]], [rst[s]])
                    cx.ew("pool", lambda e, s=s, a_=a_, sl=sl: e.tensor_tensor(out=a_[:, sl], in0=a_[:, sl], in1=rst[s][:], op=ALU.mult),
                          [a_, rst[s]], [a_])
                    cx.ew("act", lambda e, a_=a_, sl=sl, ab=ab: e.copy(out=ab[:, sl], in_=a_[:, sl]), [a_], [ab])
                dst = sc["DQT"] if kind == 0 else sc["DKT"]
                for g in range(2):
                    cx.dma("sp", dst.ap()[jp, :, c0 + g * 2048:c0 + (g + 1) * 2048], ab[:, g * 2048:(g + 1) * 2048], [ab],
                           [(dst.name, jp, half, g)])
            if kind >= 1:
                col0 = (kind - 1) * 256 + jp * 128
                for c in range(H // 512):
                    s = nn[0] % 3
                    nn[0] += 1
                    tp_ = trp[s % NB]
                    for j in range(4):
                        t0 = c * 512 + j * 128
                        cx.tr(tp_[:, j * 128:(j + 1) * 128], a_[:, t0:t0 + 128], C.identf[:], [a_, C.identf], [tp_])
                    cx.ew("act", lambda e, s=s, tp_=tp_: e.copy(out=tmo[s][:], in_=tp_[:]), [tp_], [tmo[s]])
                    cx.dma("sp", sc["DKV"].ap()[c0 + c * 512:c0 + (c + 1) * 512, col0:col0 + 128].rearrange("(j p) f -> p j f", p=128),
                           tmo[s][:].rearrange("p (j f) -> p j f", f=128), [tmo[s]], [("DKV", ti, half, c)])

        items = [lambda: stage1(0)]
        for nt in range(12):
            if nt + 1 < 12:
                items.append(lambda nt=nt: stage1(nt + 1))
            items.append(lambda nt=nt: stage2(nt))
        if host is not None:
            return items
        for it_ in items:
            it_()
        cx.P.flush()


def dn_masks():
    p = np.arange(128)[:, None]
    f = np.arange(128)[None, :]
    same = (p // 64) == (f // 64)
    m = np.zeros((128, 9, 128), np.float32)
    m[:, 0] = same & (p <= f)
    m[:, 1] = same & (p >= f)
    m[:, 2] = same
    m[:, 3] = same & (f >= p)
    m[:, 4] = same & (f <= p)
    m[:, 5] = -1.0 * (same & (f < p))
    m[:, 6] = -1.0 * (same & (f > p))
    m[:, 7] = (p // 32) == (f // 32)
    m[:64, 8, 0] = 1.0
    m[64:, 8, 1] = 1.0
    return m


def phase_dn(cx, C, din, hf):
    sc = cx.dram
    NP = S // 128
    if "dn_small" in SKIP:
        NP = 2
    with ExitStack() as st:
        mk = cx.sb(st, "dnmask", [128, 9, 128])
        cx.dma("sp", mk[:], din["dnmask"].ap(), [], [mk])
        ones = cx.sb(st, "ones_dn", [128, 128])
        cx.ew("pool", lambda e: e.memset(ones[:], 1.0), [], [ones])
        bg = cx.sb(st, "bg", [128, 64, 2, 2, 4])
        with ExitStack() as st2:
            ba = cx.sb(st2, "ba_all", [128, 64, 32])
            agr = cx.sb(st2, "agr", [128, 2, 2, 4])
            nea = cx.sb(st2, "nea", [128, 2, 4])
            tmpg = cx.sb(st2, "tmpg", [128, 64, 4])
            cx.dma("sp", ba[:], sc["BA"].ap().rearrange("(i p) f -> p i f", p=128), [], [ba])
            cx.dma("sp", agr[:], din["agrow"].ap().to_broadcast([128, 2, 2, 4]), [], [agr])
            cx.actv(nea[:], agr[:, 0], AF.Exp, [agr], [nea])
            cx.ew("dve", lambda e: e.tensor_scalar(out=nea[:], in0=nea[:], scalar1=-1.0, scalar2=None, op0=ALU.mult), [nea], [nea])
            for d in range(2):
                c0 = d * 4
                cx.actv(bg[:, :, d, 0, :], ba[:, :, c0:c0 + 4], AF.Sigmoid, [ba], [bg])
                cx.ew("dve", lambda e, d=d, c0=c0: e.tensor_tensor(out=tmpg[:], in0=ba[:, :, 8 + c0:8 + c0 + 4],
                                                                  in1=agr[:, 1, d, :].unsqueeze(1).to_broadcast([128, 64, 4]), op=ALU.add),
                      [ba, agr], [tmpg])
                cx.actv(tmpg[:], tmpg[:], AF.Exp, [tmpg], [tmpg])
                cx.actv(tmpg[:], tmpg[:], AF.Ln, [tmpg], [tmpg], bias=1.0, scale=1.0)
                cx.ew("dve", lambda e, d=d: e.tensor_tensor(out=bg[:, :, d, 1, :], in0=tmpg[:],
                                                           in1=nea[:, d, :].unsqueeze(1).to_broadcast([128, 64, 4]), op=ALU.mult),
                      [tmpg, nea], [bg])
            cx.P.flush()
        banks = [cx.ps(st, "dnb%d" % i, [128, 512]) for i in range(8)]
        nb = [0]

        def bank():
            nb[0] += 1
            return banks[nb[0] % 8]

        chains = {}
        for d in range(2):
            for jp in range(2):
                t = {}
                nm = "c%d%d_" % (d, jp)
                BFN = ("kq", "ktm", "vtm", "aT0", "aT1", "TT2", "vb", "kbg", "kdp0", "kdp1", "qg", "wT", "vnew", "Sbf")
                for name, shape in (("kq", [128, 256]), ("ktm", [128, 2, 64]), ("vtm", [128, 2, 64]), ("rhsg", [128, 2, 128]),
                                    ("gexp", [128, 2, 64]), ("gcs", [128, 6]), ("E", [128, 2, 128]), ("Emin", [128, 2, 128]),
                                    ("Emax", [128, 2, 128]), ("A", [128, 2, 128]), ("B", [128, 2, 128]), ("egr", [128, 128]),
                                    ("F2", [128, 2, 128]), ("Fm", [128, 2, 128]), ("aT0", [128, 128]), ("aT1", [128, 128]),
                                    ("TT2", [128, 2, 128]),
                                    ("sca", [128, 8]), ("vb", [128, 2, 64]), ("kbg", [128, 2, 64]), ("kdp0", [128, 128]),
                                    ("kdp1", [128, 128]), ("qg", [128, 128]), ("u", [128, 128]), ("wT", [128, 128]),
                                    ("vnew", [128, 128]), ("obuf", [128, 128]), ("Sst", [128, 128]), ("Sbf", [128, 128])):
                    t[name] = cx.sb(st, nm + name, shape, BF16 if name in BFN else F32)
                for hh in range(2):
                    for name in ("M", "Md", "Mo", "Nd", "X0", "X1", "Y0", "Y1", "PT", "W1", "Dn"):
                        t[name + str(hh)] = cx.sb(st, nm + name + str(hh), [128, 128])
                for z in ("kdp0", "kdp1", "vnew", "Sst", "Sbf"):
                    cx.ew("pool", lambda e, z=z, t=t: e.memset(t[z][:], 0.0), [], [t[z]])
                chains[(d, jp)] = t

        def inverse(t, hh, pG_):
            sfx = str(hh)
            M, Md, Mo, Nd, PT, W1, Dn = (t[n + sfx] for n in ("M", "Md", "Mo", "Nd", "PT", "W1", "Dn"))
            cx.ew("dve", lambda e: e.tensor_tensor(out=M[:], in0=pG_[:, 0:128], in1=t["Fm"][:, hh, :], op=ALU.mult), [pG_, t["Fm"]], [M])
            aT = t["aT%d" % hh]
            cx.ew("dve", lambda e: e.tensor_tensor(out=aT[:], in0=pG_[:, 128:256], in1=t["F2"][:, hh, :], op=ALU.mult), [pG_, t["F2"]], [aT])
            cx.ew("pool", lambda e: e.tensor_tensor(out=Md[:], in0=M[:], in1=mk[:, 7, :], op=ALU.mult), [M, mk], [Md])
            cx.ew("pool", lambda e: e.tensor_tensor(out=Mo[:], in0=M[:], in1=Md[:], op=ALU.subtract), [M, Md], [Mo])
            yield
            pb = bank()
            cx.tr(pb[:, 0:128], Md[:], C.identf[:], [Md, C.identf], [pb])
            cx.ew("act", lambda e, pb=pb: e.copy(out=Nd[:], in_=pb[:, 0:128]), [pb], [Nd])
            cx.ew("dve", lambda e, pb=pb: e.tensor_tensor(out=PT[:], in0=pb[:, 0:128], in1=C.identf[:], op=ALU.add), [pb, C.identf], [PT])
            yield
            X, Y = Md, Nd
            pend = None
            for j in range(1, 5):
                Xn, Yn = t["X%d" % (j % 2) + sfx], t["Y%d" % (j % 2) + sfx]
                if j < 4:
                    pb = bank()
                    cx.mm(pb[:, 0:128], X[:], Y[:], True, True, [X, Y], [pb])
                    cx.ew("act", lambda e, pb=pb, Yn=Yn: e.copy(out=Yn[:], in_=pb[:, 0:128]), [pb], [Yn])
                pb = bank()
                cx.mm(pb[:, 0:128], Y[:], X[:], True, True, [X, Y], [pb])
                cx.ew("dve", lambda e, pb=pb, Xn=Xn: e.tensor_copy(out=Xn[:], in_=pb[:, 0:128]), [pb], [Xn])
                if pend is not None:
                    pend()
                def upd(Xn=Xn):
                    pb2 = bank()
                    cx.mm(pb2[:, 0:128], Xn[:], PT[:], True, True, [Xn, PT], [pb2])
                    cx.ew("dve", lambda e, pb2=pb2: e.tensor_tensor(out=PT[:], in0=pb2[:, 0:128], in1=PT[:], op=ALU.add), [pb2, PT], [PT])
                pend = upd
                X, Y = Xn, Yn
                yield
            pend()
            yield
            pb = bank()
            cx.mm(pb[:, 0:128], Mo[:], PT[:], True, True, [Mo, PT], [pb])
            cx.ew("act", lambda e, pb=pb: e.copy(out=W1[:], in_=pb[:, 0:128]), [pb], [W1])
            pb = bank()
            cx.tr(pb[:, 0:128], PT[:], C.identf[:], [PT, C.identf], [pb])
            cx.ew("dve", lambda e, pb=pb: e.tensor_copy(out=Dn[:], in_=pb[:, 0:128]), [pb], [Dn])
            yield
            pb = bank()
            cx.mm(pb[:, 0:128], Dn[:], W1[:], True, True, [Dn, W1], [pb])
            cx.ew("dve", lambda e, pb=pb: e.tensor_tensor(out=t["TT2"][:, hh, :], in0=pb[:, 0:128], in1=PT[:], op=ALU.add),
                  [pb, PT], [t["TT2"]])
            yield

        def step(d, jp, i):
            t = chains[(d, jp)]
            t0 = i * 128
            tri = mk[:, 0 + d, :]
            ma = mk[:, 3 + d, :]
            mm_ = mk[:, 5 + d, :]
            g2 = bg[:, i, d, 1, jp * 2:jp * 2 + 2]
            b2 = bg[:, i, d, 0, jp * 2:jp * 2 + 2]
            cx.dma("sp", t["kq"][:, 0:128], sc["DKT"].ap()[jp, :, t0:t0 + 128], [], [t["kq"]])
            cx.dma("sp", t["kq"][:, 128:256], sc["DQT"].ap()[jp, :, t0:t0 + 128], [], [t["kq"]])
            cx.dma("sp", t["ktm"][:], sc["DKV"].ap()[t0:t0 + 128, jp * 128:jp * 128 + 128].rearrange("p (h f) -> p h f", f=64), [], [t["ktm"]])
            cx.dma("sp", t["vtm"][:], sc["DKV"].ap()[t0:t0 + 128, 256 + jp * 128:256 + jp * 128 + 128].rearrange("p (h f) -> p h f", f=64),
                   [], [t["vtm"]])
            cx.ew("dve", lambda e: e.tensor_tensor(out=t["rhsg"][:], in0=tri.unsqueeze(1).to_broadcast([128, 2, 128]),
                                                   in1=g2.unsqueeze(2).to_broadcast([128, 2, 128]), op=ALU.mult), [mk, bg], [t["rhsg"]])
            cx.ew("pool", lambda e: e.tensor_copy(out=t["gexp"][:], in_=g2.unsqueeze(2).to_broadcast([128, 2, 64])), [bg], [t["gexp"]])
            yield
            pA = bank()
            cx.mm(pA[:, 0:256], ones[:], t["rhsg"][:].rearrange("p h c -> p (h c)"), True, True, [ones, t["rhsg"]], [pA])
            cx.mm(pA[:, 256:258], tri, g2, True, True, [mk, bg], [pA])
            cx.mm(pA[:, 258:260], mk[:, 2, :], g2, True, True, [mk, bg], [pA])
            cx.mm(pA[:, 260:262], t["gexp"][:].rearrange("p h c -> p (h c)"), mk[:, 8, 0:2], True, True, [t["gexp"], mk], [pA])
            cx.ew("dve", lambda e: e.tensor_copy(out=t["gcs"][:], in_=pA[:, 256:262]), [pA], [t["gcs"]])
            cx.ew("dve", lambda e: e.tensor_tensor(out=t["E"][:], in0=pA[:, 0:256].rearrange("p (h c) -> p h c", h=2),
                                                   in1=t["gcs"][:, 0:2].unsqueeze(2).to_broadcast([128, 2, 128]), op=ALU.subtract),
                  [pA, t["gcs"]], [t["E"]])
            cx.actv(t["egr"][0:64, :], pA[0:64, 0:128], AF.Exp, [pA], [t["egr"]])
            cx.actv(t["egr"][64:128, :], pA[64:128, 128:256], AF.Exp, [pA], [t["egr"]])
            yield
            cx.ew("dve", lambda e: e.tensor_scalar_min(out=t["Emin"][:], in0=t["E"][:], scalar1=0.0), [t["E"]], [t["Emin"]])
            cx.ew("dve", lambda e: e.tensor_scalar_max(out=t["Emax"][:], in0=t["E"][:], scalar1=0.0), [t["E"]], [t["Emax"]])
            cx.actv(t["A"][:], t["Emin"][:], AF.Exp, [t["Emin"]], [t["A"]])
            cx.actv(t["B"][:], t["Emax"][:], AF.Exp, [t["Emax"]], [t["B"]], scale=-1.0)
            cx.actv(t["sca"][:, 0:2], t["gcs"][:, 0:2], AF.Exp, [t["gcs"]], [t["sca"]])
            cx.ew("dve", lambda e: e.tensor_tensor(out=t["sca"][:, 4:6], in0=t["gcs"][:, 2:4], in1=t["gcs"][:, 0:2], op=ALU.subtract),
                  [t["gcs"]], [t["sca"]])
            cx.actv(t["sca"][:, 4:6], t["sca"][:, 4:6], AF.Exp, [t["sca"]], [t["sca"]])
            cx.actv(t["sca"][:, 6:8], t["gcs"][:, 4:6], AF.Exp, [t["gcs"]], [t["sca"]])
            yield
            cx.ew("pool", lambda e: e.tensor_tensor(out=t["F2"][:], in0=t["A"][:], in1=ma.unsqueeze(1).to_broadcast([128, 2, 128]),
                                                    op=ALU.mult), [t["A"], mk], [t["F2"]])
            for hh in range(2):
                cx.ew("dve", lambda e, hh=hh: e.scalar_tensor_tensor(out=t["Fm"][:, hh, :], in0=t["B"][:, hh, :], scalar=b2[:, hh:hh + 1],
                                                                     in1=mm_, op0=ALU.mult, op1=ALU.mult), [t["B"], bg, mk], [t["Fm"]])
            cx.ew("dve", lambda e: e.tensor_tensor(out=t["sca"][:, 2:4], in0=t["sca"][:, 0:2], in1=b2, op=ALU.mult), [t["sca"], bg], [t["sca"]])
            cx.ew("pool", lambda e: e.tensor_tensor(out=t["vb"][:], in0=t["vtm"][:], in1=b2.unsqueeze(2).to_broadcast([128, 2, 64]),
                                                    op=ALU.mult), [t["vtm"], bg], [t["vb"]])
            cx.ew("pool", lambda e: e.tensor_tensor(out=t["kbg"][:], in0=t["ktm"][:],
                                                    in1=t["sca"][:, 2:4].unsqueeze(2).to_broadcast([128, 2, 64]), op=ALU.mult),
                  [t["ktm"], t["sca"]], [t["kbg"]])
            for hh in range(2):
                kd_ = t["kdp%d" % hh]
                cx.ew("dve", lambda e, hh=hh, kd_=kd_: e.tensor_scalar(out=kd_[:, hh * 64:(hh + 1) * 64], in0=t["ktm"][:, hh, :],
                                                                        scalar1=t["sca"][:, 4 + hh:5 + hh], scalar2=None, op0=ALU.mult),
                      [t["ktm"], t["sca"]], [kd_])
            cx.ew("pool", lambda e: e.tensor_tensor(out=t["qg"][:], in0=t["kq"][:, 128:256], in1=t["egr"][:], op=ALU.mult),
                  [t["kq"], t["egr"]], [t["qg"]])
            yield
            subs = []
            for hh in range(2):
                hs = slice(hh * 64, hh * 64 + 64)
                pG_ = bank()
                cx.mm(pG_[:, 0:256], t["kq"][hs, 0:128], t["kq"][hs, :], True, True, [t["kq"]], [pG_])
                subs.append(inverse(t, hh, pG_))
            live = list(subs)
            while live:
                for g_ in list(live):
                    try:
                        next(g_)
                    except StopIteration:
                        live.remove(g_)
                yield
            pU = bank()
            for hh in range(2):
                cx.mm(pU[:, hh * 64:(hh + 1) * 64], t["TT2"][:, hh, :], t["vb"][:, hh, :], True, True, [t["TT2"], t["vb"]], [pU])
            cx.mm(pU[:, 128:384], t["kbg"][:].rearrange("p h f -> p (h f)"), t["TT2"][:].rearrange("p h c -> p (h c)"), True, True,
                  [t["kbg"], t["TT2"]], [pU])
            cx.ew("act", lambda e: e.copy(out=t["u"][:], in_=pU[:, 0:128]), [pU], [t["u"]])
            cx.ew("dve", lambda e: e.tensor_copy(out=t["wT"][0:64, :], in_=pU[0:64, 128:256]), [pU], [t["wT"]])
            cx.ew("dve", lambda e: e.tensor_copy(out=t["wT"][64:128, :], in_=pU[64:128, 256:384]), [pU], [t["wT"]])
            yield
            for X_ in ((0, 1) if d == 0 else (1, 0)):
                rows = slice(X_ * 64, X_ * 64 + 64)
                pS1 = bank()
                cx.mm(pS1[:, 0:128], t["wT"][:], t["Sbf"][:], True, True, [t["wT"], t["Sbf"]], [pS1])
                cx.ew("dve", lambda e, rows=rows, pS1=pS1: e.tensor_tensor(out=t["vnew"][rows, :], in0=t["u"][rows, :], in1=pS1[rows, 0:128],
                                                                           op=ALU.subtract), [t["u"], pS1], [t["vnew"]])
                yield
                pS2 = bank()
                cx.mm(pS2[:, 0:128], t["qg"][:], t["Sbf"][:], True, False, [t["qg"], t["Sbf"]], [pS2])
                for hh in range(2):
                    cx.mm(pS2[:, hh * 64:(hh + 1) * 64], t["aT%d" % hh][:], t["vnew"][:, hh * 64:(hh + 1) * 64], False, hh == 1,
                          [t["aT%d" % hh], t["vnew"]], [pS2])
                cx.ew("act", lambda e, rows=rows, pS2=pS2: e.copy(out=t["obuf"][rows, :], in_=pS2[rows, 0:128]), [pS2], [t["obuf"]])
                pS3 = bank()
                for hh in range(2):
                    cx.mm(pS3[:, hh * 64:(hh + 1) * 64], t["kdp%d" % hh][rows, :], t["vnew"][rows, hh * 64:(hh + 1) * 64], True, True,
                          [t["kdp%d" % hh], t["vnew"]], [pS3])
                cx.ew("dve", lambda e, X_=X_, pS3=pS3: e.scalar_tensor_tensor(out=t["Sst"][:], in0=t["Sst"][:], scalar=t["sca"][:, 6 + X_:7 + X_],
                                                                              in1=pS3[:, 0:128], op0=ALU.mult, op1=ALU.add),
                      [t["Sst"], t["sca"], pS3], [t["Sst"]])
                cx.ew("act", lambda e: e.copy(out=t["Sbf"][:], in_=t["Sst"][:]), [t["Sst"]], [t["Sbf"]])
                yield
            cx.dma("sp", sc["OD"].ap()[d, t0:t0 + 128, jp * 128:jp * 128 + 128], t["obuf"][:], [t["obuf"]], [("OD", d, jp, i)])

        gens = {}
        nxt = {k: 0 for k in chains}
        live = True
        rnd = 0
        while live:
            live = False
            rnd += 1
            if rnd % 8 == 0 and getattr(C, "mw_gen", None) is not None:
                next(C.mw_gen, None)
            for (d, jp) in chains:
                g_ = gens.get((d, jp))
                if g_ is None:
                    tau = nxt[(d, jp)]
                    if tau >= NP:
                        continue
                    nxt[(d, jp)] = tau + 1
                    g_ = step(d, jp, tau if d == 0 else (S // 128 - 1 - tau))
                    gens[(d, jp)] = g_
                live = True
                try:
                    next(g_)
                except StopIteration:
                    gens[(d, jp)] = None
        cx.P.flush()


GROUPS = PAIRS


def collective_gather(cx, src, dst, r, w):
    if GROUPS is None:
        return
    cx.P.dma("pool", lambda e: e.collective_compute("AllGather", ALU.bypass, replica_groups=GROUPS,
                                                    ins=[src.ap().opt()], outs=[dst.ap().opt()]),
             _keys(r), _keys(w), grp="cc", inc=1)


def phase_dn_out(cx, C, din, hf):
    sc = cx.dram
    with ExitStack() as st:
        dnw = cx.sb(st, "dnw", [128, 64])
        cx.dma("sp", dnw[:], din["dnw_row"].ap().to_broadcast([128, 64]), [], [dnw])
        of = [cx.sb(st, "of%d" % i, [128, 4, 64]) for i in range(4)]
        ob = [cx.sb(st, "ob%d" % i, [128, 4, 64]) for i in range(4)]
        dzt = [cx.sb(st, "dzt%d" % i, [128, 4, 64]) for i in range(4)]
        sq = [cx.sb(st, "dsq%d" % i, [128, 4, 64]) for i in range(4)]
        ss = [cx.sb(st, "dss%d" % i, [128, 4]) for i in range(4)]
        yb = [cx.sb(st, "yb%d" % i, [128, 4, 64], BF16) for i in range(4)]
        dT = [cx.sb(st, "dT%d" % i, [128, 2, 512], BF16) for i in range(2)]
        tp = [cx.ps(st, "dtp%d" % i, [128, 512], BF16) for i in range(2)]
        tpr = [cx.ps(st, "dtpr%d" % i, [128, 512]) for i in range(2)]
        dR = [cx.sb(st, "dR%d" % i, [128, 2, 128], BF16) for i in range(4)]
        jmat = cx.sb(st, "jmat", [128, 128], BF16)
        jf = cx.sb(st, "jf", [128, 128])
        cx.ew("pool", lambda e: e.memset(jf[:], 1.0), [], [jf])
        cx.ew("pool", lambda e: e.affine_select(out=jf[:], in_=jf[:], pattern=[[1, 128]], compare_op=ALU.is_equal, fill=0.0, base=-127,
                                                channel_multiplier=1), [jf], [jf])
        cx.ew("dve", lambda e: e.tensor_copy(out=jmat[:], in_=jf[:]), [jf], [jmat])
        for i in range(S // 128):
            b = i % 4
            t0 = i * 128
            g = i // 4
            gb = g % 2
            cx.dma("sp", of[b][:], sc["OD"].ap()[0, t0:t0 + 128, :].rearrange("p (h f) -> p h f", f=64), [], [of[b]])
            cx.dma("sp", ob[b][:], sc["OD"].ap()[1, t0:t0 + 128, :].rearrange("p (h f) -> p h f", f=64), [], [ob[b]])
            cx.dma("sp", dzt[b][:], sc["DZS"].ap()[t0:t0 + 128, :].rearrange("p (h f) -> p h f", f=64), [], [dzt[b]])
            cx.ew("pool", lambda e, b=b: e.tensor_tensor(out=of[b][:], in0=of[b][:], in1=ob[b][:], op=ALU.add), [of[b], ob[b]], [of[b]])
            cx.ew("pool", lambda e, b=b: e.tensor_tensor(out=sq[b][:], in0=of[b][:], in1=of[b][:], op=ALU.mult), [of[b]], [sq[b]])
            cx.ew("dve", lambda e, b=b: e.reduce_sum(out=ss[b][:], in_=sq[b][:], axis=AX.X), [sq[b]], [ss[b]])
            cx.actv(ss[b][:], ss[b][:], AF.Sqrt, [ss[b], C.eps], [ss[b]], bias=C.eps[:], scale=1.0 / 64)
            cx.ew("dve", lambda e, b=b: e.reciprocal(out=ss[b][:], in_=ss[b][:]), [ss[b]], [ss[b]])
            cx.ew("pool", lambda e, b=b: e.tensor_tensor(out=dzt[b][:], in0=dzt[b][:], in1=dnw[:].unsqueeze(1).to_broadcast([128, 4, 64]),
                                                         op=ALU.mult), [dzt[b], dnw], [dzt[b]])
            cx.ew("dve", lambda e, b=b: e.tensor_tensor(out=of[b][:], in0=of[b][:], in1=ss[b][:].unsqueeze(2).to_broadcast([128, 4, 64]),
                                                        op=ALU.mult), [of[b], ss[b]], [of[b]])
            cx.ew("dve", lambda e, b=b: e.tensor_tensor(out=yb[b][:], in0=of[b][:], in1=dzt[b][:], op=ALU.mult), [of[b], dzt[b]], [yb[b]])
            j = i % 4
            for jp in range(2 if i < 32 else 0):
                cx.tr(tp[jp][:, j * 128:(j + 1) * 128], yb[b][:, jp * 2:jp * 2 + 2, :].rearrange("p h f -> p (h f)"), C.identb[:],
                      [yb[b], C.identb], [tp[jp]])
            if i < 32:
                if j == 3:
                    for jp in range(2):
                        cx.ew("act", lambda e, jp=jp, gb=gb: e.copy(out=dT[gb][:, jp, :], in_=tp[jp][:]), [tp[jp]], [dT[gb]])
                    cx.dma("sp", sc["DNT"].ap()[:, g * 512:(g + 1) * 512].rearrange("(jp p) t -> p jp t", p=128), dT[gb][:], [dT[gb]],
                           ["DNT"])
            else:
                for jp in range(2):
                    cx.mm(tpr[jp][:, 0:128], yb[b][:, jp * 2:jp * 2 + 2, :].rearrange("p h f -> p (h f)"), jmat[:], True, True,
                          [yb[b], jmat], [tpr[jp]])
                    cx.ew("act", lambda e, jp=jp, b=b: e.copy(out=dR[b][:, jp, :], in_=tpr[jp][:, 0:128]), [tpr[jp]], [dR[b]])
                r0 = (63 - i) * 128
                cx.dma("sp", sc["DNTR"].ap()[:, r0:r0 + 128].rearrange("(jp p) t -> p jp t", p=128), dR[b][:], [dR[b]], ["DNTR"])
        if GROUPS is not None:
            collective_gather(cx, sc["DNTR"], sc["DNGR"], ["DNTR"], ["DNGR"])
        cx.P.flush()


def merge_weights(cx, C, din, st):
    mw = {"wg": cx.sb(st, "wg", [128, 8, 2048], BF16), "wau": cx.sb(st, "wau", [128, 4, 1024], BF16),
          "wdu": cx.sb(st, "wdu", [128, 4, 1024], BF16), "wo": cx.sb(st, "wo", [128, 8, 1024], BF16),
          "wr": cx.sb(st, "wr", [128, 8, 16])}
    stg = [cx.sb(st, "mstg%d" % i, [128, 1024]) for i in range(2)]
    C.mw = mw

    def gen():
        cx.dma("sp", mw["wr"][:], din["w_router"].ap().rearrange("(k p) e -> p k e", p=128), [], [mw["wr"]])
        i = 0
        for (name, key, nk, ncols) in (("w_gates", "wg", 8, 2048), ("w_attn_up", "wau", 4, 1024), ("w_dn_up", "wdu", 4, 1024),
                                       ("w_o", "wo", 8, 1024)):
            src = din[name].ap().rearrange("(k p) f -> p k f", p=128)
            for k in range(nk):
                for c0 in range(0, ncols, 1024):
                    sg = stg[i % 2]
                    cx.dma("sp", sg[:], src[:, k, c0:c0 + 1024], [], [sg])
                    if i % 2:
                        cx.ew("act", lambda e, sg=sg, key=key, k=k, c0=c0: e.copy(out=mw[key][:, k, c0:c0 + 1024], in_=sg[:]), [sg], [mw[key]])
                    else:
                        cx.ew("dve", lambda e, sg=sg, key=key, k=k, c0=c0: e.tensor_copy(out=mw[key][:, k, c0:c0 + 1024], in_=sg[:]),
                              [sg], [mw[key]])
                    i += 1
                    yield
    C.mw_gen = gen()


def phase_merge(cx, C, din, hf, out_x):
    sc = cx.dram
    NC_ = 4096 // 512
    if "merge_small" in SKIP:
        NC_ = 1
    with ExitStack() as st:
        xa = [cx.sb(st, "mx%d" % i, [128, 1024]) for i in range(2)]
        wg, wau, wdu, wo, wr = (C.mw[k] for k in ("wg", "wau", "wdu", "wo", "wr"))
        for _ in C.mw_gen:
            pass
        hTc = [cx.sb(st, "mh%d" % i, [128, 8, 512], BF16) for i in range(2)]
        aTc = [cx.sb(st, "ma%d" % i, [128, 4, 512], BF16) for i in range(2)]
        dTc = [cx.sb(st, "md%d" % i, [128, 4, 512], BF16) for i in range(2)]
        dPr = [cx.sb(st, "mdp%d" % i, [128, 4, 512], BF16) for i in range(2)]
        sel = cx.sb(st, "sel", [128, 2])
        cx.dma("sp", sel[:], din["sel"].ap().to_broadcast([128, 2]), [], [sel])
        mT = [cx.sb(st, "mT%d" % i, [128, 8, 512], BF16) for i in range(2)]
        sga = [cx.sb(st, "sga%d" % i, [128, 512]) for i in range(2)]
        sgd = [cx.sb(st, "sgd%d" % i, [128, 512]) for i in range(2)]
        m1 = [cx.sb(st, "m1_%d" % i, [128, 512]) for i in range(2)]
        m2 = [cx.sb(st, "m2_%d" % i, [128, 512]) for i in range(2)]
        x1 = [cx.sb(st, "x1_%d" % i, [128, 1024]) for i in range(2)]
        tmpx = [cx.sb(st, "tmpx%d" % i, [128, 512]) for i in range(2)]
        ssq = [cx.sb(st, "mssq%d" % i, [128, 1]) for i in range(2)]
        rs = [cx.sb(st, "mrs%d" % i, [128, 1]) for i in range(2)]
        h2f = [cx.sb(st, "h2f%d" % i, [128, 1024]) for i in range(2)]
        h2x = [cx.sb(st, "h2x%d" % i, [128, 529]) for i in range(2)]
        h2T = [cx.sb(st, "h2T%d" % i, [128, 8, 128]) for i in range(2)]
        lg = [cx.sb(st, "lg%d" % i, [128, 16]) for i in range(2)]
        sm = [cx.sb(st, "sm%d" % i, [128, 4]) for i in range(2)]
        tid = cx.sb(st, "tid", [128, 32], I32)
        cx.ew("pool", lambda e: e.iota(tid[:], pattern=[[128, 32]], base=0, channel_multiplier=1), [], [tid])
        pg = [cx.ps(st, "pg%d" % i, [128, 512]) for i in range(2)]
        pa = cx.ps(st, "pa", [128, 512])
        pd = cx.ps(st, "pd", [128, 512])
        po = [cx.ps(st, "po%d" % i, [128, 512]) for i in range(2)]
        pt = cx.ps(st, "ptr", [128, 512])
        pl = cx.ps(st, "pl", [128, 512])
        def part2(u, li):
            for q4 in range(2):
                for k4 in range(4):
                    k = q4 * 4 + k4
                    cx.tr(pt[:, k4 * 128:(k4 + 1) * 128], h2f[u][:, k * 128:(k + 1) * 128], C.identf[:], [h2f[u], C.identf], [pt])
                cx.ew("act", lambda e, u=u, q4=q4: e.copy(out=h2T[u][:, q4 * 4:(q4 + 1) * 4, :].rearrange("p k t -> p (k t)"), in_=pt[:]),
                      [pt], [h2T[u]])
            for k in range(8):
                cx.mm(pl[:, 0:16], h2T[u][:, k, :], wr[:, k, :], k == 0, k == 7, [h2T[u], wr], [pl])
            cx.ew("dve", lambda e, u=u: e.reduce_max(out=sm[u][:, 0:1], in_=pl[:, 0:16], axis=AX.X), [pl], [sm[u]])
            cx.ew("dve", lambda e, u=u: e.tensor_scalar(out=sm[u][:, 1:2], in0=sm[u][:, 0:1], scalar1=-1.0, scalar2=None, op0=ALU.mult),
                  [sm[u]], [sm[u]])
            cx.actv(lg[u][:], pl[:, 0:16], AF.Exp, [pl, sm[u]], [lg[u], sm[u]], bias=sm[u][:, 1:2], scale=1.0, accum=sm[u][:, 2:3])
            cx.ew("dve", lambda e, u=u: e.reciprocal(out=sm[u][:, 3:4], in_=sm[u][:, 2:3]), [sm[u]], [sm[u]])
            cx.ew("dve", lambda e, u=u: e.tensor_scalar(out=h2x[u][:, 513:529], in0=lg[u][:], scalar1=sm[u][:, 3:4],
                                                        scalar2=None, op0=ALU.mult), [lg[u], sm[u]], [h2x[u]])
            cx.ew("pool", lambda e, u=u, li=li: e.tensor_copy(out=h2x[u][:, 512:513].bitcast(I32), in_=tid[:, li:li + 1]),
                  [tid], [h2x[u]])
            cx.dma("sp", sc["H2X"].ap()[li * 128:(li + 1) * 128, :], h2x[u][:], [h2x[u]], [("H2X", li)])
            cx.dma("sp", sc["AFF"].ap()[li * 128:(li + 1) * 128, :], h2x[u][:, 513:529], [h2x[u]], ["AFF"])

        xsrc = din["x"].ap()
        npo = 0
        pend2 = None
        for c in range(NC_):
            b = c % 2
            l0 = c * 512
            g0 = l0
            cx.dma("sp", hTc[b][:], sc["HT"].ap()[:, :, g0:g0 + 512], [], [hTc[b]])
            cx.dma("sp", aTc[b][:], sc["ATT"].ap()[:, l0:l0 + 512].rearrange("(k p) t -> p k t", p=128), [], [aTc[b]])
            cx.dma("sp", dTc[b][:, 0:2, :], sc["DNT"].ap()[:, l0:l0 + 512].rearrange("(k p) t -> p k t", p=128), ["DNT"], [dTc[b]])
            cx.dma("sp", dPr[b][:], sc["DNGR"].ap()[:, l0:l0 + 512].rearrange("(k p) t -> p k t", p=128), ["DNGR"], [dPr[b]])
            cx.ew("pool", lambda e, b=b: e.tensor_scalar(out=dPr[b][:, 0:2, :], in0=dPr[b][:, 0:2, :], scalar1=sel[:, 0:1], scalar2=None,
                                                         op0=ALU.mult), [dPr[b], sel], [dPr[b]])
            cx.ew("dve", lambda e, b=b: e.scalar_tensor_tensor(out=dTc[b][:, 2:4, :], in0=dPr[b][:, 2:4, :], scalar=sel[:, 1:2],
                                                                in1=dPr[b][:, 0:2, :], op0=ALU.mult, op1=ALU.add), [dPr[b], sel], [dTc[b]])
            for fo in range(8):
                s = fo % 2
                for (gi, dst) in ((0, sga[s]), (1, sgd[s])):
                    p = pg[gi]
                    for k in range(8):
                        cx.mm(p[:], wg[:, k, gi * 1024 + fo * 128:gi * 1024 + (fo + 1) * 128], hTc[b][:, k, :], k == 0, k == 7,
                              [wg, hTc[b]], [p])
                    cx.actv(dst[:], p[:], AF.Sigmoid, [p], [dst])
                for k in range(4):
                    cx.mm(pa[:], wau[:, k, fo * 128:(fo + 1) * 128], aTc[b][:, k, :], k == 0, k == 3, [wau, aTc[b]], [pa])
                for k in range(4):
                    cx.mm(pd[:], wdu[:, k, fo * 128:(fo + 1) * 128], dTc[b][:, k, :], k == 0, k == 3, [wdu, dTc[b]], [pd])
                cx.ew("dve", lambda e, s=s: e.tensor_tensor(out=m1[s][:], in0=pa[:], in1=sga[s][:], op=ALU.mult), [pa, sga[s]], [m1[s]])
                cx.ew("dve", lambda e, s=s: e.tensor_tensor(out=m2[s][:], in0=pd[:], in1=sgd[s][:], op=ALU.mult), [pd, sgd[s]], [m2[s]])
                cx.ew("pool", lambda e, s=s, b=b, fo=fo: e.tensor_tensor(out=mT[b][:, fo, :], in0=m1[s][:], in1=m2[s][:], op=ALU.add),
                      [m1[s], m2[s]], [mT[b]])
            for j in range(4):
                u = j % 2
                li = c * 4 + j
                cx.dma("sp", xa[u][:], xsrc[g0 + j * 128:g0 + (j + 1) * 128, :], [], [xa[u]])
                for n in range(2):
                    p = po[npo % 2]
                    npo += 1
                    for k in range(8):
                        cx.mm(p[:], mT[b][:, k, j * 128:(j + 1) * 128], wo[:, k, n * 512:(n + 1) * 512], k == 0, k == 7, [mT[b], wo], [p])
                    cx.ew("dve", lambda e, p=p, n=n, u=u: e.tensor_tensor(out=tmpx[n][:], in0=p[:], in1=C.gtrow[:, n * 512:(n + 1) * 512],
                                                                          op=ALU.mult), [p, C.gtrow], [tmpx[n]])
                    cx.ew("pool", lambda e, n=n, u=u: e.tensor_tensor(out=x1[u][:, n * 512:(n + 1) * 512], in0=tmpx[n][:],
                                                                      in1=xa[u][:, n * 512:(n + 1) * 512], op=ALU.add),
                          [tmpx[n], xa[u]], [x1[u]])
                cx.dma("sp", sc["ACC"].ap()[li * 128:(li + 1) * 128, :], x1[u][:], [x1[u]], [("OUTX", li)])
                cx.actv(h2f[u][:], x1[u][:], AF.Square, [x1[u]], [h2f[u], ssq[u]], accum=ssq[u][:])
                cx.actv(rs[u][:], ssq[u][:], AF.Sqrt, [ssq[u], C.eps], [rs[u]], bias=C.eps[:], scale=1.0 / D)
                cx.ew("dve", lambda e, u=u: e.reciprocal(out=rs[u][:], in_=rs[u][:]), [rs[u]], [rs[u]])
                cx.ew("dve", lambda e, u=u: e.scalar_tensor_tensor(out=h2f[u][:], in0=x1[u][:], scalar=rs[u][:, 0:1], in1=C.gtrow[:, 3072:4096],
                                                                   op0=ALU.mult, op1=ALU.mult), [x1[u], rs[u], C.gtrow], [h2f[u]])
                cx.ew("pool", lambda e, u=u: e.tensor_tensor(out=h2f[u][:], in0=h2f[u][:], in1=C.gtrow[:, 2048:3072], op=ALU.add),
                      [h2f[u], C.gtrow], [h2f[u]])
                cx.ew("act", lambda e, u=u: e.copy(out=h2x[u][:, 0:512].bitcast(BF16), in_=h2f[u][:]), [h2f[u]], [h2x[u]])
                if pend2 is not None:
                    pend2()
                pend2 = (lambda u=u, li=li: part2(u, li))
            if pend2 is not None:
                pend2()
                pend2 = None
        if "merge_small" in SKIP:
            pass
        elif GROUPS is None:
            cx.dma("sp", sc["AFFG"].ap()[0:4096, :], sc["AFF"].ap(), ["AFF"], ["AFFG"])
        else:
            collective_gather(cx, sc["AFF"], sc["AFFG"], ["AFF"], ["AFFG"])
        cx.P.flush()


BIGI = 1 << 20


def moe_prefill(cx, C):
    sc = cx.dram
    zrow = cx.sb(cx.stack, "zrow", [128, 529])
    cx.ew("pool", lambda e: e.memset(zrow[:], 0.0), [], [zrow])
    cx.ew("pool", lambda e: e.iota(zrow[:, 512:513].bitcast(I32), pattern=[[0, 1]], base=4096, channel_multiplier=1), [zrow], [zrow])
    for e_ in range(16):
        cx.dma("sp", sc["XE%d" % e_].ap().rearrange("(n p) f -> p n f", p=128), zrow[:].unsqueeze(1).to_broadcast([128, 9, 529]),
               [zrow], [("XE", e_)])


def phase_moe(cx, C, din, out_x):
    sc = cx.dram
    NE = 16 if "moe_small" not in SKIP else 2
    NIT = 32
    with ExitStack() as st:
        ones = cx.sb(st, "ones_moe", [128, 128])
        slt = cx.sb(st, "slt", [128, 128])
        cx.ew("pool", lambda e: e.memset(ones[:], 1.0), [], [ones])
        cx.ew("pool", lambda e: e.memset(slt[:], 1.0), [], [slt])
        cx.ew("pool", lambda e: e.affine_select(out=slt[:], in_=slt[:], pattern=[[1, 128]], compare_op=ALU.is_gt, fill=0.0, base=0,
                                                channel_multiplier=-1), [slt], [slt])
        desti = cx.sb(st, "desti", [128, 32, 16], I32)
        wsrc = {"g": din["w_gate"], "u": din["w_up"], "d": din["w_down"]}
        wt = {k: [cx.sb(st, "w%s%d" % (k, i), [128, 8, 1024], BF16) for i in range(2)] for k in "gud"}
        stg = [cx.sb(st, "estg%d" % i, [128, 1, 1024]) for i in range(3)]
        nstc = [0]

        hbuf = [cx.sb(st, "hbuf%d" % i, [128, 529]) for i in range(4)]
        nhb = [0]

        def scatter(e_, i):
            hb = hbuf[nhb[0] % 4]
            nhb[0] += 1
            cx.dma("sp", hb[:], sc["H2X"].ap()[i * 128:(i + 1) * 128, :], [], [hb])
            cx.P.dma("pool", lambda e, i=i, e_=e_, hb=hb: e.indirect_dma_start(
                out=sc["XE%d" % e_].ap(), out_offset=bass.IndirectOffsetOnAxis(ap=desti[:, i, e_:e_ + 1], axis=0),
                in_=hb[:], in_offset=None),
                _keys([desti, hb]), _keys([("XE", e_)]))

        def load_w_gen(e_):
            b = e_ % 2
            pend = None
            for k_ in "gud":
                src = wsrc[k_].ap()[e_].rearrange("(k p) f -> p k f", p=128)
                for c4 in range(8):
                    nst = nstc[0]
                    nstc[0] += 1
                    sg = stg[nst % 3]
                    cx.dma("sp", sg[:], src[:, c4:c4 + 1, :], [], [sg])
                    if pend is not None:
                        pend()

                    def cast(sg=sg, k_=k_, c4=c4, nst=nst):
                        if nst % 2:
                            cx.ew("act", lambda e: e.copy(out=wt[k_][b][:, c4:c4 + 1, :], in_=sg[:]), [sg], [wt[k_][b]])
                        else:
                            cx.ew("dve", lambda e: e.tensor_copy(out=wt[k_][b][:, c4:c4 + 1, :], in_=sg[:]), [sg], [wt[k_][b]])
                    pend = cast
                    yield
            pend()

        for _ in load_w_gen(0):
            pass
        pdum = cx.sb(st, "pdum", [128, 1])
        pdi = cx.sb(st, "pdi", [128, 1], I32)
        cx.ew("pool", lambda e: e.iota(pdi[:], pattern=[[0, 1]], base=1024, channel_multiplier=1), [], [pdi])
        cx.ew("dve", lambda e: e.tensor_copy(out=pdum[:], in_=pdi[:]), [pdi], [pdum])
        with ExitStack() as st2:
            affg = cx.sb(st2, "affg", [128, 64, 16])
            cmp_ = cx.sb(st2, "cmp", [128, 64, 16])
            affo = cx.sb(st2, "affo", [128, 32, 16])
            msk = cx.sb(st2, "msk", [128, 32, 16])
            pos = TL(None, cmp_.k, view=cmp_[:, 0:32, :])
            csum = TL(None, cmp_.k, view=cmp_[:, 32:64, :])
            bef = cx.sb(st2, "bef", [128, 32, 16])
            eoff = cx.sb(st2, "eoff", [128, 16])
            lo = cx.sb(st2, "lo", [128, 16])
            hi = cx.sb(st2, "hi", [128, 16])
            mid = cx.sb(st2, "mid", [128, 16])
            cnt = cx.sb(st2, "cnt", [128, 16])
            ge = cx.sb(st2, "ge", [128, 16])
            d1 = cx.sb(st2, "d1", [128, 16])
            d2 = cx.sb(st2, "d2", [128, 16])
            ptot = cx.ps(st2, "ptot", [128, 512])
            pwi = cx.ps(st2, "pwi", [128, 512])
            pcs = cx.ps(st2, "pcs", [128, 512])
            cx.dma("sp", affg[:], sc["AFFG"].ap().rearrange("(p j) e -> p j e", p=128), ["AFFG"], [affg])
            cx.dma("sp", affo[:], sc["AFF"].ap().rearrange("(i p) e -> p i e", p=128), ["AFF"], [affo])
            cx.dma("sp", eoff[:], din["eoff"].ap().to_broadcast([128, 16]), [], [eoff])
            cx.ew("dve", lambda e: e.memset(lo[:], 0.0), [], [lo])
            for it in range(NIT):
                cj = 2.0 ** -(it + 1)
                cx.ew("dve", lambda e, cj=cj: e.tensor_scalar(out=mid[:], in0=lo[:], scalar1=cj, scalar2=None, op0=ALU.add), [lo], [mid])
                cx.ew("dve", lambda e: e.tensor_tensor(out=cmp_[:], in0=affg[:], in1=mid[:].unsqueeze(1).to_broadcast([128, 64, 16]),
                                                       op=ALU.is_gt), [affg, mid], [cmp_])
                cx.ew("dve", lambda e: e.reduce_sum(out=cnt[:], in_=cmp_[:].rearrange("p j e -> p e j"), axis=AX.X), [cmp_], [cnt])
                cx.mm(ptot[:, 0:16], ones[:], cnt[:], True, True, [ones, cnt], [ptot])
                cx.ew("dve", lambda e, cj=cj: e.tensor_scalar(out=ge[:], in0=ptot[:, 0:16], scalar1=1024.0, scalar2=cj, op0=ALU.is_ge,
                                                              op1=ALU.mult), [ptot], [ge])
                cx.ew("dve", lambda e: e.tensor_tensor(out=lo[:], in0=lo[:], in1=ge[:], op=ALU.add), [lo, ge], [lo])
            cx.ew("dve", lambda e: e.tensor_tensor(out=msk[:], in0=affo[:], in1=lo[:].unsqueeze(1).to_broadcast([128, 32, 16]), op=ALU.is_gt),
                  [affo, lo], [msk])
            cx.mm(pwi[:], slt[:], msk[:].rearrange("p i e -> p (i e)"), True, True, [slt, msk], [pwi])
            cx.mm(pcs[:], ones[:], msk[:].rearrange("p i e -> p (i e)"), True, True, [ones, msk], [pcs])
            cx.ew("dve", lambda e: e.tensor_copy(out=csum[:].rearrange("p i e -> p (i e)"), in_=pcs[:]), [pcs], [csum])
            cx.ew("dve", lambda e: e.memset(bef[:, 0, :], 0.0), [], [bef])
            for i in range(1, 32):
                cx.ew("dve", lambda e, i=i: e.tensor_tensor(out=bef[:, i, :], in0=bef[:, i - 1, :], in1=csum[:, i - 1, :], op=ALU.add),
                      [bef, csum], [bef])
            cx.ew("dve", lambda e: e.tensor_tensor(out=pos[:].rearrange("p i e -> p (i e)"), in0=pwi[:],
                                                   in1=bef[:].rearrange("p i e -> p (i e)"), op=ALU.add), [pwi, bef], [pos])
            cx.ew("dve", lambda e: e.tensor_scalar(out=csum[:], in0=pos[:], scalar1=1024.0, scalar2=None, op0=ALU.is_lt), [pos], [csum])
            cx.ew("dve", lambda e: e.tensor_tensor(out=msk[:], in0=msk[:], in1=csum[:], op=ALU.mult), [msk, csum], [msk])
            cx.ew("dve", lambda e: e.tensor_scalar(out=pos[:], in0=pos[:], scalar1=pdum[:, 0:1], scalar2=None, op0=ALU.subtract), [pos, pdum], [pos])
            cx.ew("dve", lambda e: e.tensor_tensor(out=pos[:], in0=pos[:], in1=msk[:], op=ALU.mult), [pos, msk], [pos])
            cx.ew("dve", lambda e: e.tensor_scalar(out=pos[:], in0=pos[:], scalar1=pdum[:, 0:1], scalar2=None, op0=ALU.add), [pos, pdum], [pos])
            cx.ew("dve", lambda e: e.tensor_copy(out=desti[:], in_=pos[:]), [pos], [desti])
            if cx.debug:
                cx.dma("sp", sc["DBGM"].ap()[:, 0:16], lo[:], [lo], ["dbgm1"])
                cx.dma("sp", sc["DBGD"].ap().rearrange("(i p) e -> p i e", p=128), desti[:], [desti], ["dbgm2"])
            cx.P.flush()
        xeh = cx.sb(st, "xeh", [128, 8, 512])
        xem = [cx.sb(st, "xem%d" % i, [128, 8, 17]) for i in range(2)]
        xeT = cx.sb(st, "xeT", [128, 8, 1024], BF16)
        aT = cx.sb(st, "aTe", [128, 8, 1024], BF16)
        act_ = [cx.sb(st, "eact%d" % i, [128, 512]) for i in range(2)]
        yg = [cx.sb(st, "yg%d" % i, [128, 1024]) for i in range(2)]
        ptr = [cx.ps(st, "eptr%d" % i, [128, 1024], BF16) for i in range(2)]
        pg = [cx.ps(st, "epg%d" % i, [128, 512]) for i in range(2)]
        pu = [cx.ps(st, "epu%d" % i, [128, 512]) for i in range(2)]
        py = [cx.ps(st, "epy%d" % i, [128, 512]) for i in range(2)]
        nst = 0
        n1 = 0
        n2 = 0
        for i in range(32):
            scatter(0, i)
        for e_ in range(NE):
            b = e_ % 2
            nxtw = load_w_gen(e_ + 1) if e_ + 1 < NE else iter(())
            nxts = iter([(e_ + 1, i) for i in range(32)] if e_ + 1 < NE else [])
            xsrc_ = sc["XE%d" % e_].ap()[0:1024, :].rearrange("(s p) f -> p s f", p=128)
            cx.dma("sp", xeh[:], xsrc_[:, :, 0:512], [("XE", e_)], [xeh])
            cx.dma("sp", xem[b][:], xsrc_[:, :, 512:529], [("XE", e_)], [xem[b]])
            for s_ in range(8):
                pt_ = ptr[s_ % 2]
                xb_ = xeh[:, s_, :].bitcast(BF16)
                for k in range(8):
                    cx.tr(pt_[:, k * 128:(k + 1) * 128], xb_[:, k * 128:(k + 1) * 128], C.identb[:], [xeh, C.identb], [pt_])
                cx.ew("act" if s_ % 2 else "dve", lambda e, pt_=pt_, s_=s_: (e.copy if hasattr(e, "copy") else e.tensor_copy)(
                    out=xeT[:, :, s_ * 128:(s_ + 1) * 128], in_=pt_[:].rearrange("p (k t) -> p k t", k=8)), [pt_], [xeT])
            for fo in range(8):
                for hf_ in range(2):
                    g_, u_ = pg[n1 % 2], pu[n1 % 2]
                    a_ = act_[n1 % 2]
                    n1 += 1
                    cs = slice(hf_ * 512, (hf_ + 1) * 512)
                    for k in range(8):
                        cx.mm(g_[:], wt["g"][b][:, k, fo * 128:(fo + 1) * 128], xeT[:, k, cs], k == 0, k == 7, [wt["g"][b], xeT], [g_])
                    for k in range(8):
                        cx.mm(u_[:], wt["u"][b][:, k, fo * 128:(fo + 1) * 128], xeT[:, k, cs], k == 0, k == 7, [wt["u"][b], xeT], [u_])
                    cx.actv(a_[:], g_[:], AF.Silu, [g_], [a_])
                    cx.ew("dve", lambda e, a_=a_, u_=u_, fo=fo, cs=cs: e.tensor_tensor(out=aT[:, fo, cs], in0=u_[:], in1=a_[:], op=ALU.mult),
                          [u_, a_], [aT])
                    next(nxtw, None)
                    for _ in range(2):
                        sx = next(nxts, None)
                        if sx is not None:
                            scatter(*sx)
            for s_ in range(8):
                y_ = yg[s_ % 2]
                for hf_ in range(2):
                    p_ = py[n2 % 2]
                    n2 += 1
                    for fo in range(8):
                        cx.mm(p_[:], aT[:, fo, s_ * 128:(s_ + 1) * 128], wt["d"][b][:, fo, hf_ * 512:(hf_ + 1) * 512], fo == 0, fo == 7,
                              [aT, wt["d"][b]], [p_])
                    cx.ew("dve", lambda e, p_=p_, y_=y_, hf_=hf_, s_=s_, b=b, e_=e_: e.scalar_tensor_tensor(
                        out=y_[:, hf_ * 512:(hf_ + 1) * 512], in0=p_[:], scalar=xem[b][:, s_, 1 + e_:2 + e_],
                        in1=C.gtrow[:, 1024 + hf_ * 512:1024 + (hf_ + 1) * 512], op0=ALU.mult, op1=ALU.mult),
                        [p_, xem[b], C.gtrow], [y_])
                cx.P.dma("pool", lambda e, y_=y_, s_=s_, b=b: e.indirect_dma_start(
                    out=sc["ACC"].ap(), out_offset=bass.IndirectOffsetOnAxis(ap=xem[b][:, s_, 0:1].bitcast(I32), axis=0),
                    in_=y_[:], in_offset=None, compute_op=ALU.add),
                    _keys([y_, xem[b]]), _keys(["OUTACC"]))
                next(nxtw, None)
            for _ in nxtw:
                pass
        for g in range(8):
            cx.dma("sp", out_x.ap()[g * 512:(g + 1) * 512, :], sc["ACC"].ap()[g * 512:(g + 1) * 512, :], ["OUTACC"], [("OUTF", g)])
        cx.P.flush()


_CACHE = {}


def kernel(**inputs):
    inp = {k: np.asarray(v) for k, v in inputs.items()}
    if "nc" not in _CACHE:
        _CACHE["nc"] = build()
        _CACHE["tabs"] = const_tables()
    nc = _CACHE["nc"]
    tabs = _CACHE["tabs"]
    in_maps = []
    for core in range(8):
        ci = core_inputs(inp, core, tabs)
        in_maps.append({k: np.ascontiguousarray(ci[k], dtype=np.float32) for k in INPUT_SHAPES})
    res = run_bass_kernel_spmd(nc, in_maps, core_ids=list(range(8)))
    out = np.empty((4, S, D), np.float32)
    for core in range(8):
        b, hf = core // 2, core % 2
        o = np.asarray(res.results[core]["out_x"], dtype=np.float32)
        if hf == 0:
            out[b, :4096] = o
        else:
            out[b, 4096:] = o[::-1]
    return out
```

```python
import numpy as np
import concourse.bass as bass
import concourse.mybir as mybir
from concourse.bass_utils import run_bass_kernel_spmd

F32 = mybir.dt.float32
BF16 = mybir.dt.bfloat16
I32 = mybir.dt.int32
AF = mybir.ActivationFunctionType
ALU = mybir.AluOpType
AX = mybir.AxisListType

ENGS = ("pe", "act", "dve", "pool", "sp")
NDSEM = 8


class Prog:
    def __init__(self, nc, stack):
        self.nc = nc
        self.ops = {e: [] for e in ENGS}
        self.res = {}
        self.waited = {e: {} for e in ENGS}
        self.esem = {e: stack.enter_context(nc.semaphore("es_" + e)) for e in ENGS}
        self.dsem = {}
        self.dcnt = {}
        self.dnext = {}
        for q in ("sp", "pool", "act", "cc"):
            self.dsem[q] = [stack.enter_context(nc.semaphore("ds_%s%d" % (q, i))) for i in range(NDSEM)]
            self.dcnt[q] = [0] * NDSEM
            self.dnext[q] = 0

    def _deps(self, eng, reads, writes):
        deps = []
        for k in reads:
            st = self.res.get(k)
            if st is not None and st["w"] is not None:
                deps.append(st["w"])
        for k in writes:
            st = self.res.get(k)
            if st is not None:
                if st["w"] is not None:
                    deps.append(st["w"])
                deps.extend(st["r"])
        out = []
        wd = self.waited[eng]
        for d in deps:
            if d[0] == "e":
                _, src, idx = d
                if src == "pe" and eng == "pe":
                    continue
                key = ("e", src)
                if wd.get(key, -1) >= idx:
                    continue
                wd[key] = idx
                tgt = self.ops[src][idx]
                assert tgt["sig"] or not tgt.get("frozen"), "dependency on an already emitted, unsignalled op"
                tgt["sig"] = True
                out.append(d)
            else:
                _, q, slot, val = d
                key = ("d", q, slot)
                if wd.get(key, -1) >= val:
                    continue
                wd[key] = val
                out.append(d)
        return out

    def _record(self, dep, reads, writes):
        for k in writes:
            self.res[k] = {"w": dep, "r": []}
        for k in reads:
            st = self.res.setdefault(k, {"w": None, "r": []})
            st["r"].append(dep)
            if len(st["r"]) > 12:
                last = {}
                for d in st["r"]:
                    last[d[:2] if d[0] == "e" else d[:3]] = d
                st["r"] = list(last.values())

    def op(self, eng, fn, r=(), w=()):
        waits = self._deps(eng, r, w)
        idx = len(self.ops[eng])
        self.ops[eng].append({"fn": fn, "waits": waits, "sig": False, "dma": None})
        self._record(("e", eng, idx), r, w)
        return idx

    def pe(self, fn, r=(), w=()):
        return self.op("pe", fn, r, w)

    def act(self, fn, r=(), w=()):
        return self.op("act", fn, r, w)

    def dve(self, fn, r=(), w=()):
        return self.op("dve", fn, r, w)

    def pool(self, fn, r=(), w=()):
        return self.op("pool", fn, r, w)

    def dma(self, q, fn, r=(), w=(), grp=None, inc=16):
        grp = grp or q
        waits = self._deps(q, r, w)
        slot = self.dnext[grp]
        self.dnext[grp] = (slot + 1) % NDSEM
        prev = self.dcnt[grp][slot]
        wd = self.waited[q]
        if prev > 0 and wd.get(("d", grp, slot), -1) < prev:
            wd[("d", grp, slot)] = prev
            waits.append(("d", grp, slot, prev))
        val = prev + inc
        self.dcnt[grp][slot] = val
        idx = len(self.ops[q])
        self.ops[q].append({"fn": fn, "waits": waits, "sig": False, "dma": (grp, slot, inc)})
        self._record(("d", grp, slot, val), r, w)
        return idx

    def barrier(self):
        allk = "__all__"
        last = []
        for e in ENGS:
            if self.ops[e] and not self.ops[e][-1].get("frozen") and self.ops[e][-1]["fn"] is not None:
                last.append(("e", e, len(self.ops[e]) - 1))
        for q in self.dsem:
            for s in range(NDSEM):
                if self.dcnt[q][s] > 0:
                    last.append(("d", q, s, self.dcnt[q][s]))
        self.res[allk] = {"w": None, "r": last}
        for e in ENGS:
            self.op(e, None, r=(), w=(allk,))
            self.res[allk] = {"w": None, "r": last}
        del self.res[allk]

    def flush(self):
        nc = self.nc
        self.barrier()
        if not hasattr(self, "emitted"):
            self.emitted = {e: 0 for e in ENGS}
            self.sigbase = {e: 0 for e in ENGS}
        for e in ENGS:
            c = self.sigbase[e]
            for o in self.ops[e][self.emitted[e]:]:
                if o["sig"] and o["dma"] is None:
                    c += 1
                o["sigval"] = c
            self.sigbase[e] = c

        def run(e, name):
            for o in self.ops[name][self.emitted[name]:]:
                for d in o["waits"]:
                    if d[0] == "e":
                        tgt = self.ops[d[1]][d[2]]
                        assert tgt["sig"] and "sigval" in tgt
                        e.wait_ge(self.esem[d[1]], tgt["sigval"])
                    else:
                        e.wait_ge(self.dsem[d[1]][d[2]], d[3])
                if o["fn"] is None:
                    assert not o["sig"]
                    continue
                ins = o["fn"](e)
                if o["dma"] is not None:
                    q, slot, inc = o["dma"]
                    ins.then_inc(self.dsem[q][slot], inc)
                elif o["sig"]:
                    ins.then_inc(self.esem[name], 1)
                o["fn"] = None
            self.emitted[name] = len(self.ops[name])

        with nc.Block() as block:
            @block.sync
            def _(e):
                run(e, "sp")

            @block.scalar
            def _(e):
                run(e, "act")

            @block.vector
            def _(e):
                run(e, "dve")

            @block.gpsimd
            def _(e):
                run(e, "pool")

            @block.tensor
            def _(e):
                run(e, "pe")
        for e in ENGS:
            for o in self.ops[e]:
                o["frozen"] = True

    def emit(self):
        self.flush()


S = 8192
D = 1024
NCH = S // 512
EPS = 1e-6
PAIRS = [[0, 1], [2, 3], [4, 5], [6, 7]]
SKIP = set()
ATT_LAG = 2
ATT_NS = 3
ATT_NP = 4


class TL:
    def __init__(self, t, k, view=None, psum=False):
        self.t = t if view is None else view
        self.k = k
        self.psum = psum

    def __getitem__(self, idx):
        return self.t[idx]


def _keys(xs):
    return [getattr(x, "k", x) for x in xs]


def _rw(r, w):
    r2 = [x for x in r if not getattr(x, "psum", False)]
    w2 = list(w) + [x for x in r if getattr(x, "psum", False)]
    return _keys(r2), _keys(w2)


class Ctx:
    def __init__(self, nc, P, stack, debug):
        self.nc = nc
        self.P = P
        self.stack = stack
        self.debug = debug
        self.dram = {}
        self.n = 0

    def sb(self, stack, name, shape, dt=F32):
        self.n += 1
        nm = "%s_%d" % (name, self.n)
        return TL(stack.enter_context(self.nc.sbuf_tensor(nm, list(shape), dt)), nm)

    def ps(self, stack, name, shape, dt=F32):
        self.n += 1
        nm = "%s_%d" % (name, self.n)
        full = 512 if dt == F32 else 1024
        t = stack.enter_context(self.nc.psum_tensor(nm, [128, full], dt))
        assert len(shape) == 2 and shape[1] <= full
        return TL(None, nm, view=t[0:shape[0], 0:shape[1]], psum=True)

    def inp(self, name, shape, dt=F32):
        self.dram[name] = self.nc.dram_tensor(name, list(shape), dt, kind="ExternalInput")
        return self.dram[name]

    def scratch(self, name, shape, dt=F32, dbg=False):
        kind = "ExternalOutput" if (dbg and self.debug) else "Internal"
        self.dram[name] = self.nc.dram_tensor(name, list(shape), dt, kind=kind)
        return self.dram[name]

    def mm(self, out, lhsT, rhs, start, stop, r, w):
        self.P.pe(lambda e: e.matmul(out, lhsT=lhsT, rhs=rhs, start=start, stop=stop), *_rw(r, w))

    def tr(self, out, in_, ident, r, w):
        self.P.pe(lambda e: e.transpose(out=out, in_=in_, identity=ident), *_rw(r, w))

    def actv(self, out, in_, func, r, w, bias=None, scale=None, accum=None):
        kw = {}
        if bias is not None:
            kw["bias"] = bias
        if scale is not None:
            kw["scale"] = scale
        if accum is not None:
            kw["accum_out"] = accum
        self.P.act(lambda e: e.activation(out=out, in_=in_, func=func, **kw), *_rw(r, w))

    def ew(self, eng, fn, r, w):
        self.P.op(eng, fn, *_rw(r, w))

    def dma(self, q, out, in_, r, w):
        self.P.dma(q, lambda e: e.dma_start(out=out, in_=in_), *_rw(r, w))


def load_w_bf16(cx, st, wd, ncols, dst, stage, key_prefix, engs=("dve", "pool")):
    src = wd.ap().rearrange("(k p) f -> p k f", p=128)
    i = 0
    for c0 in range(0, ncols, 512):
        c1 = min(ncols, c0 + 512)
        sg = stage[i % 2]
        cx.dma("sp", sg[:, :, 0:c1 - c0], src[:, :, c0:c1], r=[], w=[sg])
        eng = engs[i % len(engs)]
        cx.ew(eng, lambda e, sg=sg, c0=c0, c1=c1: e.tensor_copy(out=dst[:, :, c0:c1], in_=sg[:, :, 0:c1 - c0]),
              r=[sg], w=[dst])
        i += 1


from contextlib import ExitStack


class Consts:
    pass


def setup_consts(cx, C, din):
    st = cx.stack
    C.identf = cx.sb(st, "identf", [128, 128])
    C.identb = cx.sb(st, "identb", [128, 128], BF16)
    cx.ew("pool", lambda e: e.memset(C.identf[:], 1.0), [], [C.identf])
    cx.ew("pool", lambda e: e.affine_select(out=C.identf[:], in_=C.identf[:], pattern=[[-1, 128]],
                                            compare_op=ALU.is_equal, fill=0.0, base=0, channel_multiplier=1),
          [C.identf], [C.identf])
    cx.ew("dve", lambda e: e.tensor_copy(out=C.identb[:], in_=C.identf[:]), [C.identf], [C.identb])
    C.eps = cx.sb(st, "epsc", [128, 1])
    cx.ew("pool", lambda e: e.memset(C.eps[:], EPS), [], [C.eps])
    C.modF = cx.sb(st, "modF", [128, 48])
    C.gtrow = cx.sb(st, "gtrow", [128, 4096])
    C.sc1F = cx.sb(st, "sc1F", [128, 8])
    C.sc2F = cx.sb(st, "sc2F", [128, 8])


def phase_adaln(cx, C, din):
    with ExitStack() as st:
        cT = cx.sb(st, "cT", [128, 8])
        sc = cx.sb(st, "sc", [128, 8])
        screp = cx.sb(st, "screp", [128, 8, 128])
        wst = [cx.sb(st, "wst%d" % i, [128, 8, 512]) for i in range(2)]
        modp = cx.ps(st, "modp", [128, 48])
        rowp = [cx.ps(st, "rowp%d" % i, [128, 512]) for i in range(2)]
        badaF = cx.sb(st, "badaF", [128, 48])
        bgt = cx.sb(st, "bgt", [128, 4096])
        n2row = cx.sb(st, "n2row", [128, 1024])
        n1 = cx.sb(st, "n1", [128, 8])
        n2 = cx.sb(st, "n2", [128, 8])
        tmp = cx.sb(st, "tmpa", [128, 8])
        cx.dma("sp", cT[:], din["cT"].ap(), [], [cT])
        cx.dma("sp", badaF[:], din["b_adaF"].ap(), [], [badaF])
        cx.dma("sp", bgt[:], din["b_gt"].ap().to_broadcast([128, 4096]), [], [bgt])
        cx.dma("sp", n2row[:], din["norm2R"].ap().to_broadcast([128, 1024]), [], [n2row])
        cx.dma("sp", n1[:], din["norm1F"].ap(), [], [n1])
        cx.dma("sp", n2[:], din["norm2F"].ap(), [], [n2])
        cx.actv(sc[:], cT[:], AF.Silu, [cT], [sc])
        for k in range(8):
            cx.ew("dve", lambda e, k=k: e.tensor_copy(out=screp[:, k, :], in_=sc[:, k:k + 1].to_broadcast([128, 128])),
                  [sc], [screp])
        wsrc = din["w_ada"].ap().rearrange("(k p) f -> p k f", p=128)
        for ch in range(12):
            ws = wst[ch % 2]
            cx.dma("sp", ws[:], wsrc[:, :, ch * 512:(ch + 1) * 512], [], [ws])
            for j in range(4):
                ft = ch * 4 + j
                for k in range(8):
                    cx.mm(modp[:, ft:ft + 1], ws[:, k, j * 128:(j + 1) * 128], sc[:, k:k + 1], k == 0, k == 7,
                          [ws, sc], [modp])
            if ch in (4, 5, 10, 11, 6, 7, 8, 9):
                rp = rowp[ch % 2]
                gi = {4: 0, 5: 1, 10: 2, 11: 3, 6: 4, 7: 5, 8: 6, 9: 7}[ch]
                for k in range(8):
                    cx.mm(rp[:], screp[:, k, :], ws[:, k, :], k == 0, k == 7, [ws, screp], [rp])
                cx.ew("dve", lambda e, rp=rp, gi=gi: e.tensor_tensor(out=C.gtrow[:, gi * 512:(gi + 1) * 512], in0=rp[:],
                                                                      in1=bgt[:, gi * 512:(gi + 1) * 512], op=ALU.add),
                      [rp, bgt], [C.gtrow])
        cx.ew("dve", lambda e: e.tensor_tensor(out=C.modF[:], in0=modp[:], in1=badaF[:], op=ALU.add),
              [modp, badaF], [C.modF])
        cx.ew("dve", lambda e: e.tensor_scalar_add(out=C.gtrow[:, 3072:4096], in0=C.gtrow[:, 3072:4096], scalar1=1.0), [C.gtrow], [C.gtrow])
        cx.ew("dve", lambda e: e.tensor_tensor(out=C.gtrow[:, 3072:4096], in0=C.gtrow[:, 3072:4096], in1=n2row[:], op=ALU.mult),
              [C.gtrow, n2row], [C.gtrow])
        for (dst, nw, lo) in ((C.sc1F, n1, 8), (C.sc2F, n2, 32)):
            cx.ew("dve", lambda e, lo=lo: e.tensor_scalar_add(out=tmp[:], in0=C.modF[:, lo:lo + 8], scalar1=1.0),
                  [C.modF], [tmp])
            cx.ew("dve", lambda e, dst=dst, nw=nw: e.tensor_tensor(out=dst[:], in0=tmp[:], in1=nw[:], op=ALU.mult),
                  [tmp, nw], [dst])
        cx.P.flush()


def rms_rows(cx, xt, ssq, rs, junk, nj):
    for j in range(nj):
        cx.actv(junk[:, j, :], xt[:, j, :], AF.Square, [xt], [junk, ssq], accum=ssq[:, j:j + 1])
    cx.actv(rs[:, 0:nj], ssq[:, 0:nj], AF.Sqrt, [ssq], [rs], bias=cx.C.eps[:], scale=1.0 / D)
    cx.ew("dve", lambda e: e.reciprocal(out=rs[:, 0:nj], in_=rs[:, 0:nj]), [rs], [rs])


def phase_proj(cx, C, din, hf):
    sc = cx.dram
    with ExitStack() as st:
        xt = [cx.sb(st, "xt%d" % i, [128, 4, 1024]) for i in range(2)]
        stage = [TL(None, xt[i].k, view=xt[i][:].rearrange("p j (a b) -> p (j a) b", b=512)) for i in range(2)]
        w_fmA = cx.sb(st, "w_fmA", [128, 8, 1024], BF16)
        w_fmQ = cx.sb(st, "w_fmQ", [128, 8, 512], BF16)
        w_tm = cx.sb(st, "w_tm", [128, 8, 416], BF16)
        load_w_bf16(cx, st, din["w_fmA"], 1024, w_fmA, stage, "wa")
        load_w_bf16(cx, st, din["w_fmQ"], 512, w_fmQ, stage, "wq")
        load_w_bf16(cx, st, din["w_tm"], 416, w_tm, stage, "wt")
        rm = cx.sb(st, "rm", [128, 128])
        bones = cx.sb(st, "bones", [128, 128])
        qkw = cx.sb(st, "qkw", [128, 2])
        cx.dma("sp", rm[:], din["Rm"].ap(), [], [rm])
        cx.dma("sp", bones[:], din["bones"].ap(), [], [bones])
        cx.dma("sp", qkw[:], din["qkw"].ap(), [], [qkw])
        xb = [cx.sb(st, "xb%d" % i, [128, 4, 1024], BF16) for i in range(2)]
        hT = [cx.sb(st, "hT%d" % i, [128, 8, 512], BF16) for i in range(2)]
        ssq = [cx.sb(st, "ssq%d" % i, [128, 4]) for i in range(2)]
        rs = [cx.sb(st, "rs%d" % i, [128, 4]) for i in range(2)]
        cs = [cx.sb(st, "cos%d" % i, [128, 512]) for i in range(2)]
        sn = [cx.sb(st, "sin%d" % i, [128, 512]) for i in range(2)]
        dbuf = [cx.sb(st, "dbuf%d" % i, [128, 6, 512]) for i in range(2)]
        qo = [cx.sb(st, "qo%d" % i, [128, 6, 512], BF16) for i in range(2)]
        va = [cx.sb(st, "va%d" % i, [128, 4, 132], BF16) for i in range(2)]
        ba = [cx.sb(st, "ba%d" % i, [128, 4, 32]) for i in range(2)]
        dz = [cx.sb(st, "dz%d" % i, [128, 4, 256]) for i in range(2)]
        qx = [cx.sb(st, "qx%d" % i, [128, 512]) for i in range(3)]
        qsq = [cx.sb(st, "qsq%d" % i, [128, 512]) for i in range(3)]
        qrs = [cx.sb(st, "qrs%d" % i, [128, 512]) for i in range(3)]
        xn = [cx.sb(st, "xn%d" % i, [128, 512]) for i in range(3)]
        t1 = [cx.sb(st, "t1%d" % i, [128, 512]) for i in range(3)]
        t2 = [cx.sb(st, "t2%d" % i, [128, 512]) for i in range(3)]
        trp = [cx.ps(st, "trp%d" % i, [128, 512], BF16) for i in range(2)]
        pj = [cx.ps(st, "pj%d" % i, [128, 512]) for i in range(2)]
        ssp = cx.ps(st, "ssp", [128, 512])
        rtp = cx.ps(st, "rtp", [128, 512])
        tmp_ = [cx.ps(st, "tmp%d" % i, [128, 512]) for i in range(2)]
        for i in range(2):
            cx.ew("pool", lambda e, i=i: e.memset(va[i][:], 1.0), [], [va[i]])
        xsrc = din["x"].ap()
        npj = 0
        nqk = 0
        for c in range(NCH):
            b = c % 2
            own = c < 8
            t0 = c * 512
            cx.dma("sp", xt[b][:], xsrc[t0:t0 + 512, :].rearrange("(j p) d -> p j d", p=128), [], [xt[b]])
            cx.dma("sp", cs[b][:], din["cosT"].ap()[:, t0:t0 + 512], [], [cs[b]])
            cx.dma("sp", sn[b][:], din["sinT"].ap()[:, t0:t0 + 512], [], [sn[b]])
            rms_rows(cx, xt[b], ssq[b], rs[b], xb[b], 4)
            for j in range(4):
                cx.ew("dve", lambda e, j=j, b=b: e.tensor_scalar(out=xb[b][:, j, :], in0=xt[b][:, j, :],
                                                                 scalar1=rs[b][:, j:j + 1], scalar2=None, op0=ALU.mult),
                      [xt[b], rs[b]], [xb[b]])
            for k in range(8):
                tp = trp[k % 2]
                for j in range(4):
                    cx.tr(tp[:, j * 128:(j + 1) * 128], xb[b][:, j, k * 128:(k + 1) * 128], C.identb[:],
                          [xb[b], C.identb], [tp])
                cx.actv(hT[b][:, k, :], tp[:], AF.Identity, [tp, C.sc1F, C.modF], [hT[b]],
                        scale=C.sc1F[:, k:k + 1], bias=C.modF[:, k:k + 1])
            cx.dma("sp", sc["HT"].ap()[:, :, t0:t0 + 512], hT[b][:], [hT[b]], [("HT", c)])
            tiles = [("A", i) for i in range(8)] + ([("Q", i) for i in range(4)] if own else [])
            if "fm" in SKIP:
                tiles = []

            def stageB(s, wc):
                cx.mm(ssp[:], bones[:], qsq[s][:], True, True, [bones, qsq[s]], [ssp])
                cx.actv(qrs[s][:], ssp[:], AF.Sqrt, [ssp], [qrs[s]], bias=C.eps[:], scale=1.0 / 64)
                cx.ew("dve", lambda e, s=s: e.reciprocal(out=qrs[s][:], in_=qrs[s][:]), [qrs[s]], [qrs[s]])
                cx.ew("dve", lambda e, s=s, wc=wc: e.scalar_tensor_tensor(out=xn[s][:], in0=qx[s][:], scalar=wc,
                                                                          in1=qrs[s][:], op0=ALU.mult, op1=ALU.mult),
                      [qx[s], qrs[s], qkw], [xn[s]])

            def stageC(s, slot, b):
                cx.mm(rtp[:], rm[:], xn[s][:], True, True, [rm, xn[s]], [rtp])
                cx.ew("pool", lambda e, s=s, b=b: e.tensor_tensor(out=t1[s][:], in0=xn[s][:], in1=cs[b][:], op=ALU.mult),
                      [xn[s], cs[b]], [t1[s]])
                cx.ew("dve", lambda e, s=s, b=b: e.tensor_tensor(out=t2[s][:], in0=rtp[:], in1=sn[b][:], op=ALU.mult),
                      [rtp, sn[b]], [t2[s]])
                cx.ew("dve", lambda e, s=s, b=b, slot=slot: e.tensor_tensor(out=qo[b][:, slot, :], in0=t1[s][:], in1=t2[s][:],
                                                                             op=ALU.add),
                      [t1[s], t2[s]], [qo[b]])

            qB = []
            qC = []

            def advance():
                if qC:
                    qC.pop(0)()
                if qB:
                    qB.pop(0)()

            def mkB(s, wc, slot, b):
                def f_():
                    stageB(s, wc)
                    qC.append(lambda: stageC(s, slot, b))
                return f_

            for (kind, i) in tiles:
                p = pj[npj % 2]
                npj += 1
                wsrc_ = w_fmA if kind == "A" else w_fmQ
                for k in range(8):
                    cx.mm(p[:], wsrc_[:, k, i * 128:(i + 1) * 128], hT[b][:, k, :], k == 0, k == 7,
                          [wsrc_, hT[b]], [p])
                if kind == "A" and i >= 2:
                    cx.ew("act", lambda e, p=p, i=i, b=b: e.copy(out=dbuf[b][:, i - 2, :], in_=p[:]), [p], [dbuf[b]])
                    advance()
                    continue
                if "qk" in SKIP:
                    continue
                s = nqk % 3
                nqk += 1
                wc = qkw[:, 0:1] if kind == "Q" else qkw[:, 1:2]
                slot = i if kind == "A" else 2 + i
                cx.ew("act", lambda e, p=p, s=s: e.copy(out=qx[s][:], in_=p[:]), [p], [qx[s]])
                cx.actv(qsq[s][:], p[:], AF.Square, [p], [qsq[s]])
                advance()
                qB.append(mkB(s, wc, slot, b))
            cx.dma("sp", sc["DPRE"].ap()[:, :, t0:t0 + 512].rearrange("j p t -> p j t"), dbuf[b][:], [dbuf[b]], [("DPRE", c)])
            for j in range(4 if "tm" not in SKIP else 0):
                tp = tmp_[j % 2]
                for k in range(8):
                    cx.mm(tp[:, 0:416], hT[b][:, k, j * 128:(j + 1) * 128], w_tm[:, k, :], k == 0, k == 7,
                          [hT[b], w_tm], [tp])
                for kv in range(2 if "tmva" not in SKIP else 0):
                    cx.ew("dve", lambda e, tp=tp, j=j, kv=kv, b=b: e.tensor_copy(out=va[b][:, j, kv * 66:kv * 66 + 64],
                                                                                in_=tp[:, kv * 64:(kv + 1) * 64]),
                          [tp], [va[b]])
                if "tmba" not in SKIP:
                    cx.ew("dve", lambda e, tp=tp, j=j, b=b: e.tensor_copy(out=ba[b][:, j, :], in_=tp[:, 128:160]), [tp], [ba[b]])
                if "tmdz" not in SKIP:
                    cx.actv(dz[b][:, j, :], tp[:, 160:416], AF.Silu, [tp], [dz[b]])
                advance()
            while qB or qC:
                advance()
            cx.dma("sp", sc["KT"].ap()[:, :, t0:t0 + 512].rearrange("j p t -> p j t"), qo[b][:, 0:2, :], [qo[b]], [("KT", c)])
            if own:
                l0 = t0
                cx.dma("sp", sc["QT"].ap()[:, :, l0:l0 + 512].rearrange("j p t -> p j t"), qo[b][:, 2:6, :], [qo[b]],
                       [("QT", c)])
            cx.dma("sp", sc["VA"].ap()[t0:t0 + 512, :].rearrange("(j p) f -> p j f", p=128), va[b][:], [va[b]], [("VA", c)])
            cx.dma("sp", sc["BA"].ap()[t0:t0 + 512, :].rearrange("(j p) f -> p j f", p=128), ba[b][:], [ba[b]], [("BA", c)])
            cx.dma("sp", sc["DZS"].ap()[t0:t0 + 512, :].rearrange("(j p) f -> p j f", p=128), dz[b][:], [dz[b]], [("DZS", c)])
        cx.P.flush()


OFF = dict(aq=0, ak=512, av=640, dq=768, dk=1280, dv=1792, dz=2304, b=2816, a=2832, g=2848)


def _fm(v, ncol):
    return np.ascontiguousarray(np.asarray(v, np.float32).reshape(ncol, 128).T)


def const_tables():
    t = {}
    freqs = (np.float32(10000.0) ** (-(np.arange(16, dtype=np.float32) * np.float32(2.0) / np.float32(32)))).astype(np.float32)
    tok = np.arange(S)
    pos = np.stack([tok // 64, tok % 64], 0).astype(np.float32)
    cosT = np.zeros((128, S), np.float32)
    sinT = np.zeros((128, S), np.float32)
    rm = np.zeros((128, 128), np.float32)
    for p in range(128):
        d = p % 64
        a = d // 32
        i = d % 32
        ang = (pos[a] * freqs[i % 16]).astype(np.float32)
        cosT[p] = np.cos(ang)
        sinT[p] = np.sin(ang)
        if i < 16:
            rm[p + 16, p] = -1.0
        else:
            rm[p - 16, p] = 1.0
    t["cosT"], t["sinT"], t["Rm"] = cosT, sinT, rm
    bones = np.zeros((128, 128), np.float32)
    bones[:64, :64] = 1.0
    bones[64:, 64:] = 1.0
    t["bones"] = bones
    t["dnmask"] = dn_masks()
    return t


def core_inputs(inp, core, tabs):
    b, hf = core // 2, core % 2
    rev = hf == 1
    w_in = inp["w_in"][0]
    d = {}
    for k in ("Rm", "bones", "dnmask"):
        d[k] = tabs[k]
    d["cosT"] = np.ascontiguousarray(tabs["cosT"][:, ::-1]) if rev else tabs["cosT"]
    d["sinT"] = np.ascontiguousarray(tabs["sinT"][:, ::-1]) if rev else tabs["sinT"]
    xb = inp["x"][b]
    d["x"] = np.ascontiguousarray(xb[::-1] if rev else xb)
    d["cT"] = _fm(inp["c"][b], 8)
    d["w_ada"] = np.ascontiguousarray(inp["w_ada"][0])
    d["b_adaF"] = _fm(inp["b_ada"][0], 48)
    ba_ = inp["b_ada"][0]
    d["b_gt"] = np.ascontiguousarray(np.concatenate([ba_[2048:3072], ba_[5120:6144], ba_[3072:4096], ba_[4096:5120]])[None, :])
    d["norm2R"] = np.ascontiguousarray(inp["norm2_w"][0][None, :].astype(np.float32))
    d["norm1F"] = _fm(inp["norm1_w"][0], 8)
    d["norm2F"] = _fm(inp["norm2_w"][0], 8)
    ak = w_in[:, OFF["ak"]:OFF["ak"] + 128]
    h0 = hf * 256
    d["w_fmA"] = np.ascontiguousarray(np.concatenate(
        [ak[:, 0:64], ak[:, 0:64], ak[:, 64:128], ak[:, 64:128],
         w_in[:, OFF["dq"] + h0:OFF["dq"] + h0 + 256], w_in[:, OFF["dk"] + h0:OFF["dk"] + h0 + 256],
         w_in[:, OFF["dv"] + h0:OFF["dv"] + h0 + 256]], axis=1))
    d["w_fmQ"] = np.ascontiguousarray(w_in[:, 0:512])
    dirs = (1, 0) if rev else (0, 1)
    hs = slice(hf * 4, hf * 4 + 4)
    bcols = [w_in[:, OFF["b"] + dd * 8 + hf * 4:OFF["b"] + dd * 8 + hf * 4 + 4] for dd in dirs]
    acols = [w_in[:, OFF["a"] + dd * 8 + hf * 4:OFF["a"] + dd * 8 + hf * 4 + 4] for dd in dirs]
    pad = w_in[:, OFF["b"]:OFF["b"] + 16]
    d["w_tm"] = np.ascontiguousarray(np.concatenate(
        [w_in[:, OFF["av"]:OFF["av"] + 128]] + bcols + acols + [pad, w_in[:, OFF["dz"] + h0:OFF["dz"] + h0 + 256]], axis=1))
    cwl = []
    cw = inp["conv_w"][0][::-1] if rev else inp["conv_w"][0]
    for base in (0, 512, 1024):
        for jp in range(2):
            c0 = base + h0 + jp * 128
            cwl.append(cw[:, c0:c0 + 128].T)
    d["conv_wF"] = np.ascontiguousarray(np.stack(cwl, 1).astype(np.float32))
    al = np.stack([inp["a_log"][0][dd, hs] for dd in dirs], 0)
    db = np.stack([inp["dt_bias"][0][dd, hs] for dd in dirs], 0)
    d["agrow"] = np.ascontiguousarray(np.stack([al, db], 0)[None].astype(np.float32))
    d["w_gates"] = np.ascontiguousarray(w_in[:, OFF["g"]:OFF["g"] + 2048])
    d["w_attn_up"] = np.ascontiguousarray(inp["w_attn_up"][0])
    wdu = inp["w_dn_up"][0]
    d["w_dn_up"] = np.ascontiguousarray(np.concatenate([wdu[h0:h0 + 256], wdu[256 - h0:512 - h0]], 0))
    d["w_o"] = np.ascontiguousarray(inp["w_o"][0])
    d["w_router"] = np.ascontiguousarray(inp["w_router"][0])
    d["eoff"] = (np.arange(16, dtype=np.float32) * 1024.0)[None, :].astype(np.float32)
    d["w_gate"] = np.ascontiguousarray(inp["w_gate"][0])
    d["w_up"] = np.ascontiguousarray(inp["w_up"][0])
    d["w_down"] = np.ascontiguousarray(inp["w_down"][0])
    d["sel"] = np.array([[1.0, 0.0]] if rev else [[0.0, 1.0]], np.float32)
    d["dnw_row"] = np.ascontiguousarray(inp["dn_norm_w"][0][None, :].astype(np.float32))
    d["qkrow"] = np.ascontiguousarray(np.stack([inp["q_norm_w"][0], inp["k_norm_w"][0]], 0)[None].astype(np.float32))
    d["qkw"] = np.ascontiguousarray(np.stack([np.tile(inp["q_norm_w"][0], 2), np.tile(inp["k_norm_w"][0], 2)], 1).astype(np.float32))
    return d


INPUT_SHAPES = dict(
    x=[S, D], cT=[128, 8], w_ada=[D, 6144], b_adaF=[128, 48], b_gt=[1, 4096], norm2R=[1, 1024], norm1F=[128, 8], norm2F=[128, 8],
    w_fmA=[D, 1024], w_fmQ=[D, 512], w_tm=[D, 416], qkw=[128, 2],
    cosT=[128, S], sinT=[128, S], Rm=[128, 128], bones=[128, 128], qkrow=[1, 2, 64], conv_wF=[128, 6, 5], dnmask=[128, 9, 128], agrow=[1, 2, 2, 4], dnw_row=[1, 64], w_gates=[D, 2048], w_attn_up=[512, D], w_dn_up=[512, D], w_o=[D, D], w_router=[D, 16], sel=[1, 2], eoff=[1, 16], w_gate=[16, D, D], w_up=[16, D, D], w_down=[16, D, D],
)


def build(hf_sym=None, debug=False, upto=99, hf=0):
    nc = bass.Bass("TRN2", target_bir_lowering=False)
    with ExitStack() as st:
        P = Prog(nc, st)
        cx = Ctx(nc, P, st, debug)
        C = Consts()
        cx.C = C
        din = {k: cx.inp(k, shp) for k, shp in INPUT_SHAPES.items()}
        cx.scratch("HT", [128, 8, S], BF16, dbg=True)
        cx.scratch("QT", [4, 128, 4096], BF16, dbg=True)
        cx.scratch("KT", [2, 128, S], BF16, dbg=True)
        cx.scratch("VA", [S, 132], BF16, dbg=True)
        cx.scratch("DPRE", [6, 128, S], F32, dbg=True)
        cx.scratch("BA", [S, 32], F32, dbg=True)
        cx.scratch("DZS", [S, 256], F32, dbg=True)
        if debug:
            dbg_mod = nc.dram_tensor("dbg_mod", [128, 48 + 2048 + 16], F32, kind="ExternalOutput")
        setup_consts(cx, C, din)
        for e_ in range(16):
            cx.scratch("XE%d" % e_, [1024 + 128, 529], F32, dbg=False)
        if upto >= 8:
            moe_prefill(cx, C)
        phase_adaln(cx, C, din)
        if debug:
            cx.dma("sp", dbg_mod.ap()[:, 0:48], C.modF[:], [C.modF], ["dbg1"])
            cx.dma("sp", dbg_mod.ap()[:, 48:48 + 2048], C.gtrow[:, 0:2048], [C.gtrow], ["dbg2"])
            cx.dma("sp", dbg_mod.ap()[:, 2096:2104], C.sc1F[:], [C.sc1F], ["dbg3"])
            cx.dma("sp", dbg_mod.ap()[:, 2104:2112], C.sc2F[:], [C.sc2F], ["dbg4"])
        if upto >= 2 and "noproj" not in SKIP:
            phase_proj(cx, C, din, hf)
        cx.scratch("ATT", [512, 4096], BF16, dbg=True)
        merged_cd = upto >= 4 and "noattn" not in SKIP and "nodnpre" not in SKIP and "mergecd" in SKIP
        cx.scratch("DQT", [2, 128, S], BF16, dbg=True)
        cx.scratch("DKT", [2, 128, S], BF16, dbg=True)
        cx.scratch("DKV", [S, 512], BF16, dbg=True)
        if merged_cd:
            phase_attn(cx, C, din, hf, with_dnpre=True)
        else:
            if upto >= 3 and "noattn" not in SKIP:
                phase_attn(cx, C, din, hf)
            if upto >= 4 and "nodnpre" not in SKIP:
                phase_dn_pre(cx, C, din, hf)
        cx.scratch("OD", [2, S, 256], F32, dbg=True)
        wstack = ExitStack()
        if upto >= 7 and "nomerge" not in SKIP:
            merge_weights(cx, C, din, wstack)
        if upto >= 5 and "nodn" not in SKIP:
            phase_dn(cx, C, din, hf)
        cx.scratch("DNT", [256, 4096], BF16, dbg=True)
        cx.scratch("DNTR", [256, 4096], BF16, dbg=(GROUPS is None))
        cx.scratch("DNGR", [512, 4096], BF16, dbg=(GROUPS is None))
        if upto >= 6 and "nodnout" not in SKIP:
            phase_dn_out(cx, C, din, hf)
        out_x = nc.dram_tensor("out_x", [4096 + 128, D], F32, kind="ExternalOutput")
        cx.dram["ACC"] = out_x
        cx.scratch("H2X", [4096, 529], F32, dbg=True)
        cx.scratch("AFF", [4096, 16], F32, dbg=False)
        cx.scratch("AFFG", [S, 16], F32, dbg=True)
        if upto >= 7 and "nomerge" not in SKIP:
            phase_merge(cx, C, din, hf, out_x)
        wstack.close()
        if debug:
            cx.scratch("DBGM", [128, 16], F32, dbg=True)
            cx.scratch("DBGD", [4096, 16], I32, dbg=True)
        if upto >= 8:
            phase_moe(cx, C, din, out_x)
        P.emit()
    return nc


def phase_attn(cx, C, din, hf, with_dnpre=False):
    sc = cx.dram
    NQC = 4096 // 256
    NKT = S // 128
    if "attn_small" in SKIP:
        NKT = 4
    with ExitStack() as st:
        kt2 = [cx.sb(st, "kt2_%d" % i, [128, S], BF16) for i in range(2)]
        qbd2 = [cx.sb(st, "qbd_%d" % i, [128, NQC, 512], BF16) for i in range(2)]
        qbd = [qbd2[i % 2] for i in range(4)]
        va = cx.sb(st, "va_all", [128, 64, 132], BF16)
        side = phase_dn_pre(cx, C, din, hf, host=st) if with_dnpre else []
        for i in range(2):
            cx.dma("sp", kt2[i][:], sc["KT"].ap()[i], [], [kt2[i]])
        for i in range(2):
            cx.ew("pool" if i % 2 else "dve", lambda e, i=i: e.memset(qbd2[i][:], 0.0), [], [qbd2[i]])

        def load_q(i):
            cx.dma("sp", qbd[i][0:64, :, 0:256], sc["QT"].ap()[i, 0:64, :].rearrange("p (c t) -> p c t", t=256), [], [qbd[i]])
            cx.dma("sp", qbd[i][64:128, :, 256:512], sc["QT"].ap()[i, 64:128, :].rearrange("p (c t) -> p c t", t=256), [], [qbd[i]])

        load_q(0)
        load_q(1)
        for g in range(4):
            cx.dma("sp", va[:, g * 16:(g + 1) * 16, :],
                   sc["VA"].ap()[g * 2048:(g + 1) * 2048, :].rearrange("(kt p) f -> p kt f", p=128), [], [va])
        wrow = cx.sb(st, "wrow", [128, 2, 64])
        wmax = cx.sb(st, "wmax", [128, 2])
        negb = cx.sb(st, "negb", [128, 1])
        cx.dma("sp", wrow[:], din["qkrow"].ap().to_broadcast([128, 2, 64]), [], [wrow])
        cx.ew("dve", lambda e: e.reduce_max(out=wmax[:], in_=wrow[:], axis=AX.X, apply_absolute_value=True), [wrow], [wmax])
        cx.ew("dve", lambda e: e.scalar_tensor_tensor(out=negb[:], in0=wmax[:, 0:1], scalar=-8.0, in1=wmax[:, 1:2],
                                                      op0=ALU.mult, op1=ALU.mult), [wmax], [negb])
        ones = cx.sb(st, "ones_att", [128, 64])
        cx.ew("pool", lambda e: e.memset(ones[:], 1.0), [], [ones])
        sT = [cx.ps(st, "sT%d" % i, [128, 512]) for i in range(ATT_NS)]
        oT = [cx.ps(st, "oT%d" % i, [128, 512]) for i in range(2)]
        bc = cx.ps(st, "bc", [128, 512])
        pT = [cx.sb(st, "pT%d" % i, [128, 512], BF16) for i in range(ATT_NP)]
        rinv = [cx.sb(st, "rinv%d" % i, [128, 512]) for i in range(2)]
        osb = [cx.sb(st, "osb%d" % i, [64, 512]) for i in range(2)]
        ao = [cx.sb(st, "ao%d" % i, [64, 512], BF16) for i in range(2)]
        it = 0
        ns = 0
        NJ = 4 if "attn_1h" not in SKIP else 1
        ngrp = NJ * NQC
        nside = 0
        for j in range(NJ):
            kv = j // 2
            if j >= 2:
                load_q(j)
            for qc in range(NQC):
                while side and nside < len(side) and nside * ngrp <= it * len(side):
                    side[nside]()
                    nside += 1
                o = oT[it % 2]
                u = it % 2

                def S_mm(kt):
                    s_ = sT[(ns + kt) % ATT_NS]
                    cx.mm(s_[:], kt2[kv][:, kt * 128:(kt + 1) * 128], qbd[j][:, qc, :], True, True, [kt2[kv], qbd[j]], [s_])
                    p_ = pT[(ns + kt) % ATT_NP]
                    cx.actv(p_[:], s_[:], AF.Exp, [s_, negb], [p_], bias=negb[:], scale=0.125)

                def PV_mm(kt):
                    p_ = pT[(ns + kt) % ATT_NP]
                    cx.mm(o[0:65, :], va[:, kt, kv * 66:kv * 66 + 65], p_[:], kt == 0, kt == NKT - 1, [va, p_], [o])

                LAG = ATT_LAG
                for kt in range(NKT + LAG):
                    if kt < NKT:
                        S_mm(kt)
                    if kt >= LAG:
                        PV_mm(kt - LAG)
                ns += NKT
                cx.ew("dve", lambda e, o=o, u=u: e.reciprocal(out=rinv[u][64:65, :], in_=o[64:65, :]), [o], [rinv[u]])
                cx.ew("act", lambda e, o=o, u=u: e.copy(out=osb[u][:], in_=o[0:64, :]), [o], [osb[u]])
                cx.mm(bc[0:64, :], ones[64:65, 0:64], rinv[u][64:65, :], True, True, [ones, rinv[u]], [bc])
                cx.ew("dve", lambda e, u=u: e.tensor_tensor(out=ao[u][:], in0=osb[u][:], in1=bc[0:64, :], op=ALU.mult),
                      [osb[u], bc], [ao[u]])
                for hh in range(2):
                    h = 2 * j + hh
                    cx.dma("sp", sc["ATT"].ap()[h * 64:(h + 1) * 64, qc * 256:(qc + 1) * 256], ao[u][:, hh * 256:(hh + 1) * 256],
                           [ao[u]], [("ATT", h, qc)])
                it += 1
        while side and nside < len(side):
            side[nside]()
            nside += 1
        cx.P.flush()


def phase_dn_pre(cx, C, din, hf, host=None):
    sc = cx.dram
    H = S // 2
    NB = 3 if host is None else 1
    with ExitStack() as st_own:
        st = st_own if host is None else host
        pre = [cx.sb(st, "pre%d" % i, [128, H + 4]) for i in range(3 if host is None else 2)]
        acc = [cx.sb(st, "acc%d" % i, [128, H]) for i in range(4 if host is None else 2)]
        cw = cx.sb(st, "cw", [128, 6, 5])
        bones = cx.sb(st, "bones2", [128, 128])
        cx.dma("sp", cw[:], din["conv_wF"].ap(), [], [cw])
        cx.dma("sp", bones[:], din["bones"].ap(), [], [bones])
        sqt = [cx.sb(st, "sqt%d" % i, [128, 512]) for i in range(3)]
        rst = [cx.sb(st, "rst%d" % i, [128, 512]) for i in range(3)]
        tmo = [cx.sb(st, "tmo%d" % i, [128, 512], BF16) for i in range(3)]
        abf = [cx.sb(st, "abf%d" % i, [128, H], BF16) for i in range(2 if host is None else 1)]
        ssp = [cx.ps(st, "ssp2_%d" % i, [128, 512]) for i in range(NB)]
        trp = [cx.ps(st, "trp2_%d" % i, [128, 512]) for i in range(NB)]
        NP_, NA_, NF_ = len(pre), len(acc), len(abf)
        nn = [0]

        def stage1(nt):
            ti, half = nt // 2, nt % 2
            p_, a_ = pre[nt % NP_], acc[nt % NA_]
            if half == 0:
                cx.ew("pool", lambda e, p_=p_: e.memset(p_[:, 0:2], 0.0), [], [p_])
                cx.dma("sp", p_[:, 2:2050], sc["DPRE"].ap()[ti, :, 0:2048], [], [p_])
                cx.dma("sp", p_[:, 2050:H + 4], sc["DPRE"].ap()[ti, :, 2048:H + 2], [], [p_])
            else:
                cx.ew("pool", lambda e, p_=p_: e.memset(p_[:, H + 2:H + 4], 0.0), [], [p_])
                cx.dma("sp", p_[:, 0:2050], sc["DPRE"].ap()[ti, :, H - 2:H + 2048], [], [p_])
                cx.dma("sp", p_[:, 2050:H + 2], sc["DPRE"].ap()[ti, :, H + 2048:S], [], [p_])
            cx.ew("dve", lambda e, p_=p_, a_=a_, ti=ti: e.tensor_scalar(out=a_[:], in0=p_[:, 0:H], scalar1=cw[:, ti, 0:1], scalar2=None,
                                                                      op0=ALU.mult), [p_, cw], [a_])
            for k in range(1, 5):
                cx.ew("dve", lambda e, p_=p_, a_=a_, ti=ti, k=k: e.scalar_tensor_tensor(out=a_[:], in0=p_[:, k:k + H], scalar=cw[:, ti, k:k + 1],
                                                                                       in1=a_[:], op0=ALU.mult, op1=ALU.add),
                      [p_, cw, a_], [a_])
            cx.actv(a_[:], a_[:], AF.Silu, [a_], [a_])

        def stage2(nt):
            ti, half = nt // 2, nt % 2
            kind, jp = ti // 2, ti % 2
            a_ = acc[nt % NA_]
            ab = abf[nt % NF_]
            c0 = half * H
            if kind < 2:
                for c in range(H // 512):
                    s = nn[0] % 3
                    nn[0] += 1
                    sl = slice(c * 512, (c + 1) * 512)
                    cx.ew("pool", lambda e, s=s, a_=a_, sl=sl: e.tensor_tensor(out=sqt[s][:], in0=a_[:, sl], in1=a_[:, sl], op=ALU.mult),
                          [a_], [sqt[s]])
                    sp_ = ssp[s % NB]
                    cx.mm(sp_[:], bones[:], sqt[s][:], True, True, [bones, sqt[s]], [sp_])
                    cx.actv(rst[s][:], sp_[:], AF.Sqrt, [sp_, C.eps], [rst[s]], bias=C.eps[:], scale=1.0)
                    cx.ew("dve", lambda e, s=s: e.reciprocal(out=rst[s][:], in_=rst[s][:]), [rst[s]], [rst[s]])
                    if kind == 0:
                        cx.ew("pool", lambda e, s=s: e.tensor_scalar(out=rst[s][:], in0=rst[s][:], scalar1=0.125, scalar2=None, op0=ALU.mult),
                              [rst[s]], [rst[s]])
                    cx.ew("pool", lambda e, s=s, a_=a_, sl=sl: e.tensor_tensor(out=a_[:, sl], in0=a_[:, sl], in1=rst[s][:], op=ALU.mult),
                          [a_, rst[s]], [a_])
                    cx.ew("act", lambda e, a_=a_, sl=sl, ab=ab: e.copy(out=ab[:, sl], in_=a_[:, sl]), [a_], [ab])
                dst = sc["DQT"] if kind == 0 else sc["DKT"]
                for g in range(2):
                    cx.dma("sp", dst.ap()[jp, :, c0 + g * 2048:c0 + (g + 1) * 2048], ab[:, g * 2048:(g + 1) * 2048], [ab],
                           [(dst.name, jp, half, g)])
            if kind >= 1:
                col0 = (kind - 1) * 256 + jp * 128
                for c in range(H // 512):
                    s = nn[0] % 3
                    nn[0] += 1
                    tp_ = trp[s % NB]
                    for j in range(4):
                        t0 = c * 512 + j * 128
                        cx.tr(tp_[:, j * 128:(j + 1) * 128], a_[:, t0:t0 + 128], C.identf[:], [a_, C.identf], [tp_])
                    cx.ew("act", lambda e, s=s, tp_=tp_: e.copy(out=tmo[s][:], in_=tp_[:]), [tp_], [tmo[s]])
                    cx.dma("sp", sc["DKV"].ap()[c0 + c * 512:c0 + (c + 1) * 512, col0:col0 + 128].rearrange("(j p) f -> p j f", p=128),
                           tmo[s][:].rearrange("p (j f) -> p j f", f=128), [tmo[s]], [("DKV", ti, half, c)])

        items = [lambda: stage1(0)]
        for nt in range(12):
            if nt + 1 < 12:
                items.append(lambda nt=nt: stage1(nt + 1))
            items.append(lambda nt=nt: stage2(nt))
        if host is not None:
            return items
        for it_ in items:
            it_()
        cx.P.flush()


def dn_masks():
    p = np.arange(128)[:, None]
    f = np.arange(128)[None, :]
    same = (p // 64) == (f // 64)
    m = np.zeros((128, 9, 128), np.float32)
    m[:, 0] = same & (p <= f)
    m[:, 1] = same & (p >= f)
    m[:, 2] = same
    m[:, 3] = same & (f >= p)
    m[:, 4] = same & (f <= p)
    m[:, 5] = -1.0 * (same & (f < p))
    m[:, 6] = -1.0 * (same & (f > p))
    m[:, 7] = (p // 32) == (f // 32)
    m[:64, 8, 0] = 1.0
    m[64:, 8, 1] = 1.0
    return m


def phase_dn(cx, C, din, hf):
    sc = cx.dram
    NP = S // 128
    if "dn_small" in SKIP:
        NP = 2
    with ExitStack() as st:
        mk = cx.sb(st, "dnmask", [128, 9, 128])
        cx.dma("sp", mk[:], din["dnmask"].ap(), [], [mk])
        ones = cx.sb(st, "ones_dn", [128, 128])
        cx.ew("pool", lambda e: e.memset(ones[:], 1.0), [], [ones])
        bg = cx.sb(st, "bg", [128, 64, 2, 2, 4])
        with ExitStack() as st2:
            ba = cx.sb(st2, "ba_all", [128, 64, 32])
            agr = cx.sb(st2, "agr", [128, 2, 2, 4])
            nea = cx.sb(st2, "nea", [128, 2, 4])
            tmpg = cx.sb(st2, "tmpg", [128, 64, 4])
            cx.dma("sp", ba[:], sc["BA"].ap().rearrange("(i p) f -> p i f", p=128), [], [ba])
            cx.dma("sp", agr[:], din["agrow"].ap().to_broadcast([128, 2, 2, 4]), [], [agr])
            cx.actv(nea[:], agr[:, 0], AF.Exp, [agr], [nea])
            cx.ew("dve", lambda e: e.tensor_scalar(out=nea[:], in0=nea[:], scalar1=-1.0, scalar2=None, op0=ALU.mult), [nea], [nea])
            for d in range(2):
                c0 = d * 4
                cx.actv(bg[:, :, d, 0, :], ba[:, :, c0:c0 + 4], AF.Sigmoid, [ba], [bg])
                cx.ew("dve", lambda e, d=d, c0=c0: e.tensor_tensor(out=tmpg[:], in0=ba[:, :, 8 + c0:8 + c0 + 4],
                                                                  in1=agr[:, 1, d, :].unsqueeze(1).to_broadcast([128, 64, 4]), op=ALU.add),
                      [ba, agr], [tmpg])
                cx.actv(tmpg[:], tmpg[:], AF.Exp, [tmpg], [tmpg])
                cx.actv(tmpg[:], tmpg[:], AF.Ln, [tmpg], [tmpg], bias=1.0, scale=1.0)
                cx.ew("dve", lambda e, d=d: e.tensor_tensor(out=bg[:, :, d, 1, :], in0=tmpg[:],
                                                           in1=nea[:, d, :].unsqueeze(1).to_broadcast([128, 64, 4]), op=ALU.mult),
                      [tmpg, nea], [bg])
            cx.P.flush()
        banks = [cx.ps(st, "dnb%d" % i, [128, 512]) for i in range(8)]
        nb = [0]

        def bank():
            nb[0] += 1
            return banks[nb[0] % 8]

        chains = {}
        for d in range(2):
            for jp in range(2):
                t = {}
                nm = "c%d%d_" % (d, jp)
                BFN = ("kq", "ktm", "vtm", "aT0", "aT1", "TT2", "vb", "kbg", "kdp0", "kdp1", "qg", "wT", "vnew", "Sbf")
                for name, shape in (("kq", [128, 256]), ("ktm", [128, 2, 64]), ("vtm", [128, 2, 64]), ("rhsg", [128, 2, 128]),
                                    ("gexp", [128, 2, 64]), ("gcs", [128, 6]), ("E", [128, 2, 128]), ("Emin", [128, 2, 128]),
                                    ("Emax", [128, 2, 128]), ("A", [128, 2, 128]), ("B", [128, 2, 128]), ("egr", [128, 128]),
                                    ("F2", [128, 2, 128]), ("Fm", [128, 2, 128]), ("aT0", [128, 128]), ("aT1", [128, 128]),
                                    ("TT2", [128, 2, 128]),
                                    ("sca", [128, 8]), ("vb", [128, 2, 64]), ("kbg", [128, 2, 64]), ("kdp0", [128, 128]),
                                    ("kdp1", [128, 128]), ("qg", [128, 128]), ("u", [128, 128]), ("wT", [128, 128]),
                                    ("vnew", [128, 128]), ("obuf", [128, 128]), ("Sst", [128, 128]), ("Sbf", [128, 128])):
                    t[name] = cx.sb(st, nm + name, shape, BF16 if name in BFN else F32)
                for hh in range(2):
                    for name in ("M", "Md", "Mo", "Nd", "X0", "X1", "Y0", "Y1", "PT", "W1", "Dn"):
                        t[name + str(hh)] = cx.sb(st, nm + name + str(hh), [128, 128])
                for z in ("kdp0", "kdp1", "vnew", "Sst", "Sbf"):
                    cx.ew("pool", lambda e, z=z, t=t: e.memset(t[z][:], 0.0), [], [t[z]])
                chains[(d, jp)] = t

        def inverse(t, hh, pG_):
            sfx = str(hh)
            M, Md, Mo, Nd, PT, W1, Dn = (t[n + sfx] for n in ("M", "Md", "Mo", "Nd", "PT", "W1", "Dn"))
            cx.ew("dve", lambda e: e.tensor_tensor(out=M[:], in0=pG_[:, 0:128], in1=t["Fm"][:, hh, :], op=ALU.mult), [pG_, t["Fm"]], [M])
            aT = t["aT%d" % hh]
            cx.ew("dve", lambda e: e.tensor_tensor(out=aT[:], in0=pG_[:, 128:256], in1=t["F2"][:, hh, :], op=ALU.mult), [pG_, t["F2"]], [aT])
            cx.ew("pool", lambda e: e.tensor_tensor(out=Md[:], in0=M[:], in1=mk[:, 7, :], op=ALU.mult), [M, mk], [Md])
            cx.ew("pool", lambda e: e.tensor_tensor(out=Mo[:], in0=M[:], in1=Md[:], op=ALU.subtract), [M, Md], [Mo])
            yield
            pb = bank()
            cx.tr(pb[:, 0:128], Md[:], C.identf[:], [Md, C.identf], [pb])
            cx.ew("act", lambda e, pb=pb: e.copy(out=Nd[:], in_=pb[:, 0:128]), [pb], [Nd])
            cx.ew("dve", lambda e, pb=pb: e.tensor_tensor(out=PT[:], in0=pb[:, 0:128], in1=C.identf[:], op=ALU.add), [pb, C.identf], [PT])
            yield
            X, Y = Md, Nd
            pend = None
            for j in range(1, 5):
                Xn, Yn = t["X%d" % (j % 2) + sfx], t["Y%d" % (j % 2) + sfx]
                if j < 4:
                    pb = bank()
                    cx.mm(pb[:, 0:128], X[:], Y[:], True, True, [X, Y], [pb])
                    cx.ew("act", lambda e, pb=pb, Yn=Yn: e.copy(out=Yn[:], in_=pb[:, 0:128]), [pb], [Yn])
                pb = bank()
                cx.mm(pb[:, 0:128], Y[:], X[:], True, True, [X, Y], [pb])
                cx.ew("dve", lambda e, pb=pb, Xn=Xn: e.tensor_copy(out=Xn[:], in_=pb[:, 0:128]), [pb], [Xn])
                if pend is not None:
                    pend()
                def upd(Xn=Xn):
                    pb2 = bank()
                    cx.mm(pb2[:, 0:128], Xn[:], PT[:], True, True, [Xn, PT], [pb2])
                    cx.ew("dve", lambda e, pb2=pb2: e.tensor_tensor(out=PT[:], in0=pb2[:, 0:128], in1=PT[:], op=ALU.add), [pb2, PT], [PT])
                pend = upd
                X, Y = Xn, Yn
                yield
            pend()
            yield
            pb = bank()
            cx.mm(pb[:, 0:128], Mo[:], PT[:], True, True, [Mo, PT], [pb])
            cx.ew("act", lambda e, pb=pb: e.copy(out=W1[:], in_=pb[:, 0:128]), [pb], [W1])
            pb = bank()
            cx.tr(pb[:, 0:128], PT[:], C.identf[:], [PT, C.identf], [pb])
            cx.ew("dve", lambda e, pb=pb: e.tensor_copy(out=Dn[:], in_=pb[:, 0:128]), [pb], [Dn])
            yield
            pb = bank()
            cx.mm(pb[:, 0:128], Dn[:], W1[:], True, True, [Dn, W1], [pb])
            cx.ew("dve", lambda e, pb=pb: e.tensor_tensor(out=t["TT2"][:, hh, :], in0=pb[:, 0:128], in1=PT[:], op=ALU.add),
                  [pb, PT], [t["TT2"]])
            yield

        def step(d, jp, i):
            t = chains[(d, jp)]
            t0 = i * 128
            tri = mk[:, 0 + d, :]
            ma = mk[:, 3 + d, :]
            mm_ = mk[:, 5 + d, :]
            g2 = bg[:, i, d, 1, jp * 2:jp * 2 + 2]
            b2 = bg[:, i, d, 0, jp * 2:jp * 2 + 2]
            cx.dma("sp", t["kq"][:, 0:128], sc["DKT"].ap()[jp, :, t0:t0 + 128], [], [t["kq"]])
            cx.dma("sp", t["kq"][:, 128:256], sc["DQT"].ap()[jp, :, t0:t0 + 128], [], [t["kq"]])
            cx.dma("sp", t["ktm"][:], sc["DKV"].ap()[t0:t0 + 128, jp * 128:jp * 128 + 128].rearrange("p (h f) -> p h f", f=64), [], [t["ktm"]])
            cx.dma("sp", t["vtm"][:], sc["DKV"].ap()[t0:t0 + 128, 256 + jp * 128:256 + jp * 128 + 128].rearrange("p (h f) -> p h f", f=64),
                   [], [t["vtm"]])
            cx.ew("dve", lambda e: e.tensor_tensor(out=t["rhsg"][:], in0=tri.unsqueeze(1).to_broadcast([128, 2, 128]),
                                                   in1=g2.unsqueeze(2).to_broadcast([128, 2, 128]), op=ALU.mult), [mk, bg], [t["rhsg"]])
            cx.ew("pool", lambda e: e.tensor_copy(out=t["gexp"][:], in_=g2.unsqueeze(2).to_broadcast([128, 2, 64])), [bg], [t["gexp"]])
            yield
            pA = bank()
            cx.mm(pA[:, 0:256], ones[:], t["rhsg"][:].rearrange("p h c -> p (h c)"), True, True, [ones, t["rhsg"]], [pA])
            cx.mm(pA[:, 256:258], tri, g2, True, True, [mk, bg], [pA])
            cx.mm(pA[:, 258:260], mk[:, 2, :], g2, True, True, [mk, bg], [pA])
            cx.mm(pA[:, 260:262], t["gexp"][:].rearrange("p h c -> p (h c)"), mk[:, 8, 0:2], True, True, [t["gexp"], mk], [pA])
            cx.ew("dve", lambda e: e.tensor_copy(out=t["gcs"][:], in_=pA[:, 256:262]), [pA], [t["gcs"]])
            cx.ew("dve", lambda e: e.tensor_tensor(out=t["E"][:], in0=pA[:, 0:256].rearrange("p (h c) -> p h c", h=2),
                                                   in1=t["gcs"][:, 0:2].unsqueeze(2).to_broadcast([128, 2, 128]), op=ALU.subtract),
                  [pA, t["gcs"]], [t["E"]])
            cx.actv(t["egr"][0:64, :], pA[0:64, 0:128], AF.Exp, [pA], [t["egr"]])
            cx.actv(t["egr"][64:128, :], pA[64:128, 128:256], AF.Exp, [pA], [t["egr"]])
            yield
            cx.ew("dve", lambda e: e.tensor_scalar_min(out=t["Emin"][:], in0=t["E"][:], scalar1=0.0), [t["E"]], [t["Emin"]])
            cx.ew("dve", lambda e: e.tensor_scalar_max(out=t["Emax"][:], in0=t["E"][:], scalar1=0.0), [t["E"]], [t["Emax"]])
            cx.actv(t["A"][:], t["Emin"][:], AF.Exp, [t["Emin"]], [t["A"]])
            cx.actv(t["B"][:], t["Emax"][:], AF.Exp, [t["Emax"]], [t["B"]], scale=-1.0)
            cx.actv(t["sca"][:, 0:2], t["gcs"][:, 0:2], AF.Exp, [t["gcs"]], [t["sca"]])
            cx.ew("dve", lambda e: e.tensor_tensor(out=t["sca"][:, 4:6], in0=t["gcs"][:, 2:4], in1=t["gcs"][:, 0:2], op=ALU.subtract),
                  [t["gcs"]], [t["sca"]])
            cx.actv(t["sca"][:, 4:6], t["sca"][:, 4:6], AF.Exp, [t["sca"]], [t["sca"]])
            cx.actv(t["sca"][:, 6:8], t["gcs"][:, 4:6], AF.Exp, [t["gcs"]], [t["sca"]])
            yield
            cx.ew("pool", lambda e: e.tensor_tensor(out=t["F2"][:], in0=t["A"][:], in1=ma.unsqueeze(1).to_broadcast([128, 2, 128]),
                                                    op=ALU.mult), [t["A"], mk], [t["F2"]])
            for hh in range(2):
                cx.ew("dve", lambda e, hh=hh: e.scalar_tensor_tensor(out=t["Fm"][:, hh, :], in0=t["B"][:, hh, :], scalar=b2[:, hh:hh + 1],
                                                                     in1=mm_, op0=ALU.mult, op1=ALU.mult), [t["B"], bg, mk], [t["Fm"]])
            cx.ew("dve", lambda e: e.tensor_tensor(out=t["sca"][:, 2:4], in0=t["sca"][:, 0:2], in1=b2, op=ALU.mult), [t["sca"], bg], [t["sca"]])
            cx.ew("pool", lambda e: e.tensor_tensor(out=t["vb"][:], in0=t["vtm"][:], in1=b2.unsqueeze(2).to_broadcast([128, 2, 64]),
                                                    op=ALU.mult), [t["vtm"], bg], [t["vb"]])
            cx.ew("pool", lambda e: e.tensor_tensor(out=t["kbg"][:], in0=t["ktm"][:],
                                                    in1=t["sca"][:, 2:4].unsqueeze(2).to_broadcast([128, 2, 64]), op=ALU.mult),
                  [t["ktm"], t["sca"]], [t["kbg"]])
            for hh in range(2):
                kd_ = t["kdp%d" % hh]
                cx.ew("dve", lambda e, hh=hh, kd_=kd_: e.tensor_scalar(out=kd_[:, hh * 64:(hh + 1) * 64], in0=t["ktm"][:, hh, :],
                                                                        scalar1=t["sca"][:, 4 + hh:5 + hh], scalar2=None, op0=ALU.mult),
                      [t["ktm"], t["sca"]], [kd_])
            cx.ew("pool", lambda e: e.tensor_tensor(out=t["qg"][:], in0=t["kq"][:, 128:256], in1=t["egr"][:], op=ALU.mult),
                  [t["kq"], t["egr"]], [t["qg"]])
            yield
            subs = []
            for hh in range(2):
                hs = slice(hh * 64, hh * 64 + 64)
                pG_ = bank()
                cx.mm(pG_[:, 0:256], t["kq"][hs, 0:128], t["kq"][hs, :], True, True, [t["kq"]], [pG_])
                subs.append(inverse(t, hh, pG_))
            live = list(subs)
            while live:
                for g_ in list(live):
                    try:
                        next(g_)
                    except StopIteration:
                        live.remove(g_)
                yield
            pU = bank()
            for hh in range(2):
                cx.mm(pU[:, hh * 64:(hh + 1) * 64], t["TT2"][:, hh, :], t["vb"][:, hh, :], True, True, [t["TT2"], t["vb"]], [pU])
            cx.mm(pU[:, 128:384], t["kbg"][:].rearrange("p h f -> p (h f)"), t["TT2"][:].rearrange("p h c -> p (h c)"), True, True,
                  [t["kbg"], t["TT2"]], [pU])
            cx.ew("act", lambda e: e.copy(out=t["u"][:], in_=pU[:, 0:128]), [pU], [t["u"]])
            cx.ew("dve", lambda e: e.tensor_copy(out=t["wT"][0:64, :], in_=pU[0:64, 128:256]), [pU], [t["wT"]])
            cx.ew("dve", lambda e: e.tensor_copy(out=t["wT"][64:128, :], in_=pU[64:128, 256:384]), [pU], [t["wT"]])
            yield
            for X_ in ((0, 1) if d == 0 else (1, 0)):
                rows = slice(X_ * 64, X_ * 64 + 64)
                pS1 = bank()
                cx.mm(pS1[:, 0:128], t["wT"][:], t["Sbf"][:], True, True, [t["wT"], t["Sbf"]], [pS1])
                cx.ew("dve", lambda e, rows=rows, pS1=pS1: e.tensor_tensor(out=t["vnew"][rows, :], in0=t["u"][rows, :], in1=pS1[rows, 0:128],
                                                                           op=ALU.subtract), [t["u"], pS1], [t["vnew"]])
                yield
                pS2 = bank()
                cx.mm(pS2[:, 0:128], t["qg"][:], t["Sbf"][:], True, False, [t["qg"], t["Sbf"]], [pS2])
                for hh in range(2):
                    cx.mm(pS2[:, hh * 64:(hh + 1) * 64], t["aT%d" % hh][:], t["vnew"][:, hh * 64:(hh + 1) * 64], False, hh == 1,
                          [t["aT%d" % hh], t["vnew"]], [pS2])
                cx.ew("act", lambda e, rows=rows, pS2=pS2: e.copy(out=t["obuf"][rows, :], in_=pS2[rows, 0:128]), [pS2], [t["obuf"]])
                pS3 = bank()
                for hh in range(2):
                    cx.mm(pS3[:, hh * 64:(hh + 1) * 64], t["kdp%d" % hh][rows, :], t["vnew"][rows, hh * 64:(hh + 1) * 64], True, True,
                          [t["kdp%d" % hh], t["vnew"]], [pS3])
                cx.ew("dve", lambda e, X_=X_, pS3=pS3: e.scalar_tensor_tensor(out=t["Sst"][:], in0=t["Sst"][:], scalar=t["sca"][:, 6 + X_:7 + X_],
                                                                              in1=pS3[:, 0:128], op0=ALU.mult, op1=ALU.add),
                      [t["Sst"], t["sca"], pS3], [t["Sst"]])
                cx.ew("act", lambda e: e.copy(out=t["Sbf"][:], in_=t["Sst"][:]), [t["Sst"]], [t["Sbf"]])
                yield
            cx.dma("sp", sc["OD"].ap()[d, t0:t0 + 128, jp * 128:jp * 128 + 128], t["obuf"][:], [t["obuf"]], [("OD", d, jp, i)])

        gens = {}
        nxt = {k: 0 for k in chains}
        live = True
        rnd = 0
        while live:
            live = False
            rnd += 1
            if rnd % 8 == 0 and getattr(C, "mw_gen", None) is not None:
                next(C.mw_gen, None)
            for (d, jp) in chains:
                g_ = gens.get((d, jp))
                if g_ is None:
                    tau = nxt[(d, jp)]
                    if tau >= NP:
                        continue
                    nxt[(d, jp)] = tau + 1
                    g_ = step(d, jp, tau if d == 0 else (S // 128 - 1 - tau))
                    gens[(d, jp)] = g_
                live = True
                try:
                    next(g_)
                except StopIteration:
                    gens[(d, jp)] = None
        cx.P.flush()


GROUPS = PAIRS


def collective_gather(cx, src, dst, r, w):
    if GROUPS is None:
        return
    cx.P.dma("pool", lambda e: e.collective_compute("AllGather", ALU.bypass, replica_groups=GROUPS,
                                                    ins=[src.ap().opt()], outs=[dst.ap().opt()]),
             _keys(r), _keys(w), grp="cc", inc=1)


def phase_dn_out(cx, C, din, hf):
    sc = cx.dram
    with ExitStack() as st:
        dnw = cx.sb(st, "dnw", [128, 64])
        cx.dma("sp", dnw[:], din["dnw_row"].ap().to_broadcast([128, 64]), [], [dnw])
        of = [cx.sb(st, "of%d" % i, [128, 4, 64]) for i in range(4)]
        ob = [cx.sb(st, "ob%d" % i, [128, 4, 64]) for i in range(4)]
        dzt = [cx.sb(st, "dzt%d" % i, [128, 4, 64]) for i in range(4)]
        sq = [cx.sb(st, "dsq%d" % i, [128, 4, 64]) for i in range(4)]
        ss = [cx.sb(st, "dss%d" % i, [128, 4]) for i in range(4)]
        yb = [cx.sb(st, "yb%d" % i, [128, 4, 64], BF16) for i in range(4)]
        dT = [cx.sb(st, "dT%d" % i, [128, 2, 512], BF16) for i in range(2)]
        tp = [cx.ps(st, "dtp%d" % i, [128, 512], BF16) for i in range(2)]
        tpr = [cx.ps(st, "dtpr%d" % i, [128, 512]) for i in range(2)]
        dR = [cx.sb(st, "dR%d" % i, [128, 2, 128], BF16) for i in range(4)]
        jmat = cx.sb(st, "jmat", [128, 128], BF16)
        jf = cx.sb(st, "jf", [128, 128])
        cx.ew("pool", lambda e: e.memset(jf[:], 1.0), [], [jf])
        cx.ew("pool", lambda e: e.affine_select(out=jf[:], in_=jf[:], pattern=[[1, 128]], compare_op=ALU.is_equal, fill=0.0, base=-127,
                                                channel_multiplier=1), [jf], [jf])
        cx.ew("dve", lambda e: e.tensor_copy(out=jmat[:], in_=jf[:]), [jf], [jmat])
        for i in range(S // 128):
            b = i % 4
            t0 = i * 128
            g = i // 4
            gb = g % 2
            cx.dma("sp", of[b][:], sc["OD"].ap()[0, t0:t0 + 128, :].rearrange("p (h f) -> p h f", f=64), [], [of[b]])
            cx.dma("sp", ob[b][:], sc["OD"].ap()[1, t0:t0 + 128, :].rearrange("p (h f) -> p h f", f=64), [], [ob[b]])
            cx.dma("sp", dzt[b][:], sc["DZS"].ap()[t0:t0 + 128, :].rearrange("p (h f) -> p h f", f=64), [], [dzt[b]])
            cx.ew("pool", lambda e, b=b: e.tensor_tensor(out=of[b][:], in0=of[b][:], in1=ob[b][:], op=ALU.add), [of[b], ob[b]], [of[b]])
            cx.ew("pool", lambda e, b=b: e.tensor_tensor(out=sq[b][:], in0=of[b][:], in1=of[b][:], op=ALU.mult), [of[b]], [sq[b]])
            cx.ew("dve", lambda e, b=b: e.reduce_sum(out=ss[b][:], in_=sq[b][:], axis=AX.X), [sq[b]], [ss[b]])
            cx.actv(ss[b][:], ss[b][:], AF.Sqrt, [ss[b], C.eps], [ss[b]], bias=C.eps[:], scale=1.0 / 64)
            cx.ew("dve", lambda e, b=b: e.reciprocal(out=ss[b][:], in_=ss[b][:]), [ss[b]], [ss[b]])
            cx.ew("pool", lambda e, b=b: e.tensor_tensor(out=dzt[b][:], in0=dzt[b][:], in1=dnw[:].unsqueeze(1).to_broadcast([128, 4, 64]),
                                                         op=ALU.mult), [dzt[b], dnw], [dzt[b]])
            cx.ew("dve", lambda e, b=b: e.tensor_tensor(out=of[b][:], in0=of[b][:], in1=ss[b][:].unsqueeze(2).to_broadcast([128, 4, 64]),
                                                        op=ALU.mult), [of[b], ss[b]], [of[b]])
            cx.ew("dve", lambda e, b=b: e.tensor_tensor(out=yb[b][:], in0=of[b][:], in1=dzt[b][:], op=ALU.mult), [of[b], dzt[b]], [yb[b]])
            j = i % 4
            for jp in range(2 if i < 32 else 0):
                cx.tr(tp[jp][:, j * 128:(j + 1) * 128], yb[b][:, jp * 2:jp * 2 + 2, :].rearrange("p h f -> p (h f)"), C.identb[:],
                      [yb[b], C.identb], [tp[jp]])
            if i < 32:
                if j == 3:
                    for jp in range(2):
                        cx.ew("act", lambda e, jp=jp, gb=gb: e.copy(out=dT[gb][:, jp, :], in_=tp[jp][:]), [tp[jp]], [dT[gb]])
                    cx.dma("sp", sc["DNT"].ap()[:, g * 512:(g + 1) * 512].rearrange("(jp p) t -> p jp t", p=128), dT[gb][:], [dT[gb]],
                           ["DNT"])
            else:
                for jp in range(2):
                    cx.mm(tpr[jp][:, 0:128], yb[b][:, jp * 2:jp * 2 + 2, :].rearrange("p h f -> p (h f)"), jmat[:], True, True,
                          [yb[b], jmat], [tpr[jp]])
                    cx.ew("act", lambda e, jp=jp, b=b: e.copy(out=dR[b][:, jp, :], in_=tpr[jp][:, 0:128]), [tpr[jp]], [dR[b]])
                r0 = (63 - i) * 128
                cx.dma("sp", sc["DNTR"].ap()[:, r0:r0 + 128].rearrange("(jp p) t -> p jp t", p=128), dR[b][:], [dR[b]], ["DNTR"])
        if GROUPS is not None:
            collective_gather(cx, sc["DNTR"], sc["DNGR"], ["DNTR"], ["DNGR"])
        cx.P.flush()


def merge_weights(cx, C, din, st):
    mw = {"wg": cx.sb(st, "wg", [128, 8, 2048], BF16), "wau": cx.sb(st, "wau", [128, 4, 1024], BF16),
          "wdu": cx.sb(st, "wdu", [128, 4, 1024], BF16), "wo": cx.sb(st, "wo", [128, 8, 1024], BF16),
          "wr": cx.sb(st, "wr", [128, 8, 16])}
    stg = [cx.sb(st, "mstg%d" % i, [128, 1024]) for i in range(2)]
    C.mw = mw

    def gen():
        cx.dma("sp", mw["wr"][:], din["w_router"].ap().rearrange("(k p) e -> p k e", p=128), [], [mw["wr"]])
        i = 0
        for (name, key, nk, ncols) in (("w_gates", "wg", 8, 2048), ("w_attn_up", "wau", 4, 1024), ("w_dn_up", "wdu", 4, 1024),
                                       ("w_o", "wo", 8, 1024)):
            src = din[name].ap().rearrange("(k p) f -> p k f", p=128)
            for k in range(nk):
                for c0 in range(0, ncols, 1024):
                    sg = stg[i % 2]
                    cx.dma("sp", sg[:], src[:, k, c0:c0 + 1024], [], [sg])
                    if i % 2:
                        cx.ew("act", lambda e, sg=sg, key=key, k=k, c0=c0: e.copy(out=mw[key][:, k, c0:c0 + 1024], in_=sg[:]), [sg], [mw[key]])
                    else:
                        cx.ew("dve", lambda e, sg=sg, key=key, k=k, c0=c0: e.tensor_copy(out=mw[key][:, k, c0:c0 + 1024], in_=sg[:]),
                              [sg], [mw[key]])
                    i += 1
                    yield
    C.mw_gen = gen()


def phase_merge(cx, C, din, hf, out_x):
    sc = cx.dram
    NC_ = 4096 // 512
    if "merge_small" in SKIP:
        NC_ = 1
    with ExitStack() as st:
        xa = [cx.sb(st, "mx%d" % i, [128, 1024]) for i in range(2)]
        wg, wau, wdu, wo, wr = (C.mw[k] for k in ("wg", "wau", "wdu", "wo", "wr"))
        for _ in C.mw_gen:
            pass
        hTc = [cx.sb(st, "mh%d" % i, [128, 8, 512], BF16) for i in range(2)]
        aTc = [cx.sb(st, "ma%d" % i, [128, 4, 512], BF16) for i in range(2)]
        dTc = [cx.sb(st, "md%d" % i, [128, 4, 512], BF16) for i in range(2)]
        dPr = [cx.sb(st, "mdp%d" % i, [128, 4, 512], BF16) for i in range(2)]
        sel = cx.sb(st, "sel", [128, 2])
        cx.dma("sp", sel[:], din["sel"].ap().to_broadcast([128, 2]), [], [sel])
        mT = [cx.sb(st, "mT%d" % i, [128, 8, 512], BF16) for i in range(2)]
        sga = [cx.sb(st, "sga%d" % i, [128, 512]) for i in range(2)]
        sgd = [cx.sb(st, "sgd%d" % i, [128, 512]) for i in range(2)]
        m1 = [cx.sb(st, "m1_%d" % i, [128, 512]) for i in range(2)]
        m2 = [cx.sb(st, "m2_%d" % i, [128, 512]) for i in range(2)]
        x1 = [cx.sb(st, "x1_%d" % i, [128, 1024]) for i in range(2)]
        tmpx = [cx.sb(st, "tmpx%d" % i, [128, 512]) for i in range(2)]
        ssq = [cx.sb(st, "mssq%d" % i, [128, 1]) for i in range(2)]
        rs = [cx.sb(st, "mrs%d" % i, [128, 1]) for i in range(2)]
        h2f = [cx.sb(st, "h2f%d" % i, [128, 1024]) for i in range(2)]
        h2x = [cx.sb(st, "h2x%d" % i, [128, 529]) for i in range(2)]
        h2T = [cx.sb(st, "h2T%d" % i, [128, 8, 128]) for i in range(2)]
        lg = [cx.sb(st, "lg%d" % i, [128, 16]) for i in range(2)]
        sm = [cx.sb(st, "sm%d" % i, [128, 4]) for i in range(2)]
        tid = cx.sb(st, "tid", [128, 32], I32)
        cx.ew("pool", lambda e: e.iota(tid[:], pattern=[[128, 32]], base=0, channel_multiplier=1), [], [tid])
        pg = [cx.ps(st, "pg%d" % i, [128, 512]) for i in range(2)]
        pa = cx.ps(st, "pa", [128, 512])
        pd = cx.ps(st, "pd", [128, 512])
        po = [cx.ps(st, "po%d" % i, [128, 512]) for i in range(2)]
        pt = cx.ps(st, "ptr", [128, 512])
        pl = cx.ps(st, "pl", [128, 512])
        def part2(u, li):
            for q4 in range(2):
                for k4 in range(4):
                    k = q4 * 4 + k4
                    cx.tr(pt[:, k4 * 128:(k4 + 1) * 128], h2f[u][:, k * 128:(k + 1) * 128], C.identf[:], [h2f[u], C.identf], [pt])
                cx.ew("act", lambda e, u=u, q4=q4: e.copy(out=h2T[u][:, q4 * 4:(q4 + 1) * 4, :].rearrange("p k t -> p (k t)"), in_=pt[:]),
                      [pt], [h2T[u]])
            for k in range(8):
                cx.mm(pl[:, 0:16], h2T[u][:, k, :], wr[:, k, :], k == 0, k == 7, [h2T[u], wr], [pl])
            cx.ew("dve", lambda e, u=u: e.reduce_max(out=sm[u][:, 0:1], in_=pl[:, 0:16], axis=AX.X), [pl], [sm[u]])
            cx.ew("dve", lambda e, u=u: e.tensor_scalar(out=sm[u][:, 1:2], in0=sm[u][:, 0:1], scalar1=-1.0, scalar2=None, op0=ALU.mult),
                  [sm[u]], [sm[u]])
            cx.actv(lg[u][:], pl[:, 0:16], AF.Exp, [pl, sm[u]], [lg[u], sm[u]], bias=sm[u][:, 1:2], scale=1.0, accum=sm[u][:, 2:3])
            cx.ew("dve", lambda e, u=u: e.reciprocal(out=sm[u][:, 3:4], in_=sm[u][:, 2:3]), [sm[u]], [sm[u]])
            cx.ew("dve", lambda e, u=u: e.tensor_scalar(out=h2x[u][:, 513:529], in0=lg[u][:], scalar1=sm[u][:, 3:4],
                                                        scalar2=None, op0=ALU.mult), [lg[u], sm[u]], [h2x[u]])
            cx.ew("pool", lambda e, u=u, li=li: e.tensor_copy(out=h2x[u][:, 512:513].bitcast(I32), in_=tid[:, li:li + 1]),
                  [tid], [h2x[u]])
            cx.dma("sp", sc["H2X"].ap()[li * 128:(li + 1) * 128, :], h2x[u][:], [h2x[u]], [("H2X", li)])
            cx.dma("sp", sc["AFF"].ap()[li * 128:(li + 1) * 128, :], h2x[u][:, 513:529], [h2x[u]], ["AFF"])

        xsrc = din["x"].ap()
        npo = 0
        pend2 = None
        for c in range(NC_):
            b = c % 2
            l0 = c * 512
            g0 = l0
            cx.dma("sp", hTc[b][:], sc["HT"].ap()[:, :, g0:g0 + 512], [], [hTc[b]])
            cx.dma("sp", aTc[b][:], sc["ATT"].ap()[:, l0:l0 + 512].rearrange("(k p) t -> p k t", p=128), [], [aTc[b]])
            cx.dma("sp", dTc[b][:, 0:2, :], sc["DNT"].ap()[:, l0:l0 + 512].rearrange("(k p) t -> p k t", p=128), ["DNT"], [dTc[b]])
            cx.dma("sp", dPr[b][:], sc["DNGR"].ap()[:, l0:l0 + 512].rearrange("(k p) t -> p k t", p=128), ["DNGR"], [dPr[b]])
            cx.ew("pool", lambda e, b=b: e.tensor_scalar(out=dPr[b][:, 0:2, :], in0=dPr[b][:, 0:2, :], scalar1=sel[:, 0:1], scalar2=None,
                                                         op0=ALU.mult), [dPr[b], sel], [dPr[b]])
            cx.ew("dve", lambda e, b=b: e.scalar_tensor_tensor(out=dTc[b][:, 2:4, :], in0=dPr[b][:, 2:4, :], scalar=sel[:, 1:2],
                                                                in1=dPr[b][:, 0:2, :], op0=ALU.mult, op1=ALU.add), [dPr[b], sel], [dTc[b]])
            for fo in range(8):
                s = fo % 2
                for (gi, dst) in ((0, sga[s]), (1, sgd[s])):
                    p = pg[gi]
                    for k in range(8):
                        cx.mm(p[:], wg[:, k, gi * 1024 + fo * 128:gi * 1024 + (fo + 1) * 128], hTc[b][:, k, :], k == 0, k == 7,
                              [wg, hTc[b]], [p])
                    cx.actv(dst[:], p[:], AF.Sigmoid, [p], [dst])
                for k in range(4):
                    cx.mm(pa[:], wau[:, k, fo * 128:(fo + 1) * 128], aTc[b][:, k, :], k == 0, k == 3, [wau, aTc[b]], [pa])
                for k in range(4):
                    cx.mm(pd[:], wdu[:, k, fo * 128:(fo + 1) * 128], dTc[b][:, k, :], k == 0, k == 3, [wdu, dTc[b]], [pd])
                cx.ew("dve", lambda e, s=s: e.tensor_tensor(out=m1[s][:], in0=pa[:], in1=sga[s][:], op=ALU.mult), [pa, sga[s]], [m1[s]])
                cx.ew("dve", lambda e, s=s: e.tensor_tensor(out=m2[s][:], in0=pd[:], in1=sgd[s][:], op=ALU.mult), [pd, sgd[s]], [m2[s]])
                cx.ew("pool", lambda e, s=s, b=b, fo=fo: e.tensor_tensor(out=mT[b][:, fo, :], in0=m1[s][:], in1=m2[s][:], op=ALU.add),
                      [m1[s], m2[s]], [mT[b]])
            for j in range(4):
                u = j % 2
                li = c * 4 + j
                cx.dma("sp", xa[u][:], xsrc[g0 + j * 128:g0 + (j + 1) * 128, :], [], [xa[u]])
                for n in range(2):
                    p = po[npo % 2]
                    npo += 1
                    for k in range(8):
                        cx.mm(p[:], mT[b][:, k, j * 128:(j + 1) * 128], wo[:, k, n * 512:(n + 1) * 512], k == 0, k == 7, [mT[b], wo], [p])
                    cx.ew("dve", lambda e, p=p, n=n, u=u: e.tensor_tensor(out=tmpx[n][:], in0=p[:], in1=C.gtrow[:, n * 512:(n + 1) * 512],
                                                                          op=ALU.mult), [p, C.gtrow], [tmpx[n]])
                    cx.ew("pool", lambda e, n=n, u=u: e.tensor_tensor(out=x1[u][:, n * 512:(n + 1) * 512], in0=tmpx[n][:],
                                                                      in1=xa[u][:, n * 512:(n + 1) * 512], op=ALU.add),
                          [tmpx[n], xa[u]], [x1[u]])
                cx.dma("sp", sc["ACC"].ap()[li * 128:(li + 1) * 128, :], x1[u][:], [x1[u]], [("OUTX", li)])
                cx.actv(h2f[u][:], x1[u][:], AF.Square, [x1[u]], [h2f[u], ssq[u]], accum=ssq[u][:])
                cx.actv(rs[u][:], ssq[u][:], AF.Sqrt, [ssq[u], C.eps], [rs[u]], bias=C.eps[:], scale=1.0 / D)
                cx.ew("dve", lambda e, u=u: e.reciprocal(out=rs[u][:], in_=rs[u][:]), [rs[u]], [rs[u]])
                cx.ew("dve", lambda e, u=u: e.scalar_tensor_tensor(out=h2f[u][:], in0=x1[u][:], scalar=rs[u][:, 0:1], in1=C.gtrow[:, 3072:4096],
                                                                   op0=ALU.mult, op1=ALU.mult), [x1[u], rs[u], C.gtrow], [h2f[u]])
                cx.ew("pool", lambda e, u=u: e.tensor_tensor(out=h2f[u][:], in0=h2f[u][:], in1=C.gtrow[:, 2048:3072], op=ALU.add),
                      [h2f[u], C.gtrow], [h2f[u]])
                cx.ew("act", lambda e, u=u: e.copy(out=h2x[u][:, 0:512].bitcast(BF16), in_=h2f[u][:]), [h2f[u]], [h2x[u]])
                if pend2 is not None:
                    pend2()
                pend2 = (lambda u=u, li=li: part2(u, li))
            if pend2 is not None:
                pend2()
                pend2 = None
        if "merge_small" in SKIP:
            pass
        elif GROUPS is None:
            cx.dma("sp", sc["AFFG"].ap()[0:4096, :], sc["AFF"].ap(), ["AFF"], ["AFFG"])
        else:
            collective_gather(cx, sc["AFF"], sc["AFFG"], ["AFF"], ["AFFG"])
        cx.P.flush()


BIGI = 1 << 20


def moe_prefill(cx, C):
    sc = cx.dram
    zrow = cx.sb(cx.stack, "zrow", [128, 529])
    cx.ew("pool", lambda e: e.memset(zrow[:], 0.0), [], [zrow])
    cx.ew("pool", lambda e: e.iota(zrow[:, 512:513].bitcast(I32), pattern=[[0, 1]], base=4096, channel_multiplier=1), [zrow], [zrow])
    for e_ in range(16):
        cx.dma("sp", sc["XE%d" % e_].ap().rearrange("(n p) f -> p n f", p=128), zrow[:].unsqueeze(1).to_broadcast([128, 9, 529]),
               [zrow], [("XE", e_)])


def phase_moe(cx, C, din, out_x):
    sc = cx.dram
    NE = 16 if "moe_small" not in SKIP else 2
    NIT = 32
    with ExitStack() as st:
        ones = cx.sb(st, "ones_moe", [128, 128])
        slt = cx.sb(st, "slt", [128, 128])
        cx.ew("pool", lambda e: e.memset(ones[:], 1.0), [], [ones])
        cx.ew("pool", lambda e: e.memset(slt[:], 1.0), [], [slt])
        cx.ew("pool", lambda e: e.affine_select(out=slt[:], in_=slt[:], pattern=[[1, 128]], compare_op=ALU.is_gt, fill=0.0, base=0,
                                                channel_multiplier=-1), [slt], [slt])
        desti = cx.sb(st, "desti", [128, 32, 16], I32)
        wsrc = {"g": din["w_gate"], "u": din["w_up"], "d": din["w_down"]}
        wt = {k: [cx.sb(st, "w%s%d" % (k, i), [128, 8, 1024], BF16) for i in range(2)] for k in "gud"}
        stg = [cx.sb(st, "estg%d" % i, [128, 1, 1024]) for i in range(3)]
        nstc = [0]

        hbuf = [cx.sb(st, "hbuf%d" % i, [128, 529]) for i in range(4)]
        nhb = [0]

        def scatter(e_, i):
            hb = hbuf[nhb[0] % 4]
            nhb[0] += 1
            cx.dma("sp", hb[:], sc["H2X"].ap()[i * 128:(i + 1) * 128, :], [], [hb])
            cx.P.dma("pool", lambda e, i=i, e_=e_, hb=hb: e.indirect_dma_start(
                out=sc["XE%d" % e_].ap(), out_offset=bass.IndirectOffsetOnAxis(ap=desti[:, i, e_:e_ + 1], axis=0),
                in_=hb[:], in_offset=None),
                _keys([desti, hb]), _keys([("XE", e_)]))

        def load_w_gen(e_):
            b = e_ % 2
            pend = None
            for k_ in "gud":
                src = wsrc[k_].ap()[e_].rearrange("(k p) f -> p k f", p=128)
                for c4 in range(8):
                    nst = nstc[0]
                    nstc[0] += 1
                    sg = stg[nst % 3]
                    cx.dma("sp", sg[:], src[:, c4:c4 + 1, :], [], [sg])
                    if pend is not None:
                        pend()

                    def cast(sg=sg, k_=k_, c4=c4, nst=nst):
                        if nst % 2:
                            cx.ew("act", lambda e: e.copy(out=wt[k_][b][:, c4:c4 + 1, :], in_=sg[:]), [sg], [wt[k_][b]])
                        else:
                            cx.ew("dve", lambda e: e.tensor_copy(out=wt[k_][b][:, c4:c4 + 1, :], in_=sg[:]), [sg], [wt[k_][b]])
                    pend = cast
                    yield
            pend()

        for _ in load_w_gen(0):
            pass
        pdum = cx.sb(st, "pdum", [128, 1])
        pdi = cx.sb(st, "pdi", [128, 1], I32)
        cx.ew("pool", lambda e: e.iota(pdi[:], pattern=[[0, 1]], base=1024, channel_multiplier=1), [], [pdi])
        cx.ew("dve", lambda e: e.tensor_copy(out=pdum[:], in_=pdi[:]), [pdi], [pdum])
        with ExitStack() as st2:
            affg = cx.sb(st2, "affg", [128, 64, 16])
            cmp_ = cx.sb(st2, "cmp", [128, 64, 16])
            affo = cx.sb(st2, "affo", [128, 32, 16])
            msk = cx.sb(st2, "msk", [128, 32, 16])
            pos = TL(None, cmp_.k, view=cmp_[:, 0:32, :])
            csum = TL(None, cmp_.k, view=cmp_[:, 32:64, :])
            bef = cx.sb(st2, "bef", [128, 32, 16])
            eoff = cx.sb(st2, "eoff", [128, 16])
            lo = cx.sb(st2, "lo", [128, 16])
            hi = cx.sb(st2, "hi", [128, 16])
            mid = cx.sb(st2, "mid", [128, 16])
            cnt = cx.sb(st2, "cnt", [128, 16])
            ge = cx.sb(st2, "ge", [128, 16])
            d1 = cx.sb(st2, "d1", [128, 16])
            d2 = cx.sb(st2, "d2", [128, 16])
            ptot = cx.ps(st2, "ptot", [128, 512])
            pwi = cx.ps(st2, "pwi", [128, 512])
            pcs = cx.ps(st2, "pcs", [128, 512])
            cx.dma("sp", affg[:], sc["AFFG"].ap().rearrange("(p j) e -> p j e", p=128), ["AFFG"], [affg])
            cx.dma("sp", affo[:], sc["AFF"].ap().rearrange("(i p) e -> p i e", p=128), ["AFF"], [affo])
            cx.dma("sp", eoff[:], din["eoff"].ap().to_broadcast([128, 16]), [], [eoff])
            cx.ew("dve", lambda e: e.memset(lo[:], 0.0), [], [lo])
            for it in range(NIT):
                cj = 2.0 ** -(it + 1)
                cx.ew("dve", lambda e, cj=cj: e.tensor_scalar(out=mid[:], in0=lo[:], scalar1=cj, scalar2=None, op0=ALU.add), [lo], [mid])
                cx.ew("dve", lambda e: e.tensor_tensor(out=cmp_[:], in0=affg[:], in1=mid[:].unsqueeze(1).to_broadcast([128, 64, 16]),
                                                       op=ALU.is_gt), [affg, mid], [cmp_])
                cx.ew("dve", lambda e: e.reduce_sum(out=cnt[:], in_=cmp_[:].rearrange("p j e -> p e j"), axis=AX.X), [cmp_], [cnt])
                cx.mm(ptot[:, 0:16], ones[:], cnt[:], True, True, [ones, cnt], [ptot])
                cx.ew("dve", lambda e, cj=cj: e.tensor_scalar(out=ge[:], in0=ptot[:, 0:16], scalar1=1024.0, scalar2=cj, op0=ALU.is_ge,
                                                              op1=ALU.mult), [ptot], [ge])
                cx.ew("dve", lambda e: e.tensor_tensor(out=lo[:], in0=lo[:], in1=ge[:], op=ALU.add), [lo, ge], [lo])
            cx.ew("dve", lambda e: e.tensor_tensor(out=msk[:], in0=affo[:], in1=lo[:].unsqueeze(1).to_broadcast([128, 32, 16]), op=ALU.is_gt),
                  [affo, lo], [msk])
            cx.mm(pwi[:], slt[:], msk[:].rearrange("p i e -> p (i e)"), True, True, [slt, msk], [pwi])
            cx.mm(pcs[:], ones[:], msk[:].rearrange("p i e -> p (i e)"), True, True, [ones, msk], [pcs])
            cx.ew("dve", lambda e: e.tensor_copy(out=csum[:].rearrange("p i e -> p (i e)"), in_=pcs[:]), [pcs], [csum])
            cx.ew("dve", lambda e: e.memset(bef[:, 0, :], 0.0), [], [bef])
            for i in range(1, 32):
                cx.ew("dve", lambda e, i=i: e.tensor_tensor(out=bef[:, i, :], in0=bef[:, i - 1, :], in1=csum[:, i - 1, :], op=ALU.add),
                      [bef, csum], [bef])
            cx.ew("dve", lambda e: e.tensor_tensor(out=pos[:].rearrange("p i e -> p (i e)"), in0=pwi[:],
                                                   in1=bef[:].rearrange("p i e -> p (i e)"), op=ALU.add), [pwi, bef], [pos])
            cx.ew("dve", lambda e: e.tensor_scalar(out=csum[:], in0=pos[:], scalar1=1024.0, scalar2=None, op0=ALU.is_lt), [pos], [csum])
            cx.ew("dve", lambda e: e.tensor_tensor(out=msk[:], in0=msk[:], in1=csum[:], op=ALU.mult), [msk, csum], [msk])
            cx.ew("dve", lambda e: e.tensor_scalar(out=pos[:], in0=pos[:], scalar1=pdum[:, 0:1], scalar2=None, op0=ALU.subtract), [pos, pdum], [pos])
            cx.ew("dve", lambda e: e.tensor_tensor(out=pos[:], in0=pos[:], in1=msk[:], op=ALU.mult), [pos, msk], [pos])
            cx.ew("dve", lambda e: e.tensor_scalar(out=pos[:], in0=pos[:], scalar1=pdum[:, 0:1], scalar2=None, op0=ALU.add), [pos, pdum], [pos])
            cx.ew("dve", lambda e: e.tensor_copy(out=desti[:], in_=pos[:]), [pos], [desti])
            if cx.debug:
                cx.dma("sp", sc["DBGM"].ap()[:, 0:16], lo[:], [lo], ["dbgm1"])
                cx.dma("sp", sc["DBGD"].ap().rearrange("(i p) e -> p i e", p=128), desti[:], [desti], ["dbgm2"])
            cx.P.flush()
        xeh = cx.sb(st, "xeh", [128, 8, 512])
        xem = [cx.sb(st, "xem%d" % i, [128, 8, 17]) for i in range(2)]
        xeT = cx.sb(st, "xeT", [128, 8, 1024], BF16)
        aT = cx.sb(st, "aTe", [128, 8, 1024], BF16)
        act_ = [cx.sb(st, "eact%d" % i, [128, 512]) for i in range(2)]
        yg = [cx.sb(st, "yg%d" % i, [128, 1024]) for i in range(2)]
        ptr = [cx.ps(st, "eptr%d" % i, [128, 1024], BF16) for i in range(2)]
        pg = [cx.ps(st, "epg%d" % i, [128, 512]) for i in range(2)]
        pu = [cx.ps(st, "epu%d" % i, [128, 512]) for i in range(2)]
        py = [cx.ps(st, "epy%d" % i, [128, 512]) for i in range(2)]
        nst = 0
        n1 = 0
        n2 = 0
        for i in range(32):
            scatter(0, i)
        for e_ in range(NE):
            b = e_ % 2
            nxtw = load_w_gen(e_ + 1) if e_ + 1 < NE else iter(())
            nxts = iter([(e_ + 1, i) for i in range(32)] if e_ + 1 < NE else [])
            xsrc_ = sc["XE%d" % e_].ap()[0:1024, :].rearrange("(s p) f -> p s f", p=128)
            cx.dma("sp", xeh[:], xsrc_[:, :, 0:512], [("XE", e_)], [xeh])
            cx.dma("sp", xem[b][:], xsrc_[:, :, 512:529], [("XE", e_)], [xem[b]])
            for s_ in range(8):
                pt_ = ptr[s_ % 2]
                xb_ = xeh[:, s_, :].bitcast(BF16)
                for k in range(8):
                    cx.tr(pt_[:, k * 128:(k + 1) * 128], xb_[:, k * 128:(k + 1) * 128], C.identb[:], [xeh, C.identb], [pt_])
                cx.ew("act" if s_ % 2 else "dve", lambda e, pt_=pt_, s_=s_: (e.copy if hasattr(e, "copy") else e.tensor_copy)(
                    out=xeT[:, :, s_ * 128:(s_ + 1) * 128], in_=pt_[:].rearrange("p (k t) -> p k t", k=8)), [pt_], [xeT])
            for fo in range(8):
                for hf_ in range(2):
                    g_, u_ = pg[n1 % 2], pu[n1 % 2]
                    a_ = act_[n1 % 2]
                    n1 += 1
                    cs = slice(hf_ * 512, (hf_ + 1) * 512)
                    for k in range(8):
                        cx.mm(g_[:], wt["g"][b][:, k, fo * 128:(fo + 1) * 128], xeT[:, k, cs], k == 0, k == 7, [wt["g"][b], xeT], [g_])
                    for k in range(8):
                        cx.mm(u_[:], wt["u"][b][:, k, fo * 128:(fo + 1) * 128], xeT[:, k, cs], k == 0, k == 7, [wt["u"][b], xeT], [u_])
                    cx.actv(a_[:], g_[:], AF.Silu, [g_], [a_])
                    cx.ew("dve", lambda e, a_=a_, u_=u_, fo=fo, cs=cs: e.tensor_tensor(out=aT[:, fo, cs], in0=u_[:], in1=a_[:], op=ALU.mult),
                          [u_, a_], [aT])
                    next(nxtw, None)
                    for _ in range(2):
                        sx = next(nxts, None)
                        if sx is not None:
                            scatter(*sx)
            for s_ in range(8):
                y_ = yg[s_ % 2]
                for hf_ in range(2):
                    p_ = py[n2 % 2]
                    n2 += 1
                    for fo in range(8):
                        cx.mm(p_[:], aT[:, fo, s_ * 128:(s_ + 1) * 128], wt["d"][b][:, fo, hf_ * 512:(hf_ + 1) * 512], fo == 0, fo == 7,
                              [aT, wt["d"][b]], [p_])
                    cx.ew("dve", lambda e, p_=p_, y_=y_, hf_=hf_, s_=s_, b=b, e_=e_: e.scalar_tensor_tensor(
                        out=y_[:, hf_ * 512:(hf_ + 1) * 512], in0=p_[:], scalar=xem[b][:, s_, 1 + e_:2 + e_],
                        in1=C.gtrow[:, 1024 + hf_ * 512:1024 + (hf_ + 1) * 512], op0=ALU.mult, op1=ALU.mult),
                        [p_, xem[b], C.gtrow], [y_])
                cx.P.dma("pool", lambda e, y_=y_, s_=s_, b=b: e.indirect_dma_start(
                    out=sc["ACC"].ap(), out_offset=bass.IndirectOffsetOnAxis(ap=xem[b][:, s_, 0:1].bitcast(I32), axis=0),
                    in_=y_[:], in_offset=None, compute_op=ALU.add),
                    _keys([y_, xem[b]]), _keys(["OUTACC"]))
                next(nxtw, None)
            for _ in nxtw:
                pass
        cx.P.flush()


_CACHE = {}


def kernel(**inputs):
    inp = {k: np.asarray(v) for k, v in inputs.items()}
    if "nc" not in _CACHE:
        _CACHE["nc"] = build()
        _CACHE["tabs"] = const_tables()
    nc = _CACHE["nc"]
    tabs = _CACHE["tabs"]
    in_maps = []
    for core in range(8):
        ci = core_inputs(inp, core, tabs)
        in_maps.append({k: np.ascontiguousarray(ci[k], dtype=np.float32) for k in INPUT_SHAPES})
    res = run_bass_kernel_spmd(nc, in_maps, core_ids=list(range(8)))
    out = np.empty((4, S, D), np.float32)
    for core in range(8):
        b, hf = core // 2, core % 2
        o = np.asarray(res.results[core]["out_x"], dtype=np.float32)[:4096]
        if hf == 0:
            out[b, :4096] = o
        else:
            out[b, 4096:] = o[::-1]
    return out
```

```python
import numpy as np
import concourse.bass as bass
import concourse.mybir as mybir
from concourse.bass_utils import run_bass_kernel_spmd

F32 = mybir.dt.float32
BF16 = mybir.dt.bfloat16
I32 = mybir.dt.int32
AF = mybir.ActivationFunctionType
ALU = mybir.AluOpType
AX = mybir.AxisListType

ENGS = ("pe", "act", "dve", "pool", "sp")
NDSEM = 8


class Prog:
    def __init__(self, nc, stack):
        self.nc = nc
        self.ops = {e: [] for e in ENGS}
        self.res = {}
        self.waited = {e: {} for e in ENGS}
        self.esem = {e: stack.enter_context(nc.semaphore("es_" + e)) for e in ENGS}
        self.dsem = {}
        self.dcnt = {}
        self.dnext = {}
        for q in ("sp", "pool", "act", "cc"):
            self.dsem[q] = [stack.enter_context(nc.semaphore("ds_%s%d" % (q, i))) for i in range(NDSEM)]
            self.dcnt[q] = [0] * NDSEM
            self.dnext[q] = 0

    def _deps(self, eng, reads, writes):
        deps = []
        for k in reads:
            st = self.res.get(k)
            if st is not None and st["w"] is not None:
                deps.append(st["w"])
        for k in writes:
            st = self.res.get(k)
            if st is not None:
                if st["w"] is not None:
                    deps.append(st["w"])
                deps.extend(st["r"])
        out = []
        wd = self.waited[eng]
        for d in deps:
            if d[0] == "e":
                _, src, idx = d
                if src == "pe" and eng == "pe":
                    continue
                key = ("e", src)
                if wd.get(key, -1) >= idx:
                    continue
                wd[key] = idx
                tgt = self.ops[src][idx]
                assert tgt["sig"] or not tgt.get("frozen"), "dependency on an already emitted, unsignalled op"
                tgt["sig"] = True
                out.append(d)
            else:
                _, q, slot, val = d
                key = ("d", q, slot)
                if wd.get(key, -1) >= val:
                    continue
                wd[key] = val
                out.append(d)
        return out

    def _record(self, dep, reads, writes):
        for k in writes:
            self.res[k] = {"w": dep, "r": []}
        for k in reads:
            st = self.res.setdefault(k, {"w": None, "r": []})
            st["r"].append(dep)
            if len(st["r"]) > 12:
                last = {}
                for d in st["r"]:
                    last[d[:2] if d[0] == "e" else d[:3]] = d
                st["r"] = list(last.values())

    def op(self, eng, fn, r=(), w=()):
        waits = self._deps(eng, r, w)
        idx = len(self.ops[eng])
        self.ops[eng].append({"fn": fn, "waits": waits, "sig": False, "dma": None})
        self._record(("e", eng, idx), r, w)
        return idx

    def pe(self, fn, r=(), w=()):
        return self.op("pe", fn, r, w)

    def act(self, fn, r=(), w=()):
        return self.op("act", fn, r, w)

    def dve(self, fn, r=(), w=()):
        return self.op("dve", fn, r, w)

    def pool(self, fn, r=(), w=()):
        return self.op("pool", fn, r, w)

    def dma(self, q, fn, r=(), w=(), grp=None, inc=16):
        grp = grp or q
        waits = self._deps(q, r, w)
        slot = self.dnext[grp]
        self.dnext[grp] = (slot + 1) % NDSEM
        prev = self.dcnt[grp][slot]
        wd = self.waited[q]
        if prev > 0 and wd.get(("d", grp, slot), -1) < prev:
            wd[("d", grp, slot)] = prev
            waits.append(("d", grp, slot, prev))
        val = prev + inc
        self.dcnt[grp][slot] = val
        idx = len(self.ops[q])
        self.ops[q].append({"fn": fn, "waits": waits, "sig": False, "dma": (grp, slot, inc)})
        self._record(("d", grp, slot, val), r, w)
        return idx

    def barrier(self):
        allk = "__all__"
        last = []
        for e in ENGS:
            if self.ops[e] and not self.ops[e][-1].get("frozen") and self.ops[e][-1]["fn"] is not None:
                last.append(("e", e, len(self.ops[e]) - 1))
        for q in self.dsem:
            for s in range(NDSEM):
                if self.dcnt[q][s] > 0:
                    last.append(("d", q, s, self.dcnt[q][s]))
        self.res[allk] = {"w": None, "r": last}
        for e in ENGS:
            self.op(e, None, r=(), w=(allk,))
            self.res[allk] = {"w": None, "r": last}
        del self.res[allk]

    def flush(self):
        nc = self.nc
        self.barrier()
        if not hasattr(self, "emitted"):
            self.emitted = {e: 0 for e in ENGS}
            self.sigbase = {e: 0 for e in ENGS}
        for e in ENGS:
            c = self.sigbase[e]
            for o in self.ops[e][self.emitted[e]:]:
                if o["sig"] and o["dma"] is None:
                    c += 1
                o["sigval"] = c
            self.sigbase[e] = c

        def run(e, name):
            for o in self.ops[name][self.emitted[name]:]:
                for d in o["waits"]:
                    if d[0] == "e":
                        tgt = self.ops[d[1]][d[2]]
                        assert tgt["sig"] and "sigval" in tgt
                        e.wait_ge(self.esem[d[1]], tgt["sigval"])
                    else:
                        e.wait_ge(self.dsem[d[1]][d[2]], d[3])
                if o["fn"] is None:
                    assert not o["sig"]
                    continue
                ins = o["fn"](e)
                if o["dma"] is not None:
                    q, slot, inc = o["dma"]
                    ins.then_inc(self.dsem[q][slot], inc)
                elif o["sig"]:
                    ins.then_inc(self.esem[name], 1)
                o["fn"] = None
            self.emitted[name] = len(self.ops[name])

        with nc.Block() as block:
            @block.sync
            def _(e):
                run(e, "sp")

            @block.scalar
            def _(e):
                run(e, "act")

            @block.vector
            def _(e):
                run(e, "dve")

            @block.gpsimd
            def _(e):
                run(e, "pool")

            @block.tensor
            def _(e):
                run(e, "pe")
        for e in ENGS:
            for o in self.ops[e]:
                o["frozen"] = True

    def emit(self):
        self.flush()


S = 8192
D = 1024
NCH = S // 512
EPS = 1e-6
PAIRS = [[0, 1], [2, 3], [4, 5], [6, 7]]
SKIP = set()
ATT_LAG = 2
ATT_NS = 3
ATT_NP = 4


class TL:
    def __init__(self, t, k, view=None, psum=False):
        self.t = t if view is None else view
        self.k = k
        self.psum = psum

    def __getitem__(self, idx):
        return self.t[idx]


def _keys(xs):
    return [getattr(x, "k", x) for x in xs]


def _rw(r, w):
    r2 = [x for x in r if not getattr(x, "psum", False)]
    w2 = list(w) + [x for x in r if getattr(x, "psum", False)]
    return _keys(r2), _keys(w2)


class Ctx:
    def __init__(self, nc, P, stack, debug):
        self.nc = nc
        self.P = P
        self.stack = stack
        self.debug = debug
        self.dram = {}
        self.n = 0

    def sb(self, stack, name, shape, dt=F32):
        self.n += 1
        nm = "%s_%d" % (name, self.n)
        return TL(stack.enter_context(self.nc.sbuf_tensor(nm, list(shape), dt)), nm)

    def ps(self, stack, name, shape, dt=F32):
        self.n += 1
        nm = "%s_%d" % (name, self.n)
        full = 512 if dt == F32 else 1024
        t = stack.enter_context(self.nc.psum_tensor(nm, [128, full], dt))
        assert len(shape) == 2 and shape[1] <= full
        return TL(None, nm, view=t[0:shape[0], 0:shape[1]], psum=True)

    def inp(self, name, shape, dt=F32):
        self.dram[name] = self.nc.dram_tensor(name, list(shape), dt, kind="ExternalInput")
        return self.dram[name]

    def scratch(self, name, shape, dt=F32, dbg=False):
        kind = "ExternalOutput" if (dbg and self.debug) else "Internal"
        self.dram[name] = self.nc.dram_tensor(name, list(shape), dt, kind=kind)
        return self.dram[name]

    def mm(self, out, lhsT, rhs, start, stop, r, w):
        self.P.pe(lambda e: e.matmul(out, lhsT=lhsT, rhs=rhs, start=start, stop=stop), *_rw(r, w))

    def tr(self, out, in_, ident, r, w):
        self.P.pe(lambda e: e.transpose(out=out, in_=in_, identity=ident), *_rw(r, w))

    def actv(self, out, in_, func, r, w, bias=None, scale=None, accum=None):
        kw = {}
        if bias is not None:
            kw["bias"] = bias
        if scale is not None:
            kw["scale"] = scale
        if accum is not None:
            kw["accum_out"] = accum
        self.P.act(lambda e: e.activation(out=out, in_=in_, func=func, **kw), *_rw(r, w))

    def ew(self, eng, fn, r, w):
        self.P.op(eng, fn, *_rw(r, w))

    def dma(self, q, out, in_, r, w):
        self.P.dma(q, lambda e: e.dma_start(out=out, in_=in_), *_rw(r, w))


def load_w_bf16(cx, st, wd, ncols, dst, stage, key_prefix, engs=("dve", "pool")):
    src = wd.ap().rearrange("(k p) f -> p k f", p=128)
    i = 0
    for c0 in range(0, ncols, 512):
        c1 = min(ncols, c0 + 512)
        sg = stage[i % 2]
        cx.dma("sp", sg[:, :, 0:c1 - c0], src[:, :, c0:c1], r=[], w=[sg])
        eng = engs[i % len(engs)]
        cx.ew(eng, lambda e, sg=sg, c0=c0, c1=c1: e.tensor_copy(out=dst[:, :, c0:c1], in_=sg[:, :, 0:c1 - c0]),
              r=[sg], w=[dst])
        i += 1


from contextlib import ExitStack


class Consts:
    pass


def setup_consts(cx, C, din):
    st = cx.stack
    C.identf = cx.sb(st, "identf", [128, 128])
    C.identb = cx.sb(st, "identb", [128, 128], BF16)
    cx.ew("pool", lambda e: e.memset(C.identf[:], 1.0), [], [C.identf])
    cx.ew("pool", lambda e: e.affine_select(out=C.identf[:], in_=C.identf[:], pattern=[[-1, 128]],
                                            compare_op=ALU.is_equal, fill=0.0, base=0, channel_multiplier=1),
          [C.identf], [C.identf])
    cx.ew("dve", lambda e: e.tensor_copy(out=C.identb[:], in_=C.identf[:]), [C.identf], [C.identb])
    C.eps = cx.sb(st, "epsc", [128, 1])
    cx.ew("pool", lambda e: e.memset(C.eps[:], EPS), [], [C.eps])
    C.modF = cx.sb(st, "modF", [128, 48])
    C.gtrow = cx.sb(st, "gtrow", [128, 4096])
    C.sc1F = cx.sb(st, "sc1F", [128, 8])
    C.sc2F = cx.sb(st, "sc2F", [128, 8])


def phase_adaln(cx, C, din):
    with ExitStack() as st:
        cT = cx.sb(st, "cT", [128, 8])
        sc = cx.sb(st, "sc", [128, 8])
        screp = cx.sb(st, "screp", [128, 8, 128])
        wst = [cx.sb(st, "wst%d" % i, [128, 8, 512]) for i in range(2)]
        modp = cx.ps(st, "modp", [128, 48])
        rowp = [cx.ps(st, "rowp%d" % i, [128, 512]) for i in range(2)]
        badaF = cx.sb(st, "badaF", [128, 48])
        bgt = cx.sb(st, "bgt", [128, 4096])
        n2row = cx.sb(st, "n2row", [128, 1024])
        n1 = cx.sb(st, "n1", [128, 8])
        n2 = cx.sb(st, "n2", [128, 8])
        tmp = cx.sb(st, "tmpa", [128, 8])
        cx.dma("sp", cT[:], din["cT"].ap(), [], [cT])
        cx.dma("sp", badaF[:], din["b_adaF"].ap(), [], [badaF])
        cx.dma("sp", bgt[:], din["b_gt"].ap().to_broadcast([128, 4096]), [], [bgt])
        cx.dma("sp", n2row[:], din["norm2R"].ap().to_broadcast([128, 1024]), [], [n2row])
        cx.dma("sp", n1[:], din["norm1F"].ap(), [], [n1])
        cx.dma("sp", n2[:], din["norm2F"].ap(), [], [n2])
        cx.actv(sc[:], cT[:], AF.Silu, [cT], [sc])
        for k in range(8):
            cx.ew("dve", lambda e, k=k: e.tensor_copy(out=screp[:, k, :], in_=sc[:, k:k + 1].to_broadcast([128, 128])),
                  [sc], [screp])
        wsrc = din["w_ada"].ap().rearrange("(k p) f -> p k f", p=128)
        for ch in range(12):
            ws = wst[ch % 2]
            cx.dma("sp", ws[:], wsrc[:, :, ch * 512:(ch + 1) * 512], [], [ws])
            for j in range(4):
                ft = ch * 4 + j
                for k in range(8):
                    cx.mm(modp[:, ft:ft + 1], ws[:, k, j * 128:(j + 1) * 128], sc[:, k:k + 1], k == 0, k == 7,
                          [ws, sc], [modp])
            if ch in (4, 5, 10, 11, 6, 7, 8, 9):
                rp = rowp[ch % 2]
                gi = {4: 0, 5: 1, 10: 2, 11: 3, 6: 4, 7: 5, 8: 6, 9: 7}[ch]
                for k in range(8):
                    cx.mm(rp[:], screp[:, k, :], ws[:, k, :], k == 0, k == 7, [ws, screp], [rp])
                cx.ew("dve", lambda e, rp=rp, gi=gi: e.tensor_tensor(out=C.gtrow[:, gi * 512:(gi + 1) * 512], in0=rp[:],
                                                                      in1=bgt[:, gi * 512:(gi + 1) * 512], op=ALU.add),
                      [rp, bgt], [C.gtrow])
        cx.ew("dve", lambda e: e.tensor_tensor(out=C.modF[:], in0=modp[:], in1=badaF[:], op=ALU.add),
              [modp, badaF], [C.modF])
        cx.ew("dve", lambda e: e.tensor_scalar_add(out=C.gtrow[:, 3072:4096], in0=C.gtrow[:, 3072:4096], scalar1=1.0), [C.gtrow], [C.gtrow])
        cx.ew("dve", lambda e: e.tensor_tensor(out=C.gtrow[:, 3072:4096], in0=C.gtrow[:, 3072:4096], in1=n2row[:], op=ALU.mult),
              [C.gtrow, n2row], [C.gtrow])
        for (dst, nw, lo) in ((C.sc1F, n1, 8), (C.sc2F, n2, 32)):
            cx.ew("dve", lambda e, lo=lo: e.tensor_scalar_add(out=tmp[:], in0=C.modF[:, lo:lo + 8], scalar1=1.0),
                  [C.modF], [tmp])
            cx.ew("dve", lambda e, dst=dst, nw=nw: e.tensor_tensor(out=dst[:], in0=tmp[:], in1=nw[:], op=ALU.mult),
                  [tmp, nw], [dst])
        cx.P.flush()


def rms_rows(cx, xt, ssq, rs, junk, nj):
    for j in range(nj):
        cx.actv(junk[:, j, :], xt[:, j, :], AF.Square, [xt], [junk, ssq], accum=ssq[:, j:j + 1])
    cx.actv(rs[:, 0:nj], ssq[:, 0:nj], AF.Sqrt, [ssq], [rs], bias=cx.C.eps[:], scale=1.0 / D)
    cx.ew("dve", lambda e: e.reciprocal(out=rs[:, 0:nj], in_=rs[:, 0:nj]), [rs], [rs])


def phase_proj(cx, C, din, hf):
    sc = cx.dram
    with ExitStack() as st:
        xt = [cx.sb(st, "xt%d" % i, [128, 4, 1024]) for i in range(2)]
        stage = [TL(None, xt[i].k, view=xt[i][:].rearrange("p j (a b) -> p (j a) b", b=512)) for i in range(2)]
        w_fmA = cx.sb(st, "w_fmA", [128, 8, 1024], BF16)
        w_fmQ = cx.sb(st, "w_fmQ", [128, 8, 512], BF16)
        w_tm = cx.sb(st, "w_tm", [128, 8, 416], BF16)
        load_w_bf16(cx, st, din["w_fmA"], 1024, w_fmA, stage, "wa")
        load_w_bf16(cx, st, din["w_fmQ"], 512, w_fmQ, stage, "wq")
        load_w_bf16(cx, st, din["w_tm"], 416, w_tm, stage, "wt")
        rm = cx.sb(st, "rm", [128, 128])
        bones = cx.sb(st, "bones", [128, 128])
        qkw = cx.sb(st, "qkw", [128, 2])
        cx.dma("sp", rm[:], din["Rm"].ap(), [], [rm])
        cx.dma("sp", bones[:], din["bones"].ap(), [], [bones])
        cx.dma("sp", qkw[:], din["qkw"].ap(), [], [qkw])
        xb = [cx.sb(st, "xb%d" % i, [128, 4, 1024], BF16) for i in range(2)]
        hT = [cx.sb(st, "hT%d" % i, [128, 8, 512], BF16) for i in range(2)]
        ssq = [cx.sb(st, "ssq%d" % i, [128, 4]) for i in range(2)]
        rs = [cx.sb(st, "rs%d" % i, [128, 4]) for i in range(2)]
        cs = [cx.sb(st, "cos%d" % i, [128, 512]) for i in range(2)]
        sn = [cx.sb(st, "sin%d" % i, [128, 512]) for i in range(2)]
        dbuf = [cx.sb(st, "dbuf%d" % i, [128, 6, 512]) for i in range(2)]
        qo = [cx.sb(st, "qo%d" % i, [128, 6, 512], BF16) for i in range(2)]
        va = [cx.sb(st, "va%d" % i, [128, 4, 132], BF16) for i in range(2)]
        ba = [cx.sb(st, "ba%d" % i, [128, 4, 32]) for i in range(2)]
        dz = [cx.sb(st, "dz%d" % i, [128, 4, 256]) for i in range(2)]
        qx = [cx.sb(st, "qx%d" % i, [128, 512]) for i in range(3)]
        qsq = [cx.sb(st, "qsq%d" % i, [128, 512]) for i in range(3)]
        qrs = [cx.sb(st, "qrs%d" % i, [128, 512]) for i in range(3)]
        xn = [cx.sb(st, "xn%d" % i, [128, 512]) for i in range(3)]
        t1 = [cx.sb(st, "t1%d" % i, [128, 512]) for i in range(3)]
        t2 = [cx.sb(st, "t2%d" % i, [128, 512]) for i in range(3)]
        trp = [cx.ps(st, "trp%d" % i, [128, 512], BF16) for i in range(2)]
        pj = [cx.ps(st, "pj%d" % i, [128, 512]) for i in range(2)]
        ssp = cx.ps(st, "ssp", [128, 512])
        rtp = cx.ps(st, "rtp", [128, 512])
        tmp_ = [cx.ps(st, "tmp%d" % i, [128, 512]) for i in range(2)]
        for i in range(2):
            cx.ew("pool", lambda e, i=i: e.memset(va[i][:], 1.0), [], [va[i]])
        xsrc = din["x"].ap()
        npj = 0
        nqk = 0
        for c in range(NCH):
            b = c % 2
            own = c < 8
            t0 = c * 512
            cx.dma("sp", xt[b][:], xsrc[t0:t0 + 512, :].rearrange("(j p) d -> p j d", p=128), [], [xt[b]])
            cx.dma("sp", cs[b][:], din["cosT"].ap()[:, t0:t0 + 512], [], [cs[b]])
            cx.dma("sp", sn[b][:], din["sinT"].ap()[:, t0:t0 + 512], [], [sn[b]])
            rms_rows(cx, xt[b], ssq[b], rs[b], xb[b], 4)
            for j in range(4):
                cx.ew("dve", lambda e, j=j, b=b: e.tensor_scalar(out=xb[b][:, j, :], in0=xt[b][:, j, :],
                                                                 scalar1=rs[b][:, j:j + 1], scalar2=None, op0=ALU.mult),
                      [xt[b], rs[b]], [xb[b]])
            for k in range(8):
                tp = trp[k % 2]
                for j in range(4):
                    cx.tr(tp[:, j * 128:(j + 1) * 128], xb[b][:, j, k * 128:(k + 1) * 128], C.identb[:],
                          [xb[b], C.identb], [tp])
                cx.actv(hT[b][:, k, :], tp[:], AF.Identity, [tp, C.sc1F, C.modF], [hT[b]],
                        scale=C.sc1F[:, k:k + 1], bias=C.modF[:, k:k + 1])
            cx.dma("sp", sc["HT"].ap()[:, :, t0:t0 + 512], hT[b][:], [hT[b]], [("HT", c)])
            tiles = [("A", i) for i in range(8)] + ([("Q", i) for i in range(4)] if own else [])
            if "fm" in SKIP:
                tiles = []

            def stageB(s, wc):
                cx.mm(ssp[:], bones[:], qsq[s][:], True, True, [bones, qsq[s]], [ssp])
                cx.actv(qrs[s][:], ssp[:], AF.Sqrt, [ssp], [qrs[s]], bias=C.eps[:], scale=1.0 / 64)
                cx.ew("dve", lambda e, s=s: e.reciprocal(out=qrs[s][:], in_=qrs[s][:]), [qrs[s]], [qrs[s]])
                cx.ew("dve", lambda e, s=s, wc=wc: e.scalar_tensor_tensor(out=xn[s][:], in0=qx[s][:], scalar=wc,
                                                                          in1=qrs[s][:], op0=ALU.mult, op1=ALU.mult),
                      [qx[s], qrs[s], qkw], [xn[s]])

            def stageC(s, slot, b):
                cx.mm(rtp[:], rm[:], xn[s][:], True, True, [rm, xn[s]], [rtp])
                cx.ew("pool", lambda e, s=s, b=b: e.tensor_tensor(out=t1[s][:], in0=xn[s][:], in1=cs[b][:], op=ALU.mult),
                      [xn[s], cs[b]], [t1[s]])
                cx.ew("dve", lambda e, s=s, b=b: e.tensor_tensor(out=t2[s][:], in0=rtp[:], in1=sn[b][:], op=ALU.mult),
                      [rtp, sn[b]], [t2[s]])
                cx.ew("dve", lambda e, s=s, b=b, slot=slot: e.tensor_tensor(out=qo[b][:, slot, :], in0=t1[s][:], in1=t2[s][:],
                                                                             op=ALU.add),
                      [t1[s], t2[s]], [qo[b]])

            qB = []
            qC = []

            def advance():
                if qC:
                    qC.pop(0)()
                if qB:
                    qB.pop(0)()

            def mkB(s, wc, slot, b):
                def f_():
                    stageB(s, wc)
                    qC.append(lambda: stageC(s, slot, b))
                return f_

            for (kind, i) in tiles:
                p = pj[npj % 2]
                npj += 1
                wsrc_ = w_fmA if kind == "A" else w_fmQ
                for k in range(8):
                    cx.mm(p[:], wsrc_[:, k, i * 128:(i + 1) * 128], hT[b][:, k, :], k == 0, k == 7,
                          [wsrc_, hT[b]], [p])
                if kind == "A" and i >= 2:
                    cx.ew("act", lambda e, p=p, i=i, b=b: e.copy(out=dbuf[b][:, i - 2, :], in_=p[:]), [p], [dbuf[b]])
                    advance()
                    continue
                if "qk" in SKIP:
                    continue
                s = nqk % 3
                nqk += 1
                wc = qkw[:, 0:1] if kind == "Q" else qkw[:, 1:2]
                slot = i if kind == "A" else 2 + i
                cx.ew("act", lambda e, p=p, s=s: e.copy(out=qx[s][:], in_=p[:]), [p], [qx[s]])
                cx.actv(qsq[s][:], p[:], AF.Square, [p], [qsq[s]])
                advance()
                qB.append(mkB(s, wc, slot, b))
            cx.dma("sp", sc["DPRE"].ap()[:, :, t0:t0 + 512].rearrange("j p t -> p j t"), dbuf[b][:], [dbuf[b]], [("DPRE", c)])
            for j in range(4 if "tm" not in SKIP else 0):
                tp = tmp_[j % 2]
                for k in range(8):
                    cx.mm(tp[:, 0:416], hT[b][:, k, j * 128:(j + 1) * 128], w_tm[:, k, :], k == 0, k == 7,
                          [hT[b], w_tm], [tp])
                for kv in range(2 if "tmva" not in SKIP else 0):
                    cx.ew("dve", lambda e, tp=tp, j=j, kv=kv, b=b: e.tensor_copy(out=va[b][:, j, kv * 66:kv * 66 + 64],
                                                                                in_=tp[:, kv * 64:(kv + 1) * 64]),
                          [tp], [va[b]])
                if "tmba" not in SKIP:
                    cx.ew("dve", lambda e, tp=tp, j=j, b=b: e.tensor_copy(out=ba[b][:, j, :], in_=tp[:, 128:160]), [tp], [ba[b]])
                if "tmdz" not in SKIP:
                    cx.actv(dz[b][:, j, :], tp[:, 160:416], AF.Silu, [tp], [dz[b]])
                advance()
            while qB or qC:
                advance()
            cx.dma("sp", sc["KT"].ap()[:, :, t0:t0 + 512].rearrange("j p t -> p j t"), qo[b][:, 0:2, :], [qo[b]], [("KT", c)])
            if own:
                l0 = t0
                cx.dma("sp", sc["QT"].ap()[:, :, l0:l0 + 512].rearrange("j p t -> p j t"), qo[b][:, 2:6, :], [qo[b]],
                       [("QT", c)])
            cx.dma("sp", sc["VA"].ap()[t0:t0 + 512, :].rearrange("(j p) f -> p j f", p=128), va[b][:], [va[b]], [("VA", c)])
            cx.dma("sp", sc["BA"].ap()[t0:t0 + 512, :].rearrange("(j p) f -> p j f", p=128), ba[b][:], [ba[b]], [("BA", c)])
            cx.dma("sp", sc["DZS"].ap()[t0:t0 + 512, :].rearrange("(j p) f -> p j f", p=128), dz[b][:], [dz[b]], [("DZS", c)])
        cx.P.flush()


OFF = dict(aq=0, ak=512, av=640, dq=768, dk=1280, dv=1792, dz=2304, b=2816, a=2832, g=2848)


def _fm(v, ncol):
    return np.ascontiguousarray(np.asarray(v, np.float32).reshape(ncol, 128).T)


def const_tables():
    t = {}
    freqs = (np.float32(10000.0) ** (-(np.arange(16, dtype=np.float32) * np.float32(2.0) / np.float32(32)))).astype(np.float32)
    tok = np.arange(S)
    pos = np.stack([tok // 64, tok % 64], 0).astype(np.float32)
    cosT = np.zeros((128, S), np.float32)
    sinT = np.zeros((128, S), np.float32)
    rm = np.zeros((128, 128), np.float32)
    for p in range(128):
        d = p % 64
        a = d // 32
        i = d % 32
        ang = (pos[a] * freqs[i % 16]).astype(np.float32)
        cosT[p] = np.cos(ang)
        sinT[p] = np.sin(ang)
        if i < 16:
            rm[p + 16, p] = -1.0
        else:
            rm[p - 16, p] = 1.0
    t["cosT"], t["sinT"], t["Rm"] = cosT, sinT, rm
    bones = np.zeros((128, 128), np.float32)
    bones[:64, :64] = 1.0
    bones[64:, 64:] = 1.0
    t["bones"] = bones
    t["dnmask"] = dn_masks()
    return t


def core_inputs(inp, core, tabs):
    b, hf = core // 2, core % 2
    rev = hf == 1
    w_in = inp["w_in"][0]
    d = {}
    for k in ("Rm", "bones", "dnmask"):
        d[k] = tabs[k]
    d["cosT"] = np.ascontiguousarray(tabs["cosT"][:, ::-1]) if rev else tabs["cosT"]
    d["sinT"] = np.ascontiguousarray(tabs["sinT"][:, ::-1]) if rev else tabs["sinT"]
    xb = inp["x"][b]
    d["x"] = np.ascontiguousarray(xb[::-1] if rev else xb)
    d["cT"] = _fm(inp["c"][b], 8)
    d["w_ada"] = np.ascontiguousarray(inp["w_ada"][0])
    d["b_adaF"] = _fm(inp["b_ada"][0], 48)
    ba_ = inp["b_ada"][0]
    d["b_gt"] = np.ascontiguousarray(np.concatenate([ba_[2048:3072], ba_[5120:6144], ba_[3072:4096], ba_[4096:5120]])[None, :])
    d["norm2R"] = np.ascontiguousarray(inp["norm2_w"][0][None, :].astype(np.float32))
    d["norm1F"] = _fm(inp["norm1_w"][0], 8)
    d["norm2F"] = _fm(inp["norm2_w"][0], 8)
    ak = w_in[:, OFF["ak"]:OFF["ak"] + 128]
    h0 = hf * 256
    d["w_fmA"] = np.ascontiguousarray(np.concatenate(
        [ak[:, 0:64], ak[:, 0:64], ak[:, 64:128], ak[:, 64:128],
         w_in[:, OFF["dq"] + h0:OFF["dq"] + h0 + 256], w_in[:, OFF["dk"] + h0:OFF["dk"] + h0 + 256],
         w_in[:, OFF["dv"] + h0:OFF["dv"] + h0 + 256]], axis=1))
    d["w_fmQ"] = np.ascontiguousarray(w_in[:, 0:512])
    dirs = (1, 0) if rev else (0, 1)
    hs = slice(hf * 4, hf * 4 + 4)
    bcols = [w_in[:, OFF["b"] + dd * 8 + hf * 4:OFF["b"] + dd * 8 + hf * 4 + 4] for dd in dirs]
    acols = [w_in[:, OFF["a"] + dd * 8 + hf * 4:OFF["a"] + dd * 8 + hf * 4 + 4] for dd in dirs]
    pad = w_in[:, OFF["b"]:OFF["b"] + 16]
    d["w_tm"] = np.ascontiguousarray(np.concatenate(
        [w_in[:, OFF["av"]:OFF["av"] + 128]] + bcols + acols + [pad, w_in[:, OFF["dz"] + h0:OFF["dz"] + h0 + 256]], axis=1))
    cwl = []
    cw = inp["conv_w"][0][::-1] if rev else inp["conv_w"][0]
    for base in (0, 512, 1024):
        for jp in range(2):
            c0 = base + h0 + jp * 128
            cwl.append(cw[:, c0:c0 + 128].T)
    d["conv_wF"] = np.ascontiguousarray(np.stack(cwl, 1).astype(np.float32))
    al = np.stack([inp["a_log"][0][dd, hs] for dd in dirs], 0)
    db = np.stack([inp["dt_bias"][0][dd, hs] for dd in dirs], 0)
    d["agrow"] = np.ascontiguousarray(np.stack([al, db], 0)[None].astype(np.float32))
    d["w_gates"] = np.ascontiguousarray(w_in[:, OFF["g"]:OFF["g"] + 2048])
    d["w_attn_up"] = np.ascontiguousarray(inp["w_attn_up"][0])
    wdu = inp["w_dn_up"][0]
    d["w_dn_up"] = np.ascontiguousarray(np.concatenate([wdu[h0:h0 + 256], wdu[256 - h0:512 - h0]], 0))
    d["w_o"] = np.ascontiguousarray(inp["w_o"][0])
    d["w_router"] = np.ascontiguousarray(inp["w_router"][0])
    d["eoff"] = (np.arange(16, dtype=np.float32) * 1024.0)[None, :].astype(np.float32)
    d["w_gate"] = np.ascontiguousarray(inp["w_gate"][0])
    d["w_up"] = np.ascontiguousarray(inp["w_up"][0])
    d["w_down"] = np.ascontiguousarray(inp["w_down"][0])
    d["sel"] = np.array([[1.0, 0.0]] if rev else [[0.0, 1.0]], np.float32)
    d["dnw_row"] = np.ascontiguousarray(inp["dn_norm_w"][0][None, :].astype(np.float32))
    d["qkrow"] = np.ascontiguousarray(np.stack([inp["q_norm_w"][0], inp["k_norm_w"][0]], 0)[None].astype(np.float32))
    d["qkw"] = np.ascontiguousarray(np.stack([np.tile(inp["q_norm_w"][0], 2), np.tile(inp["k_norm_w"][0], 2)], 1).astype(np.float32))
    return d


INPUT_SHAPES = dict(
    x=[S, D], cT=[128, 8], w_ada=[D, 6144], b_adaF=[128, 48], b_gt=[1, 4096], norm2R=[1, 1024], norm1F=[128, 8], norm2F=[128, 8],
    w_fmA=[D, 1024], w_fmQ=[D, 512], w_tm=[D, 416], qkw=[128, 2],
    cosT=[128, S], sinT=[128, S], Rm=[128, 128], bones=[128, 128], qkrow=[1, 2, 64], conv_wF=[128, 6, 5], dnmask=[128, 9, 128], agrow=[1, 2, 2, 4], dnw_row=[1, 64], w_gates=[D, 2048], w_attn_up=[512, D], w_dn_up=[512, D], w_o=[D, D], w_router=[D, 16], sel=[1, 2], eoff=[1, 16], w_gate=[16, D, D], w_up=[16, D, D], w_down=[16, D, D],
)


def build(hf_sym=None, debug=False, upto=99, hf=0):
    nc = bass.Bass("TRN2", target_bir_lowering=False)
    with ExitStack() as st:
        P = Prog(nc, st)
        cx = Ctx(nc, P, st, debug)
        C = Consts()
        cx.C = C
        din = {k: cx.inp(k, shp) for k, shp in INPUT_SHAPES.items()}
        cx.scratch("HT", [128, 8, S], BF16, dbg=True)
        cx.scratch("QT", [4, 128, 4096], BF16, dbg=True)
        cx.scratch("KT", [2, 128, S], BF16, dbg=True)
        cx.scratch("VA", [S, 132], BF16, dbg=True)
        cx.scratch("DPRE", [6, 128, S], F32, dbg=True)
        cx.scratch("BA", [S, 32], F32, dbg=True)
        cx.scratch("DZS", [S, 256], F32, dbg=True)
        if debug:
            dbg_mod = nc.dram_tensor("dbg_mod", [128, 48 + 2048 + 16], F32, kind="ExternalOutput")
        setup_consts(cx, C, din)
        for e_ in range(16):
            cx.scratch("XE%d" % e_, [1024 + 32 * 128, 529], F32, dbg=False)
        if upto >= 8:
            moe_prefill(cx, C)
        phase_adaln(cx, C, din)
        if debug:
            cx.dma("sp", dbg_mod.ap()[:, 0:48], C.modF[:], [C.modF], ["dbg1"])
            cx.dma("sp", dbg_mod.ap()[:, 48:48 + 2048], C.gtrow[:, 0:2048], [C.gtrow], ["dbg2"])
            cx.dma("sp", dbg_mod.ap()[:, 2096:2104], C.sc1F[:], [C.sc1F], ["dbg3"])
            cx.dma("sp", dbg_mod.ap()[:, 2104:2112], C.sc2F[:], [C.sc2F], ["dbg4"])
        if upto >= 2 and "noproj" not in SKIP:
            phase_proj(cx, C, din, hf)
        cx.scratch("ATT", [512, 4096], BF16, dbg=True)
        merged_cd = upto >= 4 and "noattn" not in SKIP and "nodnpre" not in SKIP and "mergecd" in SKIP
        cx.scratch("DQT", [2, 128, S], BF16, dbg=True)
        cx.scratch("DKT", [2, 128, S], BF16, dbg=True)
        cx.scratch("DKV", [S, 512], BF16, dbg=True)
        if merged_cd:
            phase_attn(cx, C, din, hf, with_dnpre=True)
        else:
            if upto >= 3 and "noattn" not in SKIP:
                phase_attn(cx, C, din, hf)
            if upto >= 4 and "nodnpre" not in SKIP:
                phase_dn_pre(cx, C, din, hf)
        cx.scratch("OD", [2, S, 256], F32, dbg=True)
        wstack = ExitStack()
        if upto >= 7 and "nomerge" not in SKIP:
            merge_weights(cx, C, din, wstack)
        if upto >= 5 and "nodn" not in SKIP:
            phase_dn(cx, C, din, hf)
        cx.scratch("DNT", [256, 4096], BF16, dbg=True)
        cx.scratch("DNTR", [256, 4096], BF16, dbg=(GROUPS is None))
        cx.scratch("DNGR", [512, 4096], BF16, dbg=(GROUPS is None))
        if upto >= 6 and "nodnout" not in SKIP:
            phase_dn_out(cx, C, din, hf)
        out_x = nc.dram_tensor("out_x", [4096 + 128, D], F32, kind="ExternalOutput")
        cx.dram["ACC"] = out_x
        cx.scratch("H2X", [4096, 529], F32, dbg=True)
        cx.scratch("AFF", [4096, 16], F32, dbg=False)
        cx.scratch("AFFG", [S, 16], F32, dbg=True)
        if upto >= 7 and "nomerge" not in SKIP:
            phase_merge(cx, C, din, hf, out_x)
        wstack.close()
        if debug:
            cx.scratch("DBGM", [128, 16], F32, dbg=True)
            cx.scratch("DBGD", [4096, 16], I32, dbg=True)
        if upto >= 8:
            phase_moe(cx, C, din, out_x)
        P.emit()
    return nc


def phase_attn(cx, C, din, hf, with_dnpre=False):
    sc = cx.dram
    NQC = 4096 // 256
    NKT = S // 128
    if "attn_small" in SKIP:
        NKT = 4
    with ExitStack() as st:
        kt2 = [cx.sb(st, "kt2_%d" % i, [128, S], BF16) for i in range(2)]
        qbd2 = [cx.sb(st, "qbd_%d" % i, [128, NQC, 512], BF16) for i in range(2)]
        qbd = [qbd2[i % 2] for i in range(4)]
        va = cx.sb(st, "va_all", [128, 64, 132], BF16)
        side = phase_dn_pre(cx, C, din, hf, host=st) if with_dnpre else []
        for i in range(2):
            cx.dma("sp", kt2[i][:], sc["KT"].ap()[i], [], [kt2[i]])
        for i in range(2):
            cx.ew("pool" if i % 2 else "dve", lambda e, i=i: e.memset(qbd2[i][:], 0.0), [], [qbd2[i]])

        def load_q(i):
            cx.dma("sp", qbd[i][0:64, :, 0:256], sc["QT"].ap()[i, 0:64, :].rearrange("p (c t) -> p c t", t=256), [], [qbd[i]])
            cx.dma("sp", qbd[i][64:128, :, 256:512], sc["QT"].ap()[i, 64:128, :].rearrange("p (c t) -> p c t", t=256), [], [qbd[i]])

        load_q(0)
        load_q(1)
        for g in range(4):
            cx.dma("sp", va[:, g * 16:(g + 1) * 16, :],
                   sc["VA"].ap()[g * 2048:(g + 1) * 2048, :].rearrange("(kt p) f -> p kt f", p=128), [], [va])
        wrow = cx.sb(st, "wrow", [128, 2, 64])
        wmax = cx.sb(st, "wmax", [128, 2])
        negb = cx.sb(st, "negb", [128, 1])
        cx.dma("sp", wrow[:], din["qkrow"].ap().to_broadcast([128, 2, 64]), [], [wrow])
        cx.ew("dve", lambda e: e.reduce_max(out=wmax[:], in_=wrow[:], axis=AX.X, apply_absolute_value=True), [wrow], [wmax])
        cx.ew("dve", lambda e: e.scalar_tensor_tensor(out=negb[:], in0=wmax[:, 0:1], scalar=-8.0, in1=wmax[:, 1:2],
                                                      op0=ALU.mult, op1=ALU.mult), [wmax], [negb])
        ones = cx.sb(st, "ones_att", [128, 64])
        cx.ew("pool", lambda e: e.memset(ones[:], 1.0), [], [ones])
        sT = [cx.ps(st, "sT%d" % i, [128, 512]) for i in range(ATT_NS)]
        oT = [cx.ps(st, "oT%d" % i, [128, 512]) for i in range(2)]
        bc = cx.ps(st, "bc", [128, 512])
        pT = [cx.sb(st, "pT%d" % i, [128, 512], BF16) for i in range(ATT_NP)]
        rinv = [cx.sb(st, "rinv%d" % i, [128, 512]) for i in range(2)]
        osb = [cx.sb(st, "osb%d" % i, [64, 512]) for i in range(2)]
        ao = [cx.sb(st, "ao%d" % i, [64, 512], BF16) for i in range(2)]
        it = 0
        ns = 0
        NJ = 4 if "attn_1h" not in SKIP else 1
        ngrp = NJ * NQC
        nside = 0
        for j in range(NJ):
            kv = j // 2
            if j >= 2:
                load_q(j)
            for qc in range(NQC):
                while side and nside < len(side) and nside * ngrp <= it * len(side):
                    side[nside]()
                    nside += 1
                o = oT[it % 2]
                u = it % 2

                def S_mm(kt):
                    s_ = sT[(ns + kt) % ATT_NS]
                    cx.mm(s_[:], kt2[kv][:, kt * 128:(kt + 1) * 128], qbd[j][:, qc, :], True, True, [kt2[kv], qbd[j]], [s_])
                    p_ = pT[(ns + kt) % ATT_NP]
                    cx.actv(p_[:], s_[:], AF.Exp, [s_, negb], [p_], bias=negb[:], scale=0.125)

                def PV_mm(kt):
                    p_ = pT[(ns + kt) % ATT_NP]
                    cx.mm(o[0:65, :], va[:, kt, kv * 66:kv * 66 + 65], p_[:], kt == 0, kt == NKT - 1, [va, p_], [o])

                LAG = ATT_LAG
                for kt in range(NKT + LAG):
                    if kt < NKT:
                        S_mm(kt)
                    if kt >= LAG:
                        PV_mm(kt - LAG)
                ns += NKT
                cx.ew("dve", lambda e, o=o, u=u: e.reciprocal(out=rinv[u][64:65, :], in_=o[64:65, :]), [o], [rinv[u]])
                cx.ew("act", lambda e, o=o, u=u: e.copy(out=osb[u][:], in_=o[0:64, :]), [o], [osb[u]])
                cx.mm(bc[0:64, :], ones[64:65, 0:64], rinv[u][64:65, :], True, True, [ones, rinv[u]], [bc])
                cx.ew("dve", lambda e, u=u: e.tensor_tensor(out=ao[u][:], in0=osb[u][:], in1=bc[0:64, :], op=ALU.mult),
                      [osb[u], bc], [ao[u]])
                for hh in range(2):
                    h = 2 * j + hh
                    cx.dma("sp", sc["ATT"].ap()[h * 64:(h + 1) * 64, qc * 256:(qc + 1) * 256], ao[u][:, hh * 256:(hh + 1) * 256],
                           [ao[u]], [("ATT", h, qc)])
                it += 1
        while side and nside < len(side):
            side[nside]()
            nside += 1
        cx.P.flush()


def phase_dn_pre(cx, C, din, hf, host=None):
    sc = cx.dram
    H = S // 2
    NB = 3 if host is None else 1
    with ExitStack() as st_own:
        st = st_own if host is None else host
        pre = [cx.sb(st, "pre%d" % i, [128, H + 4]) for i in range(3 if host is None else 2)]
        acc = [cx.sb(st, "acc%d" % i, [128, H]) for i in range(4 if host is None else 2)]
        cw = cx.sb(st, "cw", [128, 6, 5])
        bones = cx.sb(st, "bones2", [128, 128])
        cx.dma("sp", cw[:], din["conv_wF"].ap(), [], [cw])
        cx.dma("sp", bones[:], din["bones"].ap(), [], [bones])
        sqt = [cx.sb(st, "sqt%d" % i, [128, 512]) for i in range(3)]
        rst = [cx.sb(st, "rst%d" % i, [128, 512]) for i in range(3)]
        tmo = [cx.sb(st, "tmo%d" % i, [128, 512], BF16) for i in range(3)]
        abf = [cx.sb(st, "abf%d" % i, [128, H], BF16) for i in range(2 if host is None else 1)]
        ssp = [cx.ps(st, "ssp2_%d" % i, [128, 512]) for i in range(NB)]
        trp = [cx.ps(st, "trp2_%d" % i, [128, 512]) for i in range(NB)]
        NP_, NA_, NF_ = len(pre), len(acc), len(abf)
        nn = [0]

        def stage1(nt):
            ti, half = nt // 2, nt % 2
            p_, a_ = pre[nt % NP_], acc[nt % NA_]
            if half == 0:
                cx.ew("pool", lambda e, p_=p_: e.memset(p_[:, 0:2], 0.0), [], [p_])
                cx.dma("sp", p_[:, 2:2050], sc["DPRE"].ap()[ti, :, 0:2048], [], [p_])
                cx.dma("sp", p_[:, 2050:H + 4], sc["DPRE"].ap()[ti, :, 2048:H + 2], [], [p_])
            else:
                cx.ew("pool", lambda e, p_=p_: e.memset(p_[:, H + 2:H + 4], 0.0), [], [p_])
                cx.dma("sp", p_[:, 0:2050], sc["DPRE"].ap()[ti, :, H - 2:H + 2048], [], [p_])
                cx.dma("sp", p_[:, 2050:H + 2], sc["DPRE"].ap()[ti, :, H + 2048:S], [], [p_])
            cx.ew("dve", lambda e, p_=p_, a_=a_, ti=ti: e.tensor_scalar(out=a_[:], in0=p_[:, 0:H], scalar1=cw[:, ti, 0:1], scalar2=None,
                                                                      op0=ALU.mult), [p_, cw], [a_])
            for k in range(1, 5):
                cx.ew("dve", lambda e, p_=p_, a_=a_, ti=ti, k=k: e.scalar_tensor_tensor(out=a_[:], in0=p_[:, k:k + H], scalar=cw[:, ti, k:k + 1],
                                                                                       in1=a_[:], op0=ALU.mult, op1=ALU.add),
                      [p_, cw, a_], [a_])
            cx.actv(a_[:], a_[:], AF.Silu, [a_], [a_])

        def stage2(nt):
            ti, half = nt // 2, nt % 2
            kind, jp = ti // 2, ti % 2
            a_ = acc[nt % NA_]
            ab = abf[nt % NF_]
            c0 = half * H
            if kind < 2:
                for c in range(H // 512):
                    s = nn[0] % 3
                    nn[0] += 1
                    sl = slice(c * 512, (c + 1) * 512)
                    cx.ew("pool", lambda e, s=s, a_=a_, sl=sl: e.tensor_tensor(out=sqt[s][:], in0=a_[:, sl], in1=a_[:, sl], op=ALU.mult),
                          [a_], [sqt[s]])
                    sp_ = ssp[s % NB]
                    cx.mm(sp_[:], bones[:], sqt[s][:], True, True, [bones, sqt[s]], [sp_])
                    cx.actv(rst[s][:], sp_[:], AF.Sqrt, [sp_, C.eps], [rst[s]], bias=C.eps[:], scale=1.0)
                    cx.ew("dve", lambda e, s=s: e.reciprocal(out=rst[s][:], in_=rst[s][:]), [rst[s]], [rst[s]])
                    if kind == 0:
                        cx.ew("pool", lambda e, s=s: e.tensor_scalar(out=rst[s][:], in0=rst[s][:], scalar1=0.125, scalar2=None, op0=ALU.mult),
                              [rst[s]], [rst[s]])
                    cx.ew("pool", lambda e, s=s, a_=a_, sl=sl: e.tensor_tensor(out=a_[:, sl], in0=a_[:, sl], in1=rst[s][:], op=ALU.mult),
                          [a_, rst[s]], [a_])
                    cx.ew("act", lambda e, a_=a_, sl=sl, ab=ab: e.copy(out=ab[:, sl], in_=a_[:, sl]), [a_], [ab])
                dst = sc["DQT"] if kind == 0 else sc["DKT"]
                for g in range(2):
                    cx.dma("sp", dst.ap()[jp, :, c0 + g * 2048:c0 + (g + 1) * 2048], ab[:, g * 2048:(g + 1) * 2048], [ab],
                           [(dst.name, jp, half, g)])
            if kind >= 1:
                col0 = (kind - 1) * 256 + jp * 128
                for c in range(H // 512):
                    s = nn[0] % 3
                    nn[0] += 1
                    tp_ = trp[s % NB]
                    for j in range(4):
                        t0 = c * 512 + j * 128
                        cx.tr(tp_[:, j * 128:(j + 1) * 128], a_[:, t0:t0 + 128], C.identf[:], [a_, C.identf], [tp_])
                    cx.ew("act", lambda e, s=s, tp_=tp_: e.copy(out=tmo[s][:], in_=tp_[:]), [tp_], [tmo[s]])
                    cx.dma("sp", sc["DKV"].ap()[c0 + c * 512:c0 + (c + 1) * 512, col0:col0 + 128].rearrange("(j p) f -> p j f", p=128),
                           tmo[s][:].rearrange("p (j f) -> p j f", f=128), [tmo[s]], [("DKV", ti, half, c)])

        items = [lambda: stage1(0)]
        for nt in range(12):
            if nt + 1 < 12:
                items.append(lambda nt=nt: stage1(nt + 1))
            items.append(lambda nt=nt: stage2(nt))
        if host is not None:
            return items
        for it_ in items:
            it_()
        cx.P.flush()


def dn_masks():
    p = np.arange(128)[:, None]
    f = np.arange(128)[None, :]
    same = (p // 64) == (f // 64)
    m = np.zeros((128, 9, 128), np.float32)
    m[:, 0] = same & (p <= f)
    m[:, 1] = same & (p >= f)
    m[:, 2] = same
    m[:, 3] = same & (f >= p)
    m[:, 4] = same & (f <= p)
    m[:, 5] = -1.0 * (same & (f < p))
    m[:, 6] = -1.0 * (same & (f > p))
    m[:, 7] = (p // 32) == (f // 32)
    m[:64, 8, 0] = 1.0
    m[64:, 8, 1] = 1.0
    return m


def phase_dn(cx, C, din, hf):
    sc = cx.dram
    NP = S // 128
    if "dn_small" in SKIP:
        NP = 2
    with ExitStack() as st:
        mk = cx.sb(st, "dnmask", [128, 9, 128])
        cx.dma("sp", mk[:], din["dnmask"].ap(), [], [mk])
        ones = cx.sb(st, "ones_dn", [128, 128])
        cx.ew("pool", lambda e: e.memset(ones[:], 1.0), [], [ones])
        bg = cx.sb(st, "bg", [128, 64, 2, 2, 4])
        with ExitStack() as st2:
            ba = cx.sb(st2, "ba_all", [128, 64, 32])
            agr = cx.sb(st2, "agr", [128, 2, 2, 4])
            nea = cx.sb(st2, "nea", [128, 2, 4])
            tmpg = cx.sb(st2, "tmpg", [128, 64, 4])
            cx.dma("sp", ba[:], sc["BA"].ap().rearrange("(i p) f -> p i f", p=128), [], [ba])
            cx.dma("sp", agr[:], din["agrow"].ap().to_broadcast([128, 2, 2, 4]), [], [agr])
            cx.actv(nea[:], agr[:, 0], AF.Exp, [agr], [nea])
            cx.ew("dve", lambda e: e.tensor_scalar(out=nea[:], in0=nea[:], scalar1=-1.0, scalar2=None, op0=ALU.mult), [nea], [nea])
            for d in range(2):
                c0 = d * 4
                cx.actv(bg[:, :, d, 0, :], ba[:, :, c0:c0 + 4], AF.Sigmoid, [ba], [bg])
                cx.ew("dve", lambda e, d=d, c0=c0: e.tensor_tensor(out=tmpg[:], in0=ba[:, :, 8 + c0:8 + c0 + 4],
                                                                  in1=agr[:, 1, d, :].unsqueeze(1).to_broadcast([128, 64, 4]), op=ALU.add),
                      [ba, agr], [tmpg])
                cx.actv(tmpg[:], tmpg[:], AF.Exp, [tmpg], [tmpg])
                cx.actv(tmpg[:], tmpg[:], AF.Ln, [tmpg], [tmpg], bias=1.0, scale=1.0)
                cx.ew("dve", lambda e, d=d: e.tensor_tensor(out=bg[:, :, d, 1, :], in0=tmpg[:],
                                                           in1=nea[:, d, :].unsqueeze(1).to_broadcast([128, 64, 4]), op=ALU.mult),
                      [tmpg, nea], [bg])
            cx.P.flush()
        banks = [cx.ps(st, "dnb%d" % i, [128, 512]) for i in range(8)]
        nb = [0]

        def bank():
            nb[0] += 1
            return banks[nb[0] % 8]

        chains = {}
        for d in range(2):
            for jp in range(2):
                t = {}
                nm = "c%d%d_" % (d, jp)
                BFN = ("kq", "ktm", "vtm", "aT0", "aT1", "TT2", "vb", "kbg", "kdp0", "kdp1", "qg", "wT", "vnew", "Sbf")
                for name, shape in (("kq", [128, 256]), ("ktm", [128, 2, 64]), ("vtm", [128, 2, 64]), ("rhsg", [128, 2, 128]),
                                    ("gexp", [128, 2, 64]), ("gcs", [128, 6]), ("E", [128, 2, 128]), ("Emin", [128, 2, 128]),
                                    ("Emax", [128, 2, 128]), ("A", [128, 2, 128]), ("B", [128, 2, 128]), ("egr", [128, 128]),
                                    ("F2", [128, 2, 128]), ("Fm", [128, 2, 128]), ("aT0", [128, 128]), ("aT1", [128, 128]),
                                    ("TT2", [128, 2, 128]),
                                    ("sca", [128, 8]), ("vb", [128, 2, 64]), ("kbg", [128, 2, 64]), ("kdp0", [128, 128]),
                                    ("kdp1", [128, 128]), ("qg", [128, 128]), ("u", [128, 128]), ("wT", [128, 128]),
                                    ("vnew", [128, 128]), ("obuf", [128, 128]), ("Sst", [128, 128]), ("Sbf", [128, 128])):
                    t[name] = cx.sb(st, nm + name, shape, BF16 if name in BFN else F32)
                for hh in range(2):
                    for name in ("M", "Md", "Mo", "Nd", "X0", "X1", "Y0", "Y1", "PT", "W1", "Dn"):
                        t[name + str(hh)] = cx.sb(st, nm + name + str(hh), [128, 128])
                for z in ("kdp0", "kdp1", "vnew", "Sst", "Sbf"):
                    cx.ew("pool", lambda e, z=z, t=t: e.memset(t[z][:], 0.0), [], [t[z]])
                chains[(d, jp)] = t

        def inverse(t, hh, pG_):
            sfx = str(hh)
            M, Md, Mo, Nd, PT, W1, Dn = (t[n + sfx] for n in ("M", "Md", "Mo", "Nd", "PT", "W1", "Dn"))
            cx.ew("dve", lambda e: e.tensor_tensor(out=M[:], in0=pG_[:, 0:128], in1=t["Fm"][:, hh, :], op=ALU.mult), [pG_, t["Fm"]], [M])
            aT = t["aT%d" % hh]
            cx.ew("dve", lambda e: e.tensor_tensor(out=aT[:], in0=pG_[:, 128:256], in1=t["F2"][:, hh, :], op=ALU.mult), [pG_, t["F2"]], [aT])
            cx.ew("pool", lambda e: e.tensor_tensor(out=Md[:], in0=M[:], in1=mk[:, 7, :], op=ALU.mult), [M, mk], [Md])
            cx.ew("pool", lambda e: e.tensor_tensor(out=Mo[:], in0=M[:], in1=Md[:], op=ALU.subtract), [M, Md], [Mo])
            yield
            pb = bank()
            cx.tr(pb[:, 0:128], Md[:], C.identf[:], [Md, C.identf], [pb])
            cx.ew("act", lambda e, pb=pb: e.copy(out=Nd[:], in_=pb[:, 0:128]), [pb], [Nd])
            cx.ew("dve", lambda e, pb=pb: e.tensor_tensor(out=PT[:], in0=pb[:, 0:128], in1=C.identf[:], op=ALU.add), [pb, C.identf], [PT])
            yield
            X, Y = Md, Nd
            pend = None
            for j in range(1, 5):
                Xn, Yn = t["X%d" % (j % 2) + sfx], t["Y%d" % (j % 2) + sfx]
                if j < 4:
                    pb = bank()
                    cx.mm(pb[:, 0:128], X[:], Y[:], True, True, [X, Y], [pb])
                    cx.ew("act", lambda e, pb=pb, Yn=Yn: e.copy(out=Yn[:], in_=pb[:, 0:128]), [pb], [Yn])
                pb = bank()
                cx.mm(pb[:, 0:128], Y[:], X[:], True, True, [X, Y], [pb])
                cx.ew("dve", lambda e, pb=pb, Xn=Xn: e.tensor_copy(out=Xn[:], in_=pb[:, 0:128]), [pb], [Xn])
                if pend is not None:
                    pend()
                def upd(Xn=Xn):
                    pb2 = bank()
                    cx.mm(pb2[:, 0:128], Xn[:], PT[:], True, True, [Xn, PT], [pb2])
                    cx.ew("dve", lambda e, pb2=pb2: e.tensor_tensor(out=PT[:], in0=pb2[:, 0:128], in1=PT[:], op=ALU.add), [pb2, PT], [PT])
                pend = upd
                X, Y = Xn, Yn
                yield
            pend()
            yield
            pb = bank()
            cx.mm(pb[:, 0:128], Mo[:], PT[:], True, True, [Mo, PT], [pb])
            cx.ew("act", lambda e, pb=pb: e.copy(out=W1[:], in_=pb[:, 0:128]), [pb], [W1])
            pb = bank()
            cx.tr(pb[:, 0:128], PT[:], C.identf[:], [PT, C.identf], [pb])
            cx.ew("dve", lambda e, pb=pb: e.tensor_copy(out=Dn[:], in_=pb[:, 0:128]), [pb], [Dn])
            yield
            pb = bank()
            cx.mm(pb[:, 0:128], Dn[:], W1[:], True, True, [Dn, W1], [pb])
            cx.ew("dve", lambda e, pb=pb: e.tensor_tensor(out=t["TT2"][:, hh, :], in0=pb[:, 0:128], in1=PT[:], op=ALU.add),
                  [pb, PT], [t["TT2"]])
            yield

        def step(d, jp, i):
            t = chains[(d, jp)]
            t0 = i * 128
            tri = mk[:, 0 + d, :]
            ma = mk[:, 3 + d, :]
            mm_ = mk[:, 5 + d, :]
            g2 = bg[:, i, d, 1, jp * 2:jp * 2 + 2]
            b2 = bg[:, i, d, 0, jp * 2:jp * 2 + 2]
            cx.dma("sp", t["kq"][:, 0:128], sc["DKT"].ap()[jp, :, t0:t0 + 128], [], [t["kq"]])
            cx.dma("sp", t["kq"][:, 128:256], sc["DQT"].ap()[jp, :, t0:t0 + 128], [], [t["kq"]])
            cx.dma("sp", t["ktm"][:], sc["DKV"].ap()[t0:t0 + 128, jp * 128:jp * 128 + 128].rearrange("p (h f) -> p h f", f=64), [], [t["ktm"]])
            cx.dma("sp", t["vtm"][:], sc["DKV"].ap()[t0:t0 + 128, 256 + jp * 128:256 + jp * 128 + 128].rearrange("p (h f) -> p h f", f=64),
                   [], [t["vtm"]])
            cx.ew("dve", lambda e: e.tensor_tensor(out=t["rhsg"][:], in0=tri.unsqueeze(1).to_broadcast([128, 2, 128]),
                                                   in1=g2.unsqueeze(2).to_broadcast([128, 2, 128]), op=ALU.mult), [mk, bg], [t["rhsg"]])
            cx.ew("pool", lambda e: e.tensor_copy(out=t["gexp"][:], in_=g2.unsqueeze(2).to_broadcast([128, 2, 64])), [bg], [t["gexp"]])
            yield
            pA = bank()
            cx.mm(pA[:, 0:256], ones[:], t["rhsg"][:].rearrange("p h c -> p (h c)"), True, True, [ones, t["rhsg"]], [pA])
            cx.mm(pA[:, 256:258], tri, g2, True, True, [mk, bg], [pA])
            cx.mm(pA[:, 258:260], mk[:, 2, :], g2, True, True, [mk, bg], [pA])
            cx.mm(pA[:, 260:262], t["gexp"][:].rearrange("p h c -> p (h c)"), mk[:, 8, 0:2], True, True, [t["gexp"], mk], [pA])
            cx.ew("dve", lambda e: e.tensor_copy(out=t["gcs"][:], in_=pA[:, 256:262]), [pA], [t["gcs"]])
            cx.ew("dve", lambda e: e.tensor_tensor(out=t["E"][:], in0=pA[:, 0:256].rearrange("p (h c) -> p h c", h=2),
                                                   in1=t["gcs"][:, 0:2].unsqueeze(2).to_broadcast([128, 2, 128]), op=ALU.subtract),
                  [pA, t["gcs"]], [t["E"]])
            cx.actv(t["egr"][0:64, :], pA[0:64, 0:128], AF.Exp, [pA], [t["egr"]])
            cx.actv(t["egr"][64:128, :], pA[64:128, 128:256], AF.Exp, [pA], [t["egr"]])
            yield
            cx.ew("dve", lambda e: e.tensor_scalar_min(out=t["Emin"][:], in0=t["E"][:], scalar1=0.0), [t["E"]], [t["Emin"]])
            cx.ew("dve", lambda e: e.tensor_scalar_max(out=t["Emax"][:], in0=t["E"][:], scalar1=0.0), [t["E"]], [t["Emax"]])
            cx.actv(t["A"][:], t["Emin"][:], AF.Exp, [t["Emin"]], [t["A"]])
            cx.actv(t["B"][:], t["Emax"][:], AF.Exp, [t["Emax"]], [t["B"]], scale=-1.0)
            cx.actv(t["sca"][:, 0:2], t["gcs"][:, 0:2], AF.Exp, [t["gcs"]], [t["sca"]])
            cx.ew("dve", lambda e: e.tensor_tensor(out=t["sca"][:, 4:6], in0=t["gcs"][:, 2:4], in1=t["gcs"][:, 0:2], op=ALU.subtract),
                  [t["gcs"]], [t["sca"]])
            cx.actv(t["sca"][:, 4:6], t["sca"][:, 4:6], AF.Exp, [t["sca"]], [t["sca"]])
            cx.actv(t["sca"][:, 6:8], t["gcs"][:, 4:6], AF.Exp, [t["gcs"]], [t["sca"]])
            yield
            cx.ew("pool", lambda e: e.tensor_tensor(out=t["F2"][:], in0=t["A"][:], in1=ma.unsqueeze(1).to_broadcast([128, 2, 128]),
                                                    op=ALU.mult), [t["A"], mk], [t["F2"]])
            for hh in range(2):
                cx.ew("dve", lambda e, hh=hh: e.scalar_tensor_tensor(out=t["Fm"][:, hh, :], in0=t["B"][:, hh, :], scalar=b2[:, hh:hh + 1],
                                                                     in1=mm_, op0=ALU.mult, op1=ALU.mult), [t["B"], bg, mk], [t["Fm"]])
            cx.ew("dve", lambda e: e.tensor_tensor(out=t["sca"][:, 2:4], in0=t["sca"][:, 0:2], in1=b2, op=ALU.mult), [t["sca"], bg], [t["sca"]])
            cx.ew("pool", lambda e: e.tensor_tensor(out=t["vb"][:], in0=t["vtm"][:], in1=b2.unsqueeze(2).to_broadcast([128, 2, 64]),
                                                    op=ALU.mult), [t["vtm"], bg], [t["vb"]])
            cx.ew("pool", lambda e: e.tensor_tensor(out=t["kbg"][:], in0=t["ktm"][:],
                                                    in1=t["sca"][:, 2:4].unsqueeze(2).to_broadcast([128, 2, 64]), op=ALU.mult),
                  [t["ktm"], t["sca"]], [t["kbg"]])
            for hh in range(2):
                kd_ = t["kdp%d" % hh]
                cx.ew("dve", lambda e, hh=hh, kd_=kd_: e.tensor_scalar(out=kd_[:, hh * 64:(hh + 1) * 64], in0=t["ktm"][:, hh, :],
                                                                        scalar1=t["sca"][:, 4 + hh:5 + hh], scalar2=None, op0=ALU.mult),
                      [t["ktm"], t["sca"]], [kd_])
            cx.ew("pool", lambda e: e.tensor_tensor(out=t["qg"][:], in0=t["kq"][:, 128:256], in1=t["egr"][:], op=ALU.mult),
                  [t["kq"], t["egr"]], [t["qg"]])
            yield
            subs = []
            for hh in range(2):
                hs = slice(hh * 64, hh * 64 + 64)
                pG_ = bank()
                cx.mm(pG_[:, 0:256], t["kq"][hs, 0:128], t["kq"][hs, :], True, True, [t["kq"]], [pG_])
                subs.append(inverse(t, hh, pG_))
            live = list(subs)
            while live:
                for g_ in list(live):
                    try:
                        next(g_)
                    except StopIteration:
                        live.remove(g_)
                yield
            pU = bank()
            for hh in range(2):
                cx.mm(pU[:, hh * 64:(hh + 1) * 64], t["TT2"][:, hh, :], t["vb"][:, hh, :], True, True, [t["TT2"], t["vb"]], [pU])
            cx.mm(pU[:, 128:384], t["kbg"][:].rearrange("p h f -> p (h f)"), t["TT2"][:].rearrange("p h c -> p (h c)"), True, True,
                  [t["kbg"], t["TT2"]], [pU])
            cx.ew("act", lambda e: e.copy(out=t["u"][:], in_=pU[:, 0:128]), [pU], [t["u"]])
            cx.ew("dve", lambda e: e.tensor_copy(out=t["wT"][0:64, :], in_=pU[0:64, 128:256]), [pU], [t["wT"]])
            cx.ew("dve", lambda e: e.tensor_copy(out=t["wT"][64:128, :], in_=pU[64:128, 256:384]), [pU], [t["wT"]])
            yield
            for X_ in ((0, 1) if d == 0 else (1, 0)):
                rows = slice(X_ * 64, X_ * 64 + 64)
                pS1 = bank()
                cx.mm(pS1[:, 0:128], t["wT"][:], t["Sbf"][:], True, True, [t["wT"], t["Sbf"]], [pS1])
                cx.ew("dve", lambda e, rows=rows, pS1=pS1: e.tensor_tensor(out=t["vnew"][rows, :], in0=t["u"][rows, :], in1=pS1[rows, 0:128],
                                                                           op=ALU.subtract), [t["u"], pS1], [t["vnew"]])
                yield
                pS2 = bank()
                cx.mm(pS2[:, 0:128], t["qg"][:], t["Sbf"][:], True, False, [t["qg"], t["Sbf"]], [pS2])
                for hh in range(2):
                    cx.mm(pS2[:, hh * 64:(hh + 1) * 64], t["aT%d" % hh][:], t["vnew"][:, hh * 64:(hh + 1) * 64], False, hh == 1,
                          [t["aT%d" % hh], t["vnew"]], [pS2])
                cx.ew("act", lambda e, rows=rows, pS2=pS2: e.copy(out=t["obuf"][rows, :], in_=pS2[rows, 0:128]), [pS2], [t["obuf"]])
                pS3 = bank()
                for hh in range(2):
                    cx.mm(pS3[:, hh * 64:(hh + 1) * 64], t["kdp%d" % hh][rows, :], t["vnew"][rows, hh * 64:(hh + 1) * 64], True, True,
                          [t["kdp%d" % hh], t["vnew"]], [pS3])
                cx.ew("dve", lambda e, X_=X_, pS3=pS3: e.scalar_tensor_tensor(out=t["Sst"][:], in0=t["Sst"][:], scalar=t["sca"][:, 6 + X_:7 + X_],
                                                                              in1=pS3[:, 0:128], op0=ALU.mult, op1=ALU.add),
                      [t["Sst"], t["sca"], pS3], [t["Sst"]])
                cx.ew("act", lambda e: e.copy(out=t["Sbf"][:], in_=t["Sst"][:]), [t["Sst"]], [t["Sbf"]])
                yield
            cx.dma("sp", sc["OD"].ap()[d, t0:t0 + 128, jp * 128:jp * 128 + 128], t["obuf"][:], [t["obuf"]], [("OD", d, jp, i)])

        gens = {}
        nxt = {k: 0 for k in chains}
        live = True
        rnd = 0
        while live:
            live = False
            rnd += 1
            if rnd % 8 == 0 and getattr(C, "mw_gen", None) is not None:
                next(C.mw_gen, None)
            for (d, jp) in chains:
                g_ = gens.get((d, jp))
                if g_ is None:
                    tau = nxt[(d, jp)]
                    if tau >= NP:
                        continue
                    nxt[(d, jp)] = tau + 1
                    g_ = step(d, jp, tau if d == 0 else (S // 128 - 1 - tau))
                    gens[(d, jp)] = g_
                live = True
                try:
                    next(g_)
                except StopIteration:
                    gens[(d, jp)] = None
        cx.P.flush()


GROUPS = PAIRS


def collective_gather(cx, src, dst, r, w):
    if GROUPS is None:
        return
    cx.P.dma("pool", lambda e: e.collective_compute("AllGather", ALU.bypass, replica_groups=GROUPS,
                                                    ins=[src.ap().opt()], outs=[dst.ap().opt()]),
             _keys(r), _keys(w), grp="cc", inc=1)


def phase_dn_out(cx, C, din, hf):
    sc = cx.dram
    with ExitStack() as st:
        dnw = cx.sb(st, "dnw", [128, 64])
        cx.dma("sp", dnw[:], din["dnw_row"].ap().to_broadcast([128, 64]), [], [dnw])
        of = [cx.sb(st, "of%d" % i, [128, 4, 64]) for i in range(4)]
        ob = [cx.sb(st, "ob%d" % i, [128, 4, 64]) for i in range(4)]
        dzt = [cx.sb(st, "dzt%d" % i, [128, 4, 64]) for i in range(4)]
        sq = [cx.sb(st, "dsq%d" % i, [128, 4, 64]) for i in range(4)]
        ss = [cx.sb(st, "dss%d" % i, [128, 4]) for i in range(4)]
        yb = [cx.sb(st, "yb%d" % i, [128, 4, 64], BF16) for i in range(4)]
        dT = [cx.sb(st, "dT%d" % i, [128, 2, 512], BF16) for i in range(2)]
        tp = [cx.ps(st, "dtp%d" % i, [128, 512], BF16) for i in range(2)]
        tpr = [cx.ps(st, "dtpr%d" % i, [128, 512]) for i in range(2)]
        dR = [cx.sb(st, "dR%d" % i, [128, 2, 128], BF16) for i in range(4)]
        jmat = cx.sb(st, "jmat", [128, 128], BF16)
        jf = cx.sb(st, "jf", [128, 128])
        cx.ew("pool", lambda e: e.memset(jf[:], 1.0), [], [jf])
        cx.ew("pool", lambda e: e.affine_select(out=jf[:], in_=jf[:], pattern=[[1, 128]], compare_op=ALU.is_equal, fill=0.0, base=-127,
                                                channel_multiplier=1), [jf], [jf])
        cx.ew("dve", lambda e: e.tensor_copy(out=jmat[:], in_=jf[:]), [jf], [jmat])
        for i in range(S // 128):
            b = i % 4
            t0 = i * 128
            g = i // 4
            gb = g % 2
            cx.dma("sp", of[b][:], sc["OD"].ap()[0, t0:t0 + 128, :].rearrange("p (h f) -> p h f", f=64), [], [of[b]])
            cx.dma("sp", ob[b][:], sc["OD"].ap()[1, t0:t0 + 128, :].rearrange("p (h f) -> p h f", f=64), [], [ob[b]])
            cx.dma("sp", dzt[b][:], sc["DZS"].ap()[t0:t0 + 128, :].rearrange("p (h f) -> p h f", f=64), [], [dzt[b]])
            cx.ew("pool", lambda e, b=b: e.tensor_tensor(out=of[b][:], in0=of[b][:], in1=ob[b][:], op=ALU.add), [of[b], ob[b]], [of[b]])
            cx.ew("pool", lambda e, b=b: e.tensor_tensor(out=sq[b][:], in0=of[b][:], in1=of[b][:], op=ALU.mult), [of[b]], [sq[b]])
            cx.ew("dve", lambda e, b=b: e.reduce_sum(out=ss[b][:], in_=sq[b][:], axis=AX.X), [sq[b]], [ss[b]])
            cx.actv(ss[b][:], ss[b][:], AF.Sqrt, [ss[b], C.eps], [ss[b]], bias=C.eps[:], scale=1.0 / 64)
            cx.ew("dve", lambda e, b=b: e.reciprocal(out=ss[b][:], in_=ss[b][:]), [ss[b]], [ss[b]])
            cx.ew("pool", lambda e, b=b: e.tensor_tensor(out=dzt[b][:], in0=dzt[b][:], in1=dnw[:].unsqueeze(1).to_broadcast([128, 4, 64]),
                                                         op=ALU.mult), [dzt[b], dnw], [dzt[b]])
            cx.ew("dve", lambda e, b=b: e.tensor_tensor(out=of[b][:], in0=of[b][:], in1=ss[b][:].unsqueeze(2).to_broadcast([128, 4, 64]),
                                                        op=ALU.mult), [of[b], ss[b]], [of[b]])
            cx.ew("dve", lambda e, b=b: e.tensor_tensor(out=yb[b][:], in0=of[b][:], in1=dzt[b][:], op=ALU.mult), [of[b], dzt[b]], [yb[b]])
            j = i % 4
            for jp in range(2 if i < 32 else 0):
                cx.tr(tp[jp][:, j * 128:(j + 1) * 128], yb[b][:, jp * 2:jp * 2 + 2, :].rearrange("p h f -> p (h f)"), C.identb[:],
                      [yb[b], C.identb], [tp[jp]])
            if i < 32:
                if j == 3:
                    for jp in range(2):
                        cx.ew("act", lambda e, jp=jp, gb=gb: e.copy(out=dT[gb][:, jp, :], in_=tp[jp][:]), [tp[jp]], [dT[gb]])
                    cx.dma("sp", sc["DNT"].ap()[:, g * 512:(g + 1) * 512].rearrange("(jp p) t -> p jp t", p=128), dT[gb][:], [dT[gb]],
                           ["DNT"])
            else:
                for jp in range(2):
                    cx.mm(tpr[jp][:, 0:128], yb[b][:, jp * 2:jp * 2 + 2, :].rearrange("p h f -> p (h f)"), jmat[:], True, True,
                          [yb[b], jmat], [tpr[jp]])
                    cx.ew("act", lambda e, jp=jp, b=b: e.copy(out=dR[b][:, jp, :], in_=tpr[jp][:, 0:128]), [tpr[jp]], [dR[b]])
                r0 = (63 - i) * 128
                cx.dma("sp", sc["DNTR"].ap()[:, r0:r0 + 128].rearrange("(jp p) t -> p jp t", p=128), dR[b][:], [dR[b]], ["DNTR"])
        if GROUPS is not None:
            collective_gather(cx, sc["DNTR"], sc["DNGR"], ["DNTR"], ["DNGR"])
        cx.P.flush()


def merge_weights(cx, C, din, st):
    mw = {"wg": cx.sb(st, "wg", [128, 8, 2048], BF16), "wau": cx.sb(st, "wau", [128, 4, 1024], BF16),
          "wdu": cx.sb(st, "wdu", [128, 4, 1024], BF16), "wo": cx.sb(st, "wo", [128, 8, 1024], BF16),
          "wr": cx.sb(st, "wr", [128, 8, 16])}
    stg = [cx.sb(st, "mstg%d" % i, [128, 1024]) for i in range(2)]
    C.mw = mw

    def gen():
        cx.dma("sp", mw["wr"][:], din["w_router"].ap().rearrange("(k p) e -> p k e", p=128), [], [mw["wr"]])
        i = 0
        for (name, key, nk, ncols) in (("w_gates", "wg", 8, 2048), ("w_attn_up", "wau", 4, 1024), ("w_dn_up", "wdu", 4, 1024),
                                       ("w_o", "wo", 8, 1024)):
            src = din[name].ap().rearrange("(k p) f -> p k f", p=128)
            for k in range(nk):
                for c0 in range(0, ncols, 1024):
                    sg = stg[i % 2]
                    cx.dma("sp", sg[:], src[:, k, c0:c0 + 1024], [], [sg])
                    if i % 2:
                        cx.ew("act", lambda e, sg=sg, key=key, k=k, c0=c0: e.copy(out=mw[key][:, k, c0:c0 + 1024], in_=sg[:]), [sg], [mw[key]])
                    else:
                        cx.ew("dve", lambda e, sg=sg, key=key, k=k, c0=c0: e.tensor_copy(out=mw[key][:, k, c0:c0 + 1024], in_=sg[:]),
                              [sg], [mw[key]])
                    i += 1
                    yield
    C.mw_gen = gen()


def phase_merge(cx, C, din, hf, out_x):
    sc = cx.dram
    NC_ = 4096 // 512
    if "merge_small" in SKIP:
        NC_ = 1
    with ExitStack() as st:
        xa = [cx.sb(st, "mx%d" % i, [128, 1024]) for i in range(2)]
        wg, wau, wdu, wo, wr = (C.mw[k] for k in ("wg", "wau", "wdu", "wo", "wr"))
        for _ in C.mw_gen:
            pass
        hTc = [cx.sb(st, "mh%d" % i, [128, 8, 512], BF16) for i in range(2)]
        aTc = [cx.sb(st, "ma%d" % i, [128, 4, 512], BF16) for i in range(2)]
        dTc = [cx.sb(st, "md%d" % i, [128, 4, 512], BF16) for i in range(2)]
        dPr = [cx.sb(st, "mdp%d" % i, [128, 4, 512], BF16) for i in range(2)]
        sel = cx.sb(st, "sel", [128, 2])
        cx.dma("sp", sel[:], din["sel"].ap().to_broadcast([128, 2]), [], [sel])
        mT = [cx.sb(st, "mT%d" % i, [128, 8, 512], BF16) for i in range(2)]
        sga = [cx.sb(st, "sga%d" % i, [128, 512]) for i in range(2)]
        sgd = [cx.sb(st, "sgd%d" % i, [128, 512]) for i in range(2)]
        m1 = [cx.sb(st, "m1_%d" % i, [128, 512]) for i in range(2)]
        m2 = [cx.sb(st, "m2_%d" % i, [128, 512]) for i in range(2)]
        x1 = [cx.sb(st, "x1_%d" % i, [128, 1024]) for i in range(2)]
        tmpx = [cx.sb(st, "tmpx%d" % i, [128, 512]) for i in range(2)]
        ssq = [cx.sb(st, "mssq%d" % i, [128, 1]) for i in range(2)]
        rs = [cx.sb(st, "mrs%d" % i, [128, 1]) for i in range(2)]
        h2f = [cx.sb(st, "h2f%d" % i, [128, 1024]) for i in range(2)]
        h2x = [cx.sb(st, "h2x%d" % i, [128, 529]) for i in range(2)]
        h2T = [cx.sb(st, "h2T%d" % i, [128, 8, 128]) for i in range(2)]
        lg = [cx.sb(st, "lg%d" % i, [128, 16]) for i in range(2)]
        sm = [cx.sb(st, "sm%d" % i, [128, 4]) for i in range(2)]
        tid = cx.sb(st, "tid", [128, 32], I32)
        cx.ew("pool", lambda e: e.iota(tid[:], pattern=[[128, 32]], base=0, channel_multiplier=1), [], [tid])
        pg = [cx.ps(st, "pg%d" % i, [128, 512]) for i in range(2)]
        pa = cx.ps(st, "pa", [128, 512])
        pd = cx.ps(st, "pd", [128, 512])
        po = [cx.ps(st, "po%d" % i, [128, 512]) for i in range(2)]
        pt = cx.ps(st, "ptr", [128, 512])
        pl = cx.ps(st, "pl", [128, 512])
        def part2(u, li):
            for q4 in range(2):
                for k4 in range(4):
                    k = q4 * 4 + k4
                    cx.tr(pt[:, k4 * 128:(k4 + 1) * 128], h2f[u][:, k * 128:(k + 1) * 128], C.identf[:], [h2f[u], C.identf], [pt])
                cx.ew("act", lambda e, u=u, q4=q4: e.copy(out=h2T[u][:, q4 * 4:(q4 + 1) * 4, :].rearrange("p k t -> p (k t)"), in_=pt[:]),
                      [pt], [h2T[u]])
            for k in range(8):
                cx.mm(pl[:, 0:16], h2T[u][:, k, :], wr[:, k, :], k == 0, k == 7, [h2T[u], wr], [pl])
            cx.ew("dve", lambda e, u=u: e.reduce_max(out=sm[u][:, 0:1], in_=pl[:, 0:16], axis=AX.X), [pl], [sm[u]])
            cx.ew("dve", lambda e, u=u: e.tensor_scalar(out=sm[u][:, 1:2], in0=sm[u][:, 0:1], scalar1=-1.0, scalar2=None, op0=ALU.mult),
                  [sm[u]], [sm[u]])
            cx.actv(lg[u][:], pl[:, 0:16], AF.Exp, [pl, sm[u]], [lg[u], sm[u]], bias=sm[u][:, 1:2], scale=1.0, accum=sm[u][:, 2:3])
            cx.ew("dve", lambda e, u=u: e.reciprocal(out=sm[u][:, 3:4], in_=sm[u][:, 2:3]), [sm[u]], [sm[u]])
            cx.ew("dve", lambda e, u=u: e.tensor_scalar(out=h2x[u][:, 513:529], in0=lg[u][:], scalar1=sm[u][:, 3:4],
                                                        scalar2=None, op0=ALU.mult), [lg[u], sm[u]], [h2x[u]])
            cx.ew("pool", lambda e, u=u, li=li: e.tensor_copy(out=h2x[u][:, 512:513].bitcast(I32), in_=tid[:, li:li + 1]),
                  [tid], [h2x[u]])
            cx.dma("sp", sc["H2X"].ap()[li * 128:(li + 1) * 128, :], h2x[u][:], [h2x[u]], [("H2X", li)])
            cx.dma("sp", sc["AFF"].ap()[li * 128:(li + 1) * 128, :], h2x[u][:, 513:529], [h2x[u]], ["AFF"])

        xsrc = din["x"].ap()
        npo = 0
        pend2 = None
        for c in range(NC_):
            b = c % 2
            l0 = c * 512
            g0 = l0
            cx.dma("sp", hTc[b][:], sc["HT"].ap()[:, :, g0:g0 + 512], [], [hTc[b]])
            cx.dma("sp", aTc[b][:], sc["ATT"].ap()[:, l0:l0 + 512].rearrange("(k p) t -> p k t", p=128), [], [aTc[b]])
            cx.dma("sp", dTc[b][:, 0:2, :], sc["DNT"].ap()[:, l0:l0 + 512].rearrange("(k p) t -> p k t", p=128), ["DNT"], [dTc[b]])
            cx.dma("sp", dPr[b][:], sc["DNGR"].ap()[:, l0:l0 + 512].rearrange("(k p) t -> p k t", p=128), ["DNGR"], [dPr[b]])
            cx.ew("pool", lambda e, b=b: e.tensor_scalar(out=dPr[b][:, 0:2, :], in0=dPr[b][:, 0:2, :], scalar1=sel[:, 0:1], scalar2=None,
                                                         op0=ALU.mult), [dPr[b], sel], [dPr[b]])
            cx.ew("dve", lambda e, b=b: e.scalar_tensor_tensor(out=dTc[b][:, 2:4, :], in0=dPr[b][:, 2:4, :], scalar=sel[:, 1:2],
                                                                in1=dPr[b][:, 0:2, :], op0=ALU.mult, op1=ALU.add), [dPr[b], sel], [dTc[b]])
            for fo in range(8):
                s = fo % 2
                for (gi, dst) in ((0, sga[s]), (1, sgd[s])):
                    p = pg[gi]
                    for k in range(8):
                        cx.mm(p[:], wg[:, k, gi * 1024 + fo * 128:gi * 1024 + (fo + 1) * 128], hTc[b][:, k, :], k == 0, k == 7,
                              [wg, hTc[b]], [p])
                    cx.actv(dst[:], p[:], AF.Sigmoid, [p], [dst])
                for k in range(4):
                    cx.mm(pa[:], wau[:, k, fo * 128:(fo + 1) * 128], aTc[b][:, k, :], k == 0, k == 3, [wau, aTc[b]], [pa])
                for k in range(4):
                    cx.mm(pd[:], wdu[:, k, fo * 128:(fo + 1) * 128], dTc[b][:, k, :], k == 0, k == 3, [wdu, dTc[b]], [pd])
                cx.ew("dve", lambda e, s=s: e.tensor_tensor(out=m1[s][:], in0=pa[:], in1=sga[s][:], op=ALU.mult), [pa, sga[s]], [m1[s]])
                cx.ew("dve", lambda e, s=s: e.tensor_tensor(out=m2[s][:], in0=pd[:], in1=sgd[s][:], op=ALU.mult), [pd, sgd[s]], [m2[s]])
                cx.ew("pool", lambda e, s=s, b=b, fo=fo: e.tensor_tensor(out=mT[b][:, fo, :], in0=m1[s][:], in1=m2[s][:], op=ALU.add),
                      [m1[s], m2[s]], [mT[b]])
            for j in range(4):
                u = j % 2
                li = c * 4 + j
                cx.dma("sp", xa[u][:], xsrc[g0 + j * 128:g0 + (j + 1) * 128, :], [], [xa[u]])
                for n in range(2):
                    p = po[npo % 2]
                    npo += 1
                    for k in range(8):
                        cx.mm(p[:], mT[b][:, k, j * 128:(j + 1) * 128], wo[:, k, n * 512:(n + 1) * 512], k == 0, k == 7, [mT[b], wo], [p])
                    cx.ew("dve", lambda e, p=p, n=n, u=u: e.tensor_tensor(out=tmpx[n][:], in0=p[:], in1=C.gtrow[:, n * 512:(n + 1) * 512],
                                                                          op=ALU.mult), [p, C.gtrow], [tmpx[n]])
                    cx.ew("pool", lambda e, n=n, u=u: e.tensor_tensor(out=x1[u][:, n * 512:(n + 1) * 512], in0=tmpx[n][:],
                                                                      in1=xa[u][:, n * 512:(n + 1) * 512], op=ALU.add),
                          [tmpx[n], xa[u]], [x1[u]])
                cx.dma("sp", sc["ACC"].ap()[li * 128:(li + 1) * 128, :], x1[u][:], [x1[u]], [("OUTX", li)])
                cx.actv(h2f[u][:], x1[u][:], AF.Square, [x1[u]], [h2f[u], ssq[u]], accum=ssq[u][:])
                cx.actv(rs[u][:], ssq[u][:], AF.Sqrt, [ssq[u], C.eps], [rs[u]], bias=C.eps[:], scale=1.0 / D)
                cx.ew("dve", lambda e, u=u: e.reciprocal(out=rs[u][:], in_=rs[u][:]), [rs[u]], [rs[u]])
                cx.ew("dve", lambda e, u=u: e.scalar_tensor_tensor(out=h2f[u][:], in0=x1[u][:], scalar=rs[u][:, 0:1], in1=C.gtrow[:, 3072:4096],
                                                                   op0=ALU.mult, op1=ALU.mult), [x1[u], rs[u], C.gtrow], [h2f[u]])
                cx.ew("pool", lambda e, u=u: e.tensor_tensor(out=h2f[u][:], in0=h2f[u][:], in1=C.gtrow[:, 2048:3072], op=ALU.add),
                      [h2f[u], C.gtrow], [h2f[u]])
                cx.ew("act", lambda e, u=u: e.copy(out=h2x[u][:, 0:512].bitcast(BF16), in_=h2f[u][:]), [h2f[u]], [h2x[u]])
                if pend2 is not None:
                    pend2()
                pend2 = (lambda u=u, li=li: part2(u, li))
            if pend2 is not None:
                pend2()
                pend2 = None
        if "merge_small" in SKIP:
            pass
        elif GROUPS is None:
            cx.dma("sp", sc["AFFG"].ap()[0:4096, :], sc["AFF"].ap(), ["AFF"], ["AFFG"])
        else:
            collective_gather(cx, sc["AFF"], sc["AFFG"], ["AFF"], ["AFFG"])
        cx.P.flush()


BIGI = 1 << 20


def moe_prefill(cx, C):
    sc = cx.dram
    zrow = cx.sb(cx.stack, "zrow", [128, 529])
    cx.ew("pool", lambda e: e.memset(zrow[:], 0.0), [], [zrow])
    cx.ew("pool", lambda e: e.iota(zrow[:, 512:513].bitcast(I32), pattern=[[0, 1]], base=4096, channel_multiplier=1), [zrow], [zrow])
    for e_ in range(16):
        cx.dma("sp", sc["XE%d" % e_].ap()[0:1024, :].rearrange("(n p) f -> p n f", p=128), zrow[:].unsqueeze(1).to_broadcast([128, 8, 529]),
               [zrow], [("XE", e_)])


def phase_moe(cx, C, din, out_x):
    sc = cx.dram
    NE = 16 if "moe_small" not in SKIP else 2
    NIT = 32
    with ExitStack() as st:
        ones = cx.sb(st, "ones_moe", [128, 128])
        slt = cx.sb(st, "slt", [128, 128])
        cx.ew("pool", lambda e: e.memset(ones[:], 1.0), [], [ones])
        cx.ew("pool", lambda e: e.memset(slt[:], 1.0), [], [slt])
        cx.ew("pool", lambda e: e.affine_select(out=slt[:], in_=slt[:], pattern=[[1, 128]], compare_op=ALU.is_gt, fill=0.0, base=0,
                                                channel_multiplier=-1), [slt], [slt])
        desti = cx.sb(st, "desti", [128, 32, 16], I32)
        wsrc = {"g": din["w_gate"], "u": din["w_up"], "d": din["w_down"]}
        wt = {k: [cx.sb(st, "w%s%d" % (k, i), [128, 8, 1024], BF16) for i in range(2)] for k in "gud"}
        stg = [cx.sb(st, "estg%d" % i, [128, 1, 1024]) for i in range(3)]
        nstc = [0]

        hbuf = [cx.sb(st, "hbuf%d" % i, [128, 529]) for i in range(4)]
        nhb = [0]

        def scatter(e_, i):
            hb = hbuf[nhb[0] % 4]
            nhb[0] += 1
            cx.dma("sp", hb[:], sc["H2X"].ap()[i * 128:(i + 1) * 128, :], [], [hb])
            cx.P.dma("pool", lambda e, i=i, e_=e_, hb=hb: e.indirect_dma_start(
                out=sc["XE%d" % e_].ap(), out_offset=bass.IndirectOffsetOnAxis(ap=desti[:, i, e_:e_ + 1], axis=0),
                in_=hb[:], in_offset=None),
                _keys([desti, hb, ("XE", e_)]), _keys([("XEs", e_, i)]))

        def load_w_gen(e_):
            b = e_ % 2
            pend = None
            for k_ in "gud":
                src = wsrc[k_].ap()[e_].rearrange("(k p) f -> p k f", p=128)
                for c4 in range(8):
                    nst = nstc[0]
                    nstc[0] += 1
                    sg = stg[nst % 3]
                    cx.dma("sp", sg[:], src[:, c4:c4 + 1, :], [], [sg])
                    if pend is not None:
                        pend()

                    def cast(sg=sg, k_=k_, c4=c4, nst=nst):
                        if nst % 2:
                            cx.ew("act", lambda e: e.copy(out=wt[k_][b][:, c4:c4 + 1, :], in_=sg[:]), [sg], [wt[k_][b]])
                        else:
                            cx.ew("dve", lambda e: e.tensor_copy(out=wt[k_][b][:, c4:c4 + 1, :], in_=sg[:]), [sg], [wt[k_][b]])
                    pend = cast
                    yield
            pend()

        for _ in load_w_gen(0):
            pass
        pdum = cx.sb(st, "pdum", [128, 32])
        pdi = cx.sb(st, "pdi", [128, 32], I32)
        cx.ew("pool", lambda e: e.iota(pdi[:], pattern=[[128, 32]], base=1024, channel_multiplier=1), [], [pdi])
        cx.ew("dve", lambda e: e.tensor_copy(out=pdum[:], in_=pdi[:]), [pdi], [pdum])
        with ExitStack() as st2:
            affg = cx.sb(st2, "affg", [128, 64, 16])
            cmp_ = cx.sb(st2, "cmp", [128, 64, 16])
            affo = cx.sb(st2, "affo", [128, 32, 16])
            msk = cx.sb(st2, "msk", [128, 32, 16])
            pos = TL(None, cmp_.k, view=cmp_[:, 0:32, :])
            csum = TL(None, cmp_.k, view=cmp_[:, 32:64, :])
            bef = cx.sb(st2, "bef", [128, 32, 16])
            eoff = cx.sb(st2, "eoff", [128, 16])
            lo = cx.sb(st2, "lo", [128, 16])
            hi = cx.sb(st2, "hi", [128, 16])
            mid = cx.sb(st2, "mid", [128, 16])
            cnt = cx.sb(st2, "cnt", [128, 16])
            ge = cx.sb(st2, "ge", [128, 16])
            d1 = cx.sb(st2, "d1", [128, 16])
            d2 = cx.sb(st2, "d2", [128, 16])
            ptot = cx.ps(st2, "ptot", [128, 512])
            pwi = cx.ps(st2, "pwi", [128, 512])
            pcs = cx.ps(st2, "pcs", [128, 512])
            cx.dma("sp", affg[:], sc["AFFG"].ap().rearrange("(p j) e -> p j e", p=128), ["AFFG"], [affg])
            cx.dma("sp", affo[:], sc["AFF"].ap().rearrange("(i p) e -> p i e", p=128), ["AFF"], [affo])
            cx.dma("sp", eoff[:], din["eoff"].ap().to_broadcast([128, 16]), [], [eoff])
            cx.ew("dve", lambda e: e.memset(lo[:], 0.0), [], [lo])
            for it in range(NIT):
                cj = 2.0 ** -(it + 1)
                cx.ew("dve", lambda e, cj=cj: e.tensor_scalar(out=mid[:], in0=lo[:], scalar1=cj, scalar2=None, op0=ALU.add), [lo], [mid])
                cx.ew("dve", lambda e: e.tensor_tensor(out=cmp_[:], in0=affg[:], in1=mid[:].unsqueeze(1).to_broadcast([128, 64, 16]),
                                                       op=ALU.is_gt), [affg, mid], [cmp_])
                cx.ew("dve", lambda e: e.reduce_sum(out=cnt[:], in_=cmp_[:].rearrange("p j e -> p e j"), axis=AX.X), [cmp_], [cnt])
                cx.mm(ptot[:, 0:16], ones[:], cnt[:], True, True, [ones, cnt], [ptot])
                cx.ew("dve", lambda e, cj=cj: e.tensor_scalar(out=ge[:], in0=ptot[:, 0:16], scalar1=1024.0, scalar2=cj, op0=ALU.is_ge,
                                                              op1=ALU.mult), [ptot], [ge])
                cx.ew("dve", lambda e: e.tensor_tensor(out=lo[:], in0=lo[:], in1=ge[:], op=ALU.add), [lo, ge], [lo])
            cx.ew("dve", lambda e: e.tensor_tensor(out=msk[:], in0=affo[:], in1=lo[:].unsqueeze(1).to_broadcast([128, 32, 16]), op=ALU.is_gt),
                  [affo, lo], [msk])
            cx.mm(pwi[:], slt[:], msk[:].rearrange("p i e -> p (i e)"), True, True, [slt, msk], [pwi])
            cx.mm(pcs[:], ones[:], msk[:].rearrange("p i e -> p (i e)"), True, True, [ones, msk], [pcs])
            cx.ew("dve", lambda e: e.tensor_copy(out=csum[:].rearrange("p i e -> p (i e)"), in_=pcs[:]), [pcs], [csum])
            cx.ew("dve", lambda e: e.memset(bef[:, 0, :], 0.0), [], [bef])
            for i in range(1, 32):
                cx.ew("dve", lambda e, i=i: e.tensor_tensor(out=bef[:, i, :], in0=bef[:, i - 1, :], in1=csum[:, i - 1, :], op=ALU.add),
                      [bef, csum], [bef])
            cx.ew("dve", lambda e: e.tensor_tensor(out=pos[:].rearrange("p i e -> p (i e)"), in0=pwi[:],
                                                   in1=bef[:].rearrange("p i e -> p (i e)"), op=ALU.add), [pwi, bef], [pos])
            cx.ew("dve", lambda e: e.tensor_scalar(out=csum[:], in0=pos[:], scalar1=1024.0, scalar2=None, op0=ALU.is_lt), [pos], [csum])
            cx.ew("dve", lambda e: e.tensor_tensor(out=msk[:], in0=msk[:], in1=csum[:], op=ALU.mult), [msk, csum], [msk])
            cx.ew("dve", lambda e: e.tensor_tensor(out=pos[:], in0=pos[:], in1=pdum[:].unsqueeze(2).to_broadcast([128, 32, 16]),
                                                   op=ALU.subtract), [pos, pdum], [pos])
            cx.ew("dve", lambda e: e.tensor_tensor(out=pos[:], in0=pos[:], in1=msk[:], op=ALU.mult), [pos, msk], [pos])
            cx.ew("dve", lambda e: e.tensor_tensor(out=pos[:], in0=pos[:], in1=pdum[:].unsqueeze(2).to_broadcast([128, 32, 16]),
                                                   op=ALU.add), [pos, pdum], [pos])
            cx.ew("dve", lambda e: e.tensor_copy(out=desti[:], in_=pos[:]), [pos], [desti])
            if cx.debug:
                cx.dma("sp", sc["DBGM"].ap()[:, 0:16], lo[:], [lo], ["dbgm1"])
                cx.dma("sp", sc["DBGD"].ap().rearrange("(i p) e -> p i e", p=128), desti[:], [desti], ["dbgm2"])
            cx.P.flush()
        xeh = cx.sb(st, "xeh", [128, 8, 512])
        xem = [cx.sb(st, "xem%d" % i, [128, 8, 17]) for i in range(2)]
        xeT = cx.sb(st, "xeT", [128, 8, 1024], BF16)
        aT = cx.sb(st, "aTe", [128, 8, 1024], BF16)
        act_ = [cx.sb(st, "eact%d" % i, [128, 512]) for i in range(2)]
        yg = [cx.sb(st, "yg%d" % i, [128, 1024]) for i in range(2)]
        ptr = [cx.ps(st, "eptr%d" % i, [128, 1024], BF16) for i in range(2)]
        pg = [cx.ps(st, "epg%d" % i, [128, 512]) for i in range(2)]
        pu = [cx.ps(st, "epu%d" % i, [128, 512]) for i in range(2)]
        py = [cx.ps(st, "epy%d" % i, [128, 512]) for i in range(2)]
        nst = 0
        n1 = 0
        n2 = 0
        for i in range(32):
            scatter(0, i)
        for e_ in range(NE):
            b = e_ % 2
            nxtw = load_w_gen(e_ + 1) if e_ + 1 < NE else iter(())
            nxts = iter([(e_ + 1, i) for i in range(32)] if e_ + 1 < NE else [])
            xsrc_ = sc["XE%d" % e_].ap()[0:1024, :].rearrange("(s p) f -> p s f", p=128)
            sdeps = [("XE", e_)] + [("XEs", e_, i) for i in range(32)]
            cx.dma("sp", xeh[:], xsrc_[:, :, 0:512], sdeps, [xeh])
            cx.dma("sp", xem[b][:], xsrc_[:, :, 512:529], sdeps, [xem[b]])
            for s_ in range(8):
                pt_ = ptr[s_ % 2]
                xb_ = xeh[:, s_, :].bitcast(BF16)
                for k in range(8):
                    cx.tr(pt_[:, k * 128:(k + 1) * 128], xb_[:, k * 128:(k + 1) * 128], C.identb[:], [xeh, C.identb], [pt_])
                cx.ew("act" if s_ % 2 else "dve", lambda e, pt_=pt_, s_=s_: (e.copy if hasattr(e, "copy") else e.tensor_copy)(
                    out=xeT[:, :, s_ * 128:(s_ + 1) * 128], in_=pt_[:].rearrange("p (k t) -> p k t", k=8)), [pt_], [xeT])
            for fo in range(8):
                for hf_ in range(2):
                    g_, u_ = pg[n1 % 2], pu[n1 % 2]
                    a_ = act_[n1 % 2]
                    n1 += 1
                    cs = slice(hf_ * 512, (hf_ + 1) * 512)
                    for k in range(8):
                        cx.mm(g_[:], wt["g"][b][:, k, fo * 128:(fo + 1) * 128], xeT[:, k, cs], k == 0, k == 7, [wt["g"][b], xeT], [g_])
                    for k in range(8):
                        cx.mm(u_[:], wt["u"][b][:, k, fo * 128:(fo + 1) * 128], xeT[:, k, cs], k == 0, k == 7, [wt["u"][b], xeT], [u_])
                    cx.actv(a_[:], g_[:], AF.Silu, [g_], [a_])
                    cx.ew("dve", lambda e, a_=a_, u_=u_, fo=fo, cs=cs: e.tensor_tensor(out=aT[:, fo, cs], in0=u_[:], in1=a_[:], op=ALU.mult),
                          [u_, a_], [aT])
                    next(nxtw, None)
                    for _ in range(2):
                        sx = next(nxts, None)
                        if sx is not None:
                            scatter(*sx)
            for s_ in range(8):
                y_ = yg[s_ % 2]
                for hf_ in range(2):
                    p_ = py[n2 % 2]
                    n2 += 1
                    for fo in range(8):
                        cx.mm(p_[:], aT[:, fo, s_ * 128:(s_ + 1) * 128], wt["d"][b][:, fo, hf_ * 512:(hf_ + 1) * 512], fo == 0, fo == 7,
                              [aT, wt["d"][b]], [p_])
                    cx.ew("dve", lambda e, p_=p_, y_=y_, hf_=hf_, s_=s_, b=b, e_=e_: e.scalar_tensor_tensor(
                        out=y_[:, hf_ * 512:(hf_ + 1) * 512], in0=p_[:], scalar=xem[b][:, s_, 1 + e_:2 + e_],
                        in1=C.gtrow[:, 1024 + hf_ * 512:1024 + (hf_ + 1) * 512], op0=ALU.mult, op1=ALU.mult),
                        [p_, xem[b], C.gtrow], [y_])
                cx.P.dma("pool", lambda e, y_=y_, s_=s_, b=b: e.indirect_dma_start(
                    out=sc["ACC"].ap(), out_offset=bass.IndirectOffsetOnAxis(ap=xem[b][:, s_, 0:1].bitcast(I32), axis=0),
                    in_=y_[:], in_offset=None, compute_op=ALU.add),
                    _keys([y_, xem[b]]), _keys(["OUTACC"]))
                next(nxtw, None)
            for _ in nxtw:
                pass
        cx.P.flush()


_CACHE = {}


def kernel(**inputs):
    inp = {k: np.asarray(v) for k, v in inputs.items()}
    if "nc" not in _CACHE:
        _CACHE["nc"] = build()
        _CACHE["tabs"] = const_tables()
    nc = _CACHE["nc"]
    tabs = _CACHE["tabs"]
    in_maps = []
    for core in range(8):
        ci = core_inputs(inp, core, tabs)
        in_maps.append({k: np.ascontiguousarray(ci[k], dtype=np.float32) for k in INPUT_SHAPES})
    res = run_bass_kernel_spmd(nc, in_maps, core_ids=list(range(8)))
    out = np.empty((4, S, D), np.float32)
    for core in range(8):
        b, hf = core // 2, core % 2
        o = np.asarray(res.results[core]["out_x"], dtype=np.float32)[:4096]
        if hf == 0:
            out[b, :4096] = o
        else:
            out[b, 4096:] = o[::-1]
    return out
```
